# Optimizing a Trainium2 kernel written in Bass

```python
import math
import jax, jax.numpy as jnp
from jax import lax
import numpy as np

D_MODEL = 2048
BATCH = 4
SEQ = 4096
DEPTH = 1

PLE_DIM = 256
N_HEADS = 16
HEAD_DIM = 128
ATTN_WIDTH = N_HEADS * HEAD_DIM
MOBA_BLOCK = 256
MOBA_TOPK = 3
QUERY_CHUNK = 16
N_BUCKETS = 32
MAX_DISTANCE = 128
CONV_WIDTH = D_MODEL
CONV_KERNEL = 31
N_GROUPS = 4
EXPERTS_PER_GROUP = 4
N_EXPERTS = N_GROUPS * EXPERTS_PER_GROUP
EXPERT_TOPK = 2
EXPERT_FF = 512
IN_COLS = 3 * ATTN_WIDTH + 2 * CONV_WIDTH + 2 * D_MODEL
EPS = 1e-6
NEG = -1e30

kernel_name = "hybrid_moba_conformer_hmoe_block"


def rmsnorm(x, g):
    x32 = x.astype(jnp.float32)
    y = x32 * lax.rsqrt(jnp.mean(x32 * x32, axis=-1, keepdims=True) + EPS)
    return (y * g.astype(jnp.float32)).astype(x.dtype)


def layernorm(x, g, b):
    x32 = x.astype(jnp.float32)
    mu = jnp.mean(x32, axis=-1, keepdims=True)
    xc = x32 - mu
    var = jnp.mean(xc * xc, axis=-1, keepdims=True)
    y = xc * lax.rsqrt(var + EPS) * g.astype(jnp.float32) + b.astype(jnp.float32)
    return y.astype(x.dtype)


def rel_bucket(dist):
    n = jnp.maximum(dist, 0)
    max_exact = N_BUCKETS // 2
    nf = jnp.maximum(n, 1).astype(jnp.float32)
    large = max_exact + (jnp.log(nf / max_exact) / math.log(MAX_DISTANCE / max_exact)
                         * (N_BUCKETS - max_exact)).astype(jnp.int32)
    large = jnp.minimum(large, N_BUCKETS - 1)
    return jnp.where(n < max_exact, n, large)


def moba_attention(q, k, v, rel_bias):
    B, S = q.shape[0], q.shape[1]
    S_pad = -(-S // MOBA_BLOCK) * MOBA_BLOCK
    pad = S_pad - S
    q, k, v = [jnp.pad(t, ((0, 0), (0, pad), (0, 0), (0, 0))).transpose(0, 2, 1, 3) for t in (q, k, v)]
    NB = S_pad // MOBA_BLOCK
    kb = k.reshape(B, N_HEADS, NB, MOBA_BLOCK, HEAD_DIM)
    vb = v.reshape(B, N_HEADS, NB, MOBA_BLOCK, HEAD_DIM)
    kmean = jnp.mean(kb.astype(jnp.float32), axis=3)
    gate = jnp.einsum('bhsd,bhnd->bhsn', q.astype(jnp.float32), kmean)
    pos = jnp.arange(S_pad)
    qblk = pos // MOBA_BLOCK
    past = jnp.arange(NB)[None, :] < qblk[:, None]
    gate = jnp.where(past, gate, NEG)
    n_sel = min(MOBA_TOPK, NB)
    _, sel_idx = lax.top_k(gate, n_sel)
    sel_valid = sel_idx < qblk[:, None]

    NC = S_pad // QUERY_CHUNK

    def chunks(t):
        return jnp.moveaxis(t.reshape(B, N_HEADS, NC, QUERY_CHUNK, *t.shape[3:]), 2, 0)

    bias_table = rel_bias.astype(jnp.float32).T
    hi = jnp.arange(N_HEADS)
    bi = jnp.arange(B)
    scale = HEAD_DIM ** -0.5
    blk_off = jnp.arange(MOBA_BLOCK)

    def chunk_attn(args):
        q_c, idx_c, valid_c, n = args
        t = n * QUERY_CHUNK + jnp.arange(QUERY_CHUNK)
        c = (n * QUERY_CHUNK) // MOBA_BLOCK
        k_own = lax.dynamic_index_in_dim(kb, c, axis=2, keepdims=False)
        v_own = lax.dynamic_index_in_dim(vb, c, axis=2, keepdims=False)
        rel_own = t[:, None] - (c * MOBA_BLOCK + blk_off)[None, :]
        l_own = (jnp.einsum('bhqd,bhkd->bhqk', q_c, k_own).astype(jnp.float32) * scale
                 + bias_table[:, rel_bucket(rel_own)])
        l_own = jnp.where(rel_own >= 0, l_own, NEG)
        k_sel = kb[bi[:, None, None, None], hi[None, :, None, None], idx_c]
        v_sel = vb[bi[:, None, None, None], hi[None, :, None, None], idx_c]
        sel_pos = idx_c[..., None] * MOBA_BLOCK + blk_off
        rel_sel = t[:, None, None] - sel_pos
        l_sel = (jnp.einsum('bhqd,bhqnkd->bhqnk', q_c, k_sel).astype(jnp.float32) * scale
                 + bias_table[hi[None, :, None, None, None], rel_bucket(rel_sel)])
        l_sel = jnp.where(valid_c[..., None], l_sel, NEG)
        logits = jnp.concatenate([l_sel.reshape(B, N_HEADS, QUERY_CHUNK, n_sel * MOBA_BLOCK), l_own], axis=-1)
        probs = jax.nn.softmax(logits, axis=-1)
        p_sel = probs[..., :n_sel * MOBA_BLOCK].reshape(B, N_HEADS, QUERY_CHUNK, n_sel, MOBA_BLOCK).astype(v.dtype)
        p_own = probs[..., n_sel * MOBA_BLOCK:].astype(v.dtype)
        return (jnp.einsum('bhqnk,bhqnkd->bhqd', p_sel, v_sel)
                + jnp.einsum('bhqk,bhkd->bhqd', p_own, v_own))

    out = lax.map(chunk_attn, (chunks(q), chunks(sel_idx), chunks(sel_valid), jnp.arange(NC)))
    out = jnp.moveaxis(out, 0, 2).reshape(B, N_HEADS, S_pad, HEAD_DIM)[:, :, :S]
    return out.transpose(0, 2, 1, 3).reshape(B, S, ATTN_WIDTH)


def conformer_conv(u_pre, conv_w, conv_b, ln_g, ln_b, w_out):
    a, g = jnp.split(u_pre, 2, axis=-1)
    u = a * jax.nn.sigmoid(g)
    u = lax.conv_general_dilated(u, conv_w[:, None, :], window_strides=(1,),
                                 padding=((CONV_KERNEL - 1, 0),),
                                 dimension_numbers=('NWC', 'WIO', 'NWC'),
                                 feature_group_count=CONV_WIDTH) + conv_b
    u = jax.nn.silu(layernorm(u, ln_g, ln_b))
    return u @ w_out


def hier_moe(xn, w_rg, b_rg, w_re, b_re, w_gate, w_up, w_down):
    shp = xn.shape
    xt = xn.reshape(-1, D_MODEL)
    cl = (xt @ w_rg).astype(jnp.float32) + b_rg.astype(jnp.float32)
    cp = jax.nn.softmax(cl, axis=-1)
    _, g_star = lax.top_k(cl, 1)
    p_g = jnp.take_along_axis(cp, g_star, axis=-1)
    fl = ((xt @ w_re).astype(jnp.float32) + b_re.astype(jnp.float32)).reshape(-1, N_GROUPS, EXPERTS_PER_GROUP)
    f_sel = jnp.take_along_axis(fl, g_star[:, :, None], axis=1)[:, 0]
    top_v, top_i = lax.top_k(f_sel, EXPERT_TOPK)
    fw = jax.nn.softmax(top_v, axis=-1)
    fine = jnp.einsum('tk,tke->te', fw, jax.nn.one_hot(top_i, EXPERTS_PER_GROUP, dtype=jnp.float32))
    comb = (jax.nn.one_hot(g_star[:, 0], N_GROUPS, dtype=jnp.float32)[:, :, None]
            * (p_g * fine)[:, None, :]).reshape(-1, N_EXPERTS)
    h = jax.nn.silu(jnp.einsum('td,edf->tef', xt, w_gate)) * jnp.einsum('td,edf->tef', xt, w_up)
    y = jnp.einsum('tef,efd->td', h * comb.astype(h.dtype)[:, :, None], w_down)
    return y.reshape(shp)


def setup_inputs(seed: int = 0) -> dict:
    key = jax.random.key(seed)
    ks = jax.random.split(key, 26)
    f32 = jnp.float32

    def nrm(k, shape, scale):
        return jax.random.normal(k, shape, f32) * scale

    L = DEPTH
    return {
        "x": nrm(ks[0], (BATCH, SEQ, D_MODEL), 1.0),
        "p": nrm(ks[1], (DEPTH, BATCH, SEQ, PLE_DIM), 1.0),
        "g_mix": 1.0 + nrm(ks[2], (L, D_MODEL), 0.1),
        "w_in": nrm(ks[3], (L, D_MODEL, IN_COLS), D_MODEL ** -0.5),
        "rel_bias": nrm(ks[4], (N_BUCKETS, N_HEADS), 0.5),
        "w_attn_br": nrm(ks[5], (L, ATTN_WIDTH, D_MODEL), ATTN_WIDTH ** -0.5),
        "conv_w": nrm(ks[6], (L, CONV_KERNEL, CONV_WIDTH), CONV_KERNEL ** -0.5),
        "conv_b": nrm(ks[7], (L, CONV_WIDTH), 0.02),
        "ln_g": 1.0 + nrm(ks[8], (L, CONV_WIDTH), 0.1),
        "ln_b": nrm(ks[9], (L, CONV_WIDTH), 0.02),
        "w_conv_br": nrm(ks[10], (L, CONV_WIDTH, D_MODEL), CONV_WIDTH ** -0.5),
        "w_o": nrm(ks[11], (L, D_MODEL, D_MODEL), D_MODEL ** -0.5),
        "g_ffn": 1.0 + nrm(ks[12], (L, D_MODEL), 0.1),
        "w_router_g": nrm(ks[13], (L, D_MODEL, N_GROUPS), D_MODEL ** -0.5),
        "b_router_g": nrm(ks[14], (L, N_GROUPS), 0.01),
        "w_router_e": nrm(ks[15], (L, D_MODEL, N_EXPERTS), D_MODEL ** -0.5),
        "b_router_e": nrm(ks[16], (L, N_EXPERTS), 0.01),
        "w_e_gate": nrm(ks[17], (L, N_EXPERTS, D_MODEL, EXPERT_FF), D_MODEL ** -0.5),
        "w_e_up": nrm(ks[18], (L, N_EXPERTS, D_MODEL, EXPERT_FF), D_MODEL ** -0.5),
        "w_e_down": nrm(ks[19], (L, N_EXPERTS, EXPERT_FF, D_MODEL), EXPERT_FF ** -0.5),
        "g_ple": 1.0 + nrm(ks[20], (L, D_MODEL), 0.1),
        "w_ple_gate": nrm(ks[21], (L, D_MODEL, D_MODEL), D_MODEL ** -0.5),
        "w_ple_proj": nrm(ks[22], (L, PLE_DIM, D_MODEL), PLE_DIM ** -0.5),
        "g_final": 1.0 + nrm(ks[23], (D_MODEL,), 0.1),
    }


def reference(x, p, g_mix, w_in, rel_bias, w_attn_br, conv_w, conv_b, ln_g, ln_b, w_conv_br, w_o,
              g_ffn, w_router_g, b_router_g, w_router_e, b_router_e, w_e_gate, w_e_up, w_e_down,
              g_ple, w_ple_gate, w_ple_proj, g_final):
    B, S = x.shape[0], x.shape[1]
    h = x
    splits = [ATTN_WIDTH, 2 * ATTN_WIDTH, 3 * ATTN_WIDTH, 3 * ATTN_WIDTH + 2 * CONV_WIDTH]
    for i in range(DEPTH):
        xn = rmsnorm(h, g_mix[i])
        proj = xn @ w_in[i]
        q, k, v, conv_in, gate_logits = jnp.split(proj, splits, axis=-1)
        q = q.reshape(B, S, N_HEADS, HEAD_DIM)
        k = k.reshape(B, S, N_HEADS, HEAD_DIM)
        v = v.reshape(B, S, N_HEADS, HEAD_DIM)
        y_attn = moba_attention(q, k, v, rel_bias) @ w_attn_br[i]
        y_conv = conformer_conv(conv_in, conv_w[i], conv_b[i], ln_g[i], ln_b[i], w_conv_br[i])
        g_attn, g_conv = jnp.split(jax.nn.sigmoid(gate_logits), 2, axis=-1)
        h = h + (g_attn * y_attn + g_conv * y_conv) @ w_o[i]
        h = h + hier_moe(rmsnorm(h, g_ffn[i]), w_router_g[i], b_router_g[i], w_router_e[i],
                         b_router_e[i], w_e_gate[i], w_e_up[i], w_e_down[i])
        h = h + (p[i] @ w_ple_proj[i]) * jax.nn.sigmoid(rmsnorm(h, g_ple[i]) @ w_ple_gate[i])
    return rmsnorm(h, g_final)
```

```python
import math
from contextlib import ExitStack
import numpy as np
import concourse.bass as bass
import concourse.mybir as mybir
from concourse.bass_utils import run_bass_kernel_spmd

F32 = mybir.dt.float32
BF16 = mybir.dt.bfloat16
AF = mybir.ActivationFunctionType
ALU = mybir.AluOpType
AX = mybir.AxisListType

D = 2048
T = 2048
NEG = -1e30
EPS = 1e-6
SCALE = 128 ** -0.5


class Buf:
    __slots__ = ("name", "w", "r")

    def __init__(self, name):
        self.name = name
        self.w = None
        self.r = {}


class Eng:
    def __init__(self, name, handle, sem):
        self.name = name
        self.h = handle
        self.sem = sem
        self.count = 0
        self.waited = {}


class Emitter:
    def __init__(self, nc, es):
        self.nc = nc
        self.es = es
        self.sems = {}
        self.engs = {}
        for name, h in (("pe", nc.tensor), ("dve", nc.vector), ("act", nc.scalar),
                        ("pool", nc.gpsimd), ("sp", nc.sync)):
            self.sems["sem_" + name] = es.enter_context(nc.semaphore("sem_" + name))
            self.engs[name] = Eng(name, h, "sem_" + name)
        self.dma_cnt = {}
        self.keymap = {}
        self.pool_keys = []

    def _wait(self, e, deps, skip_self=False):
        for key, val in deps:
            if skip_self and key == e.sem:
                continue
            if e.waited.get(key, 0) < val:
                e.h.wait_ge(self.sems[key], val)
                e.waited[key] = val

    @staticmethod
    def _deps(reads, writes):
        deps = {}
        for b in reads:
            if b.w is not None:
                k, v = b.w
                if deps.get(k, 0) < v:
                    deps[k] = v
        for b in writes:
            if b.w is not None:
                k, v = b.w
                if deps.get(k, 0) < v:
                    deps[k] = v
            for k, v in b.r.items():
                if deps.get(k, 0) < v:
                    deps[k] = v
        return list(deps.items())

    @staticmethod
    def _mark(ev, reads, writes):
        k, v = ev
        for b in reads:
            if b.r.get(k, 0) < v:
                b.r[k] = v
        for b in writes:
            b.w = ev
            b.r = {}

    def op(self, eng, fn, reads=(), writes=()):
        e = self.engs[eng]
        self._wait(e, self._deps(reads, writes), skip_self=(eng == "pe"))
        ins = fn()
        e.count += 1
        ins.then_inc(self.sems[e.sem], 1)
        self._mark((e.sem, e.count), reads, writes)
        return ins

    def dma(self, q, semkey, out, in_, reads=(), writes=()):
        e = self.engs[q]
        if semkey not in self.keymap:
            idx = len(self.keymap)
            if idx >= len(self.pool_keys):
                k = f"dq{idx}"
                self.sems[k] = self.es.enter_context(self.nc.semaphore(k))
                self.dma_cnt[k] = 0
                self.pool_keys.append(k)
            self.keymap[semkey] = self.pool_keys[idx]
        semkey = self.keymap[semkey]
        self._wait(e, self._deps(reads, writes))
        ins = e.h.dma_start(out=out, in_=in_)
        self.dma_cnt[semkey] += 16
        ins.then_inc(self.sems[semkey], 16)
        self._mark((semkey, self.dma_cnt[semkey]), reads, writes)
        return ins

    def barrier(self):
        evs = [(e.sem, e.count) for e in self.engs.values() if e.count > 0]
        evs += [(k, v) for k, v in self.dma_cnt.items() if v > 0]
        for e in self.engs.values():
            self._wait(e, evs)
        self.keymap = {}


class _Stop(Exception):
    pass


def build(debug=False, stop=None):
    nc = bass.Bass("TRN2", target_bir_lowering=False)

    def checkpoint(name):
        if stop == name:
            raise _Stop()

    def din(name, shape, dt=F32):
        return nc.dram_tensor(name, shape, dt, kind="ExternalInput").ap()

    def dscr(name, shape, dt):
        return nc.dram_tensor(name, shape, dt, kind=("ExternalOutput" if (debug and name in debug) else "Internal")).ap()

    xa = din("xa", [4096 + 128, D])
    pp = din("pp", [T, 256])
    w_in = din("w_in", [D, 14336])
    w_attn = din("w_attn_br", [D, D])
    w_conv = din("w_conv_br", [D, D])
    w_o = din("w_o", [D, D])
    w_pg = din("w_ple_gate", [D, D])
    w_pp = din("w_ple_proj", [256, D])
    w_eg = din("w_e_gate", [16, D, 512])
    w_eu = din("w_e_up", [16, D, 512])
    w_ed = din("w_e_down", [16 * 512, D])
    w_r = din("w_r", [D, 20])
    b_r = din("b_r", [1, 20])
    g_mix = din("g_mix", [1, D]); g_ffn = din("g_ffn", [1, D]); g_ple = din("g_ple", [1, D]); g_fin = din("g_final", [1, D])
    conv_wT = din("conv_wT", [128, 16, 31])
    conv_b = din("conv_b", [128, 16]); ln_g = din("ln_g", [128, 16]); ln_b = din("ln_b", [128, 16])
    ident_d = din("ident", [128, 128])
    sel16_d = din("sel16", [16, D])
    bias_self = din("bias_self", [16, 128, 512])
    bias_adj = din("bias_adj", [16, 128, 512])
    t31_d = din("t31", [128, 16])
    pastb_d = din("pastb", [128, 256])
    pastm_d = din("pastm", [128, 256])
    out_d = nc.dram_tensor("out", [T, D], F32, kind="ExternalOutput").ap()

    S_qT = dscr("S_qT", [16, 128, T], BF16)
    S_kT = dscr("S_kT", [16, 128, 4096], BF16)
    S_V = dscr("S_V", [4096, D], BF16)
    S_cT = dscr("S_cT", [16, 128, T], F32)
    S_gaT = dscr("S_gaT", [16, 128, T], F32)
    S_gcT = dscr("S_gcT", [16, 128, T], F32)
    S_z1T = dscr("S_z1T", [16, 128, T], F32)
    S_zT = dscr("S_zT", [16, 128, T], BF16)
    S_h1 = dscr("S_h1", [T, D], F32)
    S_h2 = dscr("S_h2", [T, D], F32)
    S_h3 = dscr("S_h3", [T, D], F32)
    S_HT = dscr("S_HT", [64, 128, T], BF16)
    S_attnT = dscr("S_attnT", [16, 128, T], BF16)
    S_mu = dscr("S_mu", [128, T], F32)
    S_rs = dscr("S_rs", [128, T], F32)
    S_convT = dscr("S_convT", [16, 128, T], BF16)
    S_comb = dscr("S_comb", [128, 16, 16], F32)
    B_d = {k: Buf(k) for k in "qT kT V cT gaT gcT z1T zT h1 h2 h3 HT out attnT mu".split()}

    es = ExitStack()
    if True:
      em = Emitter(nc, es)
      try:

        uid = [0]

        def SB(st, name, shape, dt):
            uid[0] += 1
            return st.enter_context(nc.sbuf_tensor(f"s{uid[0]}_{name}", shape, dt))

        def PS(st, name, shape, dt):
            uid[0] += 1
            return st.enter_context(nc.psum_tensor(f"p{uid[0]}_{name}", shape, dt))

        idf = SB(es, "idf", [128, 128], F32)
        idb = SB(es, "idb", [128, 128], BF16)
        onesf = SB(es, "onesf", [128, 128], F32)
        B_c = Buf("consts")
        em.dma("sp", "ld_c0", idf[:], ident_d[:, :], writes=[B_c])
        em.op("dve", lambda: nc.vector.tensor_copy(out=idb[:], in_=idf[:]), reads=[B_c], writes=[B_c])
        em.op("dve", lambda: nc.vector.memset(onesf[:], 1.0), writes=[B_c])

        def rms_stats(st, src_tile, ntiles, tag):
            xt = [SB(st, f"xs{tag}{i}", [128, D], F32) for i in range(2)]
            Bx = [Buf("xs0"), Buf("xs1")]
            junk = SB(st, f"junk{tag}", [128, D], BF16)
            Bj = Buf("junk")
            ss = SB(st, f"ss{tag}", [128, ntiles], F32)
            rstd = SB(st, f"rstd{tag}", [128, ntiles], F32)
            Bss = Buf("ss")
            em.op("dve", lambda: nc.vector.memset(ss[:], 0.0), writes=[Bss])
            for t in range(ntiles):
                s = t % 2
                em.dma("sp", f"ld_xs{s}", xt[s][:], src_tile(t), writes=[Bx[s]])
                em.op("act", lambda: nc.scalar.activation(out=junk[:], in_=xt[s][:], func=AF.Square, accum_out=ss[:, t:t + 1]),
                      reads=[Bx[s]], writes=[Bj, Bss])
            em.op("dve", lambda: nc.vector.tensor_scalar(out=rstd[:], in0=ss[:], scalar1=1.0 / D, scalar2=EPS, op0=ALU.mult, op1=ALU.add),
                  reads=[Bss], writes=[Bss])
            em.op("act", lambda: nc.scalar.activation(out=rstd[:], in_=rstd[:], func=AF.Sqrt), reads=[Bss], writes=[Bss])
            em.op("dve", lambda: nc.vector.reciprocal(out=rstd[:], in_=rstd[:]), reads=[Bss], writes=[Bss])
            return rstd, Bss

        def norm_T(st, src_tile, ntiles, g_ap, actT, Bact, tag):
            with ExitStack() as s1:
                rstd, Bss = rms_stats(s1, src_tile, ntiles, tag)
                gB = SB(s1, f"gB{tag}", [128, D], F32)
                BgB = Buf("gB")
                em.dma("sp", "ld_g", gB[:], g_ap.partition_broadcast(128), writes=[BgB])
                xt = [SB(s1, f"xt{tag}{i}", [128, D], F32) for i in range(2)]
                Bx = [Buf("xt0"), Buf("xt1")]
                xn = [SB(s1, f"xn{tag}{i}", [128, D], BF16) for i in range(2)]
                Bxn = [Buf("xn0"), Buf("xn1")]
                pt = [PS(s1, f"pt{tag}{i}", [128, 8, 128], BF16) for i in range(2)]
                Bpt = [Buf("pt0"), Buf("pt1")]
                for t in range(ntiles):
                    s = t % 2
                    em.dma("sp", f"ld_xt{s}", xt[s][:], src_tile(t), writes=[Bx[s]])
                    em.op("dve", lambda: nc.vector.scalar_tensor_tensor(out=xn[s][:], in0=xt[s][:], scalar=rstd[:, t:t + 1], in1=gB[:],
                                                                        op0=ALU.mult, op1=ALU.mult),
                          reads=[Bx[s], Bss, BgB], writes=[Bxn[s]])
                    for half in range(2):
                        def f():
                            for j in range(8):
                                c = half * 8 + j
                                ins = nc.tensor.transpose(out=pt[half][:, j, :], in_=xn[s][:, c * 128:(c + 1) * 128], identity=idb[:])
                            return ins
                        em.op("pe", f, reads=[Bxn[s], B_c], writes=[Bpt[half]])
                        dst = actT[:, half * 8:(half + 1) * 8, t * 128:(t + 1) * 128]
                        if half == 0:
                            em.op("act", lambda: nc.scalar.copy(out=dst, in_=pt[half][:, :, :]), reads=[Bpt[half]], writes=[Bact])
                        else:
                            em.op("dve", lambda: nc.vector.tensor_copy(out=dst, in_=pt[half][:, :, :]), reads=[Bpt[half]], writes=[Bact])
                em.barrier()

        class Gemm:
            def __init__(self, st, tag, nbanks=4, nw=3):
                self.wb = [SB(st, f"wb{tag}{i}", [128, 16, 512], BF16) for i in range(nw)]
                self.Bw = [[Buf(f"wb{i}a"), Buf(f"wb{i}b")] for i in range(nw)]
                self.pb = [PS(st, f"pb{tag}{i}", [128, 512], F32) for i in range(nbanks)]
                self.Bp = [Buf(f"pb{i}") for i in range(nbanks)]
                self.nw = nw
                self.bank = 0

            def next_bank(self):
                b = self.bank
                self.bank = (self.bank + 1) % len(self.pb)
                return b

            def wload(self, slot, c0, c1, src):
                part = 0 if c0 == 0 else 1
                em.dma("pool", f"ld_w{slot}_{part}", self.wb[slot][:, :, c0:c1], src.rearrange("(k p) n -> p k n", p=128), writes=[self.Bw[slot][part]])

            def run(self, blocks):
                n = len(blocks)
                for b in range(min(self.nw - 1, n)):
                    blocks[b]["load"](b % self.nw)
                for b in range(n):
                    if b + self.nw - 1 < n:
                        blocks[b + self.nw - 1]["load"]((b + self.nw - 1) % self.nw)
                    blocks[b]["run"](b % self.nw)

            def mm_fm(self, slot, c0, actT, Bact, t0, tn, nk=16):
                bk = self.next_bank()
                pbk = self.pb[bk]
                wbs = self.wb[slot]

                def f():
                    for kc in range(nk):
                        ins = nc.tensor.matmul(pbk[:, 0:tn], lhsT=wbs[:, kc, c0:c0 + 128], rhs=actT[:, kc, t0:t0 + tn],
                                               start=(kc == 0), stop=(kc == nk - 1))
                    return ins
                em.op("pe", f, reads=self.Bw[slot] + [Bact], writes=[self.Bp[bk]])
                return pbk, self.Bp[bk]

            def mm_tm(self, slot, actT, Bact, t, ncols=512, bk=None, first=True, last=True, nk=16, kofs=0):
                if bk is None:
                    bk = self.next_bank()
                pbk = self.pb[bk]
                wbs = self.wb[slot]

                def f():
                    for kc in range(nk):
                        ins = nc.tensor.matmul(pbk[:, 0:ncols], lhsT=actT[:, kofs + kc, t * 128:(t + 1) * 128], rhs=wbs[:, kc, 0:ncols],
                                               start=(first and kc == 0), stop=(last and kc == nk - 1))
                    return ins
                em.op("pe", f, reads=self.Bw[slot] + [Bact], writes=[self.Bp[bk]])
                return pbk, self.Bp[bk]

        cpy_ctr = [0]
        act_only = [False]

        def evac_copy(out, in_, reads, writes):
            cpy_ctr[0] += 1
            if act_only[0] or cpy_ctr[0] % 2:
                em.op("act", lambda: nc.scalar.copy(out=out, in_=in_), reads=reads, writes=writes)
            else:
                em.op("dve", lambda: nc.vector.tensor_copy(out=out, in_=in_), reads=reads, writes=writes)

        TG4 = [(i * 512, 512) for i in range(4)]

        def kv_blocks(G, st, actT, Bact, tok_off, tgroups, tag):
            kst = [SB(st, f"kst{tag}{i}", [128, 512], BF16) for i in range(2)]
            Bkst = [Buf("kst0"), Buf("kst1")]
            vst = [SB(st, f"vst{tag}{i}", [128, 512], BF16) for i in range(2)]
            Bvst = [Buf("vst0"), Buf("vst1")]
            blocks = []
            cnt = [0, 0]
            for kb in range(4):
                def load(slot, kb=kb):
                    G.wload(slot, 0, 512, w_in[:, 2048 + kb * 512: 2048 + (kb + 1) * 512])

                def run(slot, kb=kb):
                    for sub in range(4):
                        h = kb * 4 + sub
                        for (t0, tn) in tgroups:
                            s = cnt[0] % 2
                            cnt[0] += 1
                            pbk, Bp = G.mm_fm(slot, sub * 128, actT, Bact, t0, tn)
                            evac_copy(kst[s][:, 0:tn], pbk[:, 0:tn], [Bp], [Bkst[s]])
                            em.dma("sp", f"st_k{s}", S_kT[h, :, tok_off + t0:tok_off + t0 + tn], kst[s][:, 0:tn], reads=[Bkst[s]], writes=[B_d["kT"]])
                blocks.append(dict(load=load, run=run))
            for vb in range(4):
                def load(slot, vb=vb):
                    G.wload(slot, 0, 512, w_in[:, 4096 + vb * 512: 4096 + (vb + 1) * 512])

                def run(slot, vb=vb):
                    for t in range(16):
                        s = cnt[1] % 2
                        cnt[1] += 1
                        pbk, Bp = G.mm_tm(slot, actT, Bact, t)
                        evac_copy(vst[s][:], pbk[:, :], [Bp], [Bvst[s]])
                        em.dma("sp", f"st_v{s}", S_V[tok_off + t * 128: tok_off + (t + 1) * 128, vb * 512:(vb + 1) * 512], vst[s][:],
                               reads=[Bvst[s]], writes=[B_d["V"]])
                blocks.append(dict(load=load, run=run))
            return blocks

        with ExitStack() as st:
            actT = SB(st, "actT0", [128, 16, T], BF16)
            Bact = Buf("actT0")
            norm_T(st, lambda t: xa[2048 + t * 128: 2048 + (t + 1) * 128, :], 16, g_mix, actT, Bact, "a")
            G = Gemm(st, "a")
            G.run(kv_blocks(G, st, actT, Bact, 2048, TG4, "a"))
            em.barrier()

        checkpoint("B0")
        with ExitStack() as st:
            TA = T + 128
            actT = SB(st, "actT1", [128, 16, TA], BF16)
            Bact = Buf("actT1")

            def src1(t):
                if t < 16:
                    return xa[t * 128:(t + 1) * 128, :]
                return xa[4096:4096 + 128, :]
            norm_T(st, src1, 17, g_mix, actT, Bact, "b")
            act_only[0] = True
            G = Gemm(st, "b")
            cw = SB(st, "cw", [128, 16, 31], F32); cb = SB(st, "cb", [128, 16], F32)
            Bcw = Buf("cw")
            em.dma("sp", "ld_cw", cw[:], conv_wT[:, :, :], writes=[Bcw])
            em.dma("sp", "ld_cw", cb[:], conv_b[:, :], writes=[Bcw])
            csum = SB(st, "csum", [128, T], F32); csq = SB(st, "csq", [128, T], F32)
            Bcs = Buf("csum"); Bcq = Buf("csq")
            em.op("pool", lambda: nc.gpsimd.memset(csum[:], 0.0), writes=[Bcs])
            em.op("pool", lambda: nc.gpsimd.memset(csq[:], 0.0), writes=[Bcq])

            blocks_other = []
            qst = [SB(st, f"qst{i}", [128, 512], BF16) for i in range(2)]
            Bqst = [Buf("qst0"), Buf("qst1")]
            qcnt = [0]
            for qb in range(4):
                def load(slot, qb=qb):
                    G.wload(slot, 0, 512, w_in[:, qb * 512:(qb + 1) * 512])

                def run(slot, qb=qb):
                    for sub in range(4):
                        h = qb * 4 + sub
                        for (t0, tn) in TG4:
                            s = qcnt[0] % 2
                            qcnt[0] += 1
                            pbk, Bp = G.mm_fm(slot, sub * 128, actT, Bact, t0, tn)
                            evac_copy(qst[s][:, 0:tn], pbk[:, 0:tn], [Bp], [Bqst[s]])
                            em.dma("sp", f"st_q{s}", S_qT[h, :, t0:t0 + tn], qst[s][:, 0:tn], reads=[Bqst[s]], writes=[B_d["qT"]])
                blocks_other.append(dict(load=load, run=run))
            blocks_other += kv_blocks(G, st, actT, Bact, 0, TG4, "b")
            gst = [SB(st, f"gst{i}", [128, 512], F32) for i in range(2)]
            Bgst = [Buf("gst0"), Buf("gst1")]
            gcnt = [0]
            for gb in range(8):
                def load(slot, gb=gb):
                    G.wload(slot, 0, 512, w_in[:, 10240 + gb * 512: 10240 + (gb + 1) * 512])

                def run(slot, gb=gb):
                    for sub in range(4):
                        ch = (gb % 4) * 4 + sub
                        dst = S_gaT if gb < 4 else S_gcT
                        for (t0, tn) in TG4:
                            s = gcnt[0] % 2
                            gcnt[0] += 1
                            pbk, Bp = G.mm_fm(slot, sub * 128, actT, Bact, t0, tn)
                            em.op("act", lambda: nc.scalar.activation(out=gst[s][:, 0:tn], in_=pbk[:, 0:tn], func=AF.Sigmoid),
                                  reads=[Bp], writes=[Bgst[s]])
                            em.dma("sp", f"st_g{s}", dst[ch, :, t0:t0 + tn], gst[s][:, 0:tn], reads=[Bgst[s]], writes=[B_d["gaT" if gb < 4 else "gcT"]])
                blocks_other.append(dict(load=load, run=run))
            A_sb = SB(st, "A_sb", [128, TA], F32); BA = Buf("A")
            Us = [SB(st, f"U{i}", [128, 32 + T], F32) for i in range(2)]; BUs = [Buf("U0"), Buf("U1")]
            U = Us[0]; BU = BUs[0]
            acc = [SB(st, f"cacc{i}", [128, T], F32) for i in range(2)]
            Bacc = [Buf("cacc0"), Buf("cacc1")]
            sqb = SB(st, "sqb", [128, T], F32); Bsq = Buf("sqb")
            TG5 = TG4 + [(T, 128)]
            pending_stats = []

            def flush_stats():
                while pending_stats:
                    pending_stats.pop(0)()
            blocks_conv = []
            for cc in range(16):
                def load(slot, cc=cc):
                    G.wload(slot, 0, 128, w_in[:, 6144 + cc * 128: 6144 + (cc + 1) * 128])
                    G.wload(slot, 128, 256, w_in[:, 8192 + cc * 128: 8192 + (cc + 1) * 128])

                def run(slot, cc=cc):
                    flush_stats()
                    U = Us[cc % 2]
                    BU = BUs[cc % 2]
                    for (t0, tn) in TG5:
                        pbk, Bp = G.mm_fm(slot, 0, actT, Bact, t0, tn)
                        em.op("act", lambda: nc.scalar.copy(out=A_sb[:, t0:t0 + tn], in_=pbk[:, 0:tn]), reads=[Bp], writes=[BA])
                    for (t0, tn) in TG5:
                        pbk, Bp = G.mm_fm(slot, 128, actT, Bact, t0, tn)
                        if t0 < T:
                            em.op("act", lambda: nc.scalar.activation(out=U[:, 32 + t0:32 + t0 + tn], in_=pbk[:, 0:tn], func=AF.Sigmoid),
                                  reads=[Bp], writes=[BU])
                        else:
                            em.op("act", lambda: nc.scalar.activation(out=U[:, 0:32], in_=pbk[:, 96:128], func=AF.Sigmoid),
                                  reads=[Bp], writes=[BU])
                    em.op("dve", lambda: nc.vector.tensor_tensor(out=U[:, 0:32], in0=U[:, 0:32], in1=A_sb[:, T + 96:T + 128], op=ALU.mult),
                          reads=[BA, BU], writes=[BU])
                    em.op("dve", lambda: nc.vector.tensor_tensor(out=U[:, 32:32 + T], in0=U[:, 32:32 + T], in1=A_sb[:, 0:T], op=ALU.mult),
                          reads=[BA, BU], writes=[BU])
                    a = acc[cc % 2]
                    Ba = Bacc[cc % 2]
                    em.op("dve", lambda: nc.vector.tensor_scalar(out=a[:], in0=U[:, 2:2 + T], scalar1=cw[:, cc, 0:1], scalar2=cb[:, cc:cc + 1],
                                                                 op0=ALU.mult, op1=ALU.add), reads=[BU, Bcw], writes=[Ba])
                    for j in range(1, 31):
                        em.op("dve", lambda: nc.vector.scalar_tensor_tensor(out=a[:], in0=U[:, 2 + j:2 + j + T], scalar=cw[:, cc, j:j + 1], in1=a[:],
                                                                            op0=ALU.mult, op1=ALU.add), reads=[BU, Bcw, Ba], writes=[Ba])
                    em.dma("sp", f"st_c{cc % 2}", S_cT[cc, :, :], a[:], reads=[Ba], writes=[B_d["cT"]])

                    def stats(a=a, Ba=Ba):
                        em.op("pool", lambda: nc.gpsimd.tensor_tensor(out=sqb[:], in0=a[:], in1=a[:], op=ALU.mult), reads=[Ba], writes=[Bsq])
                        em.op("pool", lambda: nc.gpsimd.tensor_tensor(out=csum[:], in0=csum[:], in1=a[:], op=ALU.add), reads=[Ba, Bcs], writes=[Bcs])
                        em.op("pool", lambda: nc.gpsimd.tensor_tensor(out=csq[:], in0=csq[:], in1=sqb[:], op=ALU.add), reads=[Bsq, Bcq], writes=[Bcq])
                    pending_stats.append(stats)
                blocks_conv.append(dict(load=load, run=run))
            order = []
            for n in range(len(blocks_other)):
                order.append(blocks_other[n])
                if n < 16:
                    order.append(blocks_conv[n])
            G.run(order)
            flush_stats()
            act_only[0] = False
            mu = A_sb[:, 0:T]; rs = U[:, 0:T]
            Bmu = BA; Brs = BU
            for (t0, tn) in TG4:
                bk = G.next_bank()
                em.op("pe", lambda: nc.tensor.matmul(G.pb[bk][:, :], lhsT=onesf[:], rhs=csum[:, t0:t0 + tn], start=True, stop=True),
                      reads=[Bcs, B_c], writes=[G.Bp[bk]])
                em.op("dve", lambda: nc.vector.tensor_scalar(out=mu[:, t0:t0 + tn], in0=G.pb[bk][:, :], scalar1=1.0 / D, scalar2=None, op0=ALU.mult),
                      reads=[G.Bp[bk]], writes=[Bmu])
                bk = G.next_bank()
                em.op("pe", lambda: nc.tensor.matmul(G.pb[bk][:, :], lhsT=onesf[:], rhs=csq[:, t0:t0 + tn], start=True, stop=True),
                      reads=[Bcq, B_c], writes=[G.Bp[bk]])
                em.op("dve", lambda: nc.vector.tensor_scalar(out=rs[:, t0:t0 + tn], in0=G.pb[bk][:, :], scalar1=1.0 / D, scalar2=EPS, op0=ALU.mult, op1=ALU.add),
                      reads=[G.Bp[bk]], writes=[Brs])
            em.op("dve", lambda: nc.vector.tensor_tensor(out=sqb[:], in0=mu, in1=mu, op=ALU.mult), reads=[Bmu], writes=[Bsq])
            em.op("dve", lambda: nc.vector.tensor_tensor(out=rs, in0=rs, in1=sqb[:], op=ALU.subtract), reads=[Brs, Bsq], writes=[Brs])
            em.op("act", lambda: nc.scalar.activation(out=rs, in_=rs, func=AF.Sqrt), reads=[Brs], writes=[Brs])
            em.op("dve", lambda: nc.vector.reciprocal(out=rs, in_=rs), reads=[Brs], writes=[Brs])
            em.dma("sp", "st_mu", S_mu[:, :], mu, reads=[Bmu], writes=[B_d["mu"]])
            em.dma("sp", "st_mu", S_rs[:, :], rs, reads=[Brs], writes=[B_d["mu"]])
            em.barrier()

        checkpoint("B1")
        with ExitStack() as st:
            attnT = SB(st, "attnT", [128, 16, T], BF16)
            BattnT = Buf("attnT")
            qs = [SB(st, f"qs{i}", [128, 8, 256], BF16) for i in range(2)]
            ks = [SB(st, f"ks{i}", [128, 16, 256], BF16) for i in range(2)]
            vs = [SB(st, f"vs{i}", [128, 32, 136], BF16) for i in range(2)]
            bsf = [SB(st, f"bsf{i}", [128, 512], F32) for i in range(2)]
            baj = [SB(st, f"baj{i}", [128, 512], F32) for i in range(2)]
            Bhq = [Buf("hq0"), Buf("hq1")]; Bhk = [Buf("hk0"), Buf("hk1")]; Bhv = [Buf("hv0"), Buf("hv1")]; Bhb = [Buf("hb0"), Buf("hb1")]
            t31 = SB(st, "t31", [128, 16], F32)
            pastb = SB(st, "pastb", [128, 16, 16], F32)
            pastm = SB(st, "pastm", [128, 16, 16], F32)
            Bmk = Buf("masks")
            em.dma("sp", "ld_mk", t31[:], t31_d[:, :], writes=[Bmk])
            em.dma("sp", "ld_mk", pastb[:, :, :], pastb_d.rearrange("p (a b) -> p a b", b=16), writes=[Bmk])
            em.dma("sp", "ld_mk", pastm[:, :, :], pastm_d.rearrange("p (a b) -> p a b", b=16), writes=[Bmk])
            for i in range(2):
                em.op("dve", lambda: nc.vector.memset(vs[i][:, :, 128:136], 0.0), writes=[Bhv[i]])
                em.op("dve", lambda: nc.vector.memset(vs[i][:, :, 128:129], 1.0), writes=[Bhv[i]])
            km = SB(st, "km", [128, 16], F32); kmb = SB(st, "kmb", [128, 16], BF16); Bkm = Buf("km")
            gate = SB(st, "gate", [128, 16, 16], F32); Bgate = Buf("gate")
            m8 = SB(st, "m8", [128, 16, 8], F32); Bm8 = Buf("m8")
            selm = [SB(st, f"selm{i}", [128, 16, 16], F32) for i in range(2)]
            Bsel = [Buf("sel0"), Buf("sel1")]
            tmpb = [SB(st, f"tmpb{i}", [128, 512], F32) for i in range(2)]
            Btmp = [Buf("tmpb0"), Buf("tmpb1")]
            PT = [SB(st, f"PT{i}", [128, 512], BF16) for i in range(3)]
            BPT = [Buf(f"PT{i}") for i in range(3)]
            oacc = [SB(st, f"oacc{i}", [128, 2, 129], F32) for i in range(2)]
            Boacc = [Buf("oacc0"), Buf("oacc1")]
            rden = SB(st, "rden", [128, 2], F32); Brden = Buf("rden")
            obf = SB(st, "obf", [128, 2, 128], BF16); Bobf = Buf("obf")
            pS = [PS(st, f"pS{i}", [128, 512], F32) for i in range(3)]
            BpS = [Buf(f"pS{i}") for i in range(3)]
            pO = [PS(st, f"pO{i}", [128, 2, 256], F32) for i in range(3)]
            BpO = [Buf(f"pO{i}") for i in range(3)]
            pG = PS(st, "pG", [128, 32, 16], F32); BpG = Buf("pG")
            pTr = PS(st, "pTr", [128, 1024], BF16); BpTr = Buf("pTr")

            def head_load(h):
                s = h % 2
                em.dma("sp", f"ld_hq{s}", qs[s][:, :, :], S_qT[h].rearrange("p (j t) -> p j t", t=256), reads=[B_d["qT"]], writes=[Bhq[s]])
                em.dma("sp", f"ld_hk{s}", ks[s][:, :, :], S_kT[h].rearrange("p (j t) -> p j t", t=256), reads=[B_d["kT"]], writes=[Bhk[s]])
                em.dma("sp", f"ld_hv{s}", vs[s][:, :, 0:128], S_V[:, h * 128:(h + 1) * 128].rearrange("(kt p) d -> p kt d", p=128),
                       reads=[B_d["V"]], writes=[Bhv[s]])
                em.dma("sp", f"ld_hb{s}", bsf[s][:], bias_self[h], writes=[Bhb[s]])
                em.dma("sp", f"ld_hc{s}", baj[s][:], bias_adj[h], writes=[Bhb[s]])

            def head_prologue(h):
                s = h % 2
                em.op("dve", lambda: nc.vector.tensor_reduce(out=km[:], in_=ks[s][:, :, :], axis=AX.X, op=ALU.add), reads=[Bhk[s]], writes=[Bkm])
                em.op("dve", lambda: nc.vector.tensor_scalar(out=kmb[:], in0=km[:], scalar1=1.0 / 256, scalar2=None, op0=ALU.mult), reads=[Bkm], writes=[Bkm])

                def f():
                    for qt in range(16):
                        ins = nc.tensor.matmul(pG[:, qt, :], lhsT=qs[s][:, qt // 2, (qt % 2) * 128:(qt % 2 + 1) * 128], rhs=kmb[:, :], start=True, stop=True)
                    return ins
                em.op("pe", f, reads=[Bhq[s], Bkm], writes=[BpG])
                em.op("dve", lambda: nc.vector.tensor_tensor(out=gate[:, :, :], in0=pG[:, 0:16, :], in1=pastb[:, :, :], op=ALU.add),
                      reads=[BpG, Bmk], writes=[Bgate])
                for qt in range(16):
                    em.op("dve", lambda: nc.vector.max(out=m8[:, qt, :], in_=gate[:, qt, :]), reads=[Bgate], writes=[Bm8])
                sm = selm[s]
                for qt in range(16):
                    em.op("dve", lambda: nc.vector.tensor_scalar(out=sm[:, qt, :], in0=gate[:, qt, :], scalar1=m8[:, qt, 2:3], scalar2=None, op0=ALU.is_ge),
                          reads=[Bgate, Bm8], writes=[Bsel[s]])
                em.op("dve", lambda: nc.vector.tensor_tensor(out=sm[:, :, :], in0=sm[:, :, :], in1=pastm[:, :, :], op=ALU.mult),
                      reads=[Bsel[s], Bmk], writes=[Bsel[s]])

            pairs = []
            for h in range(16):
                for i in range(8):
                    lst = [(h, i, i, 0)]
                    for j in range(i):
                        lst.append((h, i, j, 1 if j == i - 1 else 2))
                    for k in range(8):
                        lst.append((h, i, 8 + k, 1 if (i == 0 and k == 7) else 2))
                    for n, p in enumerate(lst):
                        pairs.append(p + (n == 0, n == len(lst) - 1))

            def emit_S(n):
                h, i, j, kind, first, last = pairs[n]
                s = h % 2
                b = n % 3

                def f():
                    for kt in range(2):
                        ins = nc.tensor.matmul(pS[b][:, kt * 256:(kt + 1) * 256], lhsT=ks[s][:, j, kt * 128:(kt + 1) * 128], rhs=qs[s][:, i, :],
                                               start=True, stop=True)
                    return ins
                em.op("pe", f, reads=[Bhq[s], Bhk[s]], writes=[BpS[b]])

            head_load(0)
            head_prologue(0)
            emit_S(0)
            emit_S(1)
            for n in range(len(pairs)):
                h, i, j, kind, first, last = pairs[n]
                s = h % 2
                b = n % 3
                if first and i == 0 and h + 1 < 16:
                    head_load(h + 1)
                if n + 2 < len(pairs):
                    if pairs[n + 2][0] != pairs[n + 1][0]:
                        head_prologue(pairs[n + 2][0])
                    emit_S(n + 2)
                if kind == 2:
                    em.op("act", lambda: nc.scalar.activation(out=PT[b][:], in_=pS[b][:], func=AF.Exp, bias=t31[:, h:h + 1], scale=SCALE),
                          reads=[BpS[b], Bmk], writes=[BPT[b]])
                else:
                    tb = n % 2
                    btile = bsf[s] if kind == 0 else baj[s]
                    em.op("dve", lambda: nc.vector.scalar_tensor_tensor(out=tmpb[tb][:], in0=pS[b][:], scalar=SCALE, in1=btile[:], op0=ALU.mult, op1=ALU.add),
                          reads=[BpS[b], Bhb[s]], writes=[Btmp[tb]])
                    em.op("act", lambda: nc.scalar.activation(out=PT[b][:], in_=tmpb[tb][:], func=AF.Exp), reads=[Btmp[tb]], writes=[BPT[b]])

                def f():
                    for q2 in range(2):
                        for kt in range(2):
                            ins = nc.tensor.matmul(pO[b][:, q2, 0:132], lhsT=PT[b][:, kt * 256 + q2 * 128: kt * 256 + (q2 + 1) * 128],
                                                   rhs=vs[s][:, j * 2 + kt, 0:132], start=(kt == 0), stop=(kt == 1))
                    return ins
                em.op("pe", f, reads=[BPT[b], Bhv[s]], writes=[BpO[b]])
                oa = oacc[i % 2]
                Boa = Boacc[i % 2]
                if first:
                    em.op("dve", lambda: nc.vector.tensor_copy(out=oa[:, :, :], in_=pO[b][:, :, 0:129]), reads=[BpO[b]], writes=[Boa])
                else:
                    for q2 in range(2):
                        em.op("dve", lambda: nc.vector.scalar_tensor_tensor(out=oa[:, q2, :], in0=pO[b][:, q2, 0:129], scalar=selm[s][:, i * 2 + q2, j:j + 1],
                                                                            in1=oa[:, q2, :], op0=ALU.mult, op1=ALU.add),
                              reads=[BpO[b], Bsel[s], Boa], writes=[Boa])
                if last:
                    em.op("dve", lambda: nc.vector.reciprocal(out=rden[:, :], in_=oa[:, :, 128]), reads=[Boa], writes=[Brden])
                    for q2 in range(2):
                        em.op("dve", lambda: nc.vector.tensor_scalar(out=obf[:, q2, :], in0=oa[:, q2, 0:128], scalar1=rden[:, q2:q2 + 1], scalar2=None, op0=ALU.mult),
                              reads=[Boa, Brden], writes=[Bobf])

                    def f():
                        for q2 in range(2):
                            ins = nc.tensor.transpose(out=pTr[:, q2 * 128:(q2 + 1) * 128], in_=obf[:, q2, :], identity=idb[:])
                        return ins
                    em.op("pe", f, reads=[Bobf, B_c], writes=[BpTr])
                    em.op("act", lambda: nc.scalar.copy(out=attnT[:, h, i * 256:(i + 1) * 256], in_=pTr[:, 0:256]), reads=[BpTr], writes=[BattnT])
            em.dma("sp", "st_at", S_attnT.rearrange("c p t -> p c t"), attnT[:, :, :], reads=[BattnT], writes=[B_d["attnT"]])
            em.barrier()

        checkpoint("C")
        with ExitStack() as st:
            D1_attnT = SB(st, "attnT1", [128, 16, T], BF16)
            D1_B = Buf("attnT1")
            em.dma("sp", "ld_act", D1_attnT[:, :, :], S_attnT.rearrange("c p t -> p c t"), reads=[B_d["attnT"]], writes=[D1_B])
            if True:
                st2 = st
                G = Gemm(st2, "d1")
                gsb = [SB(st2, f"gsb{i}", [128, T], F32) for i in range(2)]
                Bgsb = [Buf("gsb0"), Buf("gsb1")]
                zst = [SB(st2, f"zst{i}", [128, T], F32) for i in range(2)]
                Bzst = [Buf("zst0"), Buf("zst1")]
                blocks = []
                cnt = [0]
                for ob in range(4):
                    def load(slot, ob=ob):
                        G.wload(slot, 0, 512, w_attn[:, ob * 512:(ob + 1) * 512])

                    def run(slot, ob=ob):
                        for sub in range(4):
                            ch = ob * 4 + sub
                            s = cnt[0] % 2
                            cnt[0] += 1
                            em.dma("sp", f"ld_gs{s}", gsb[s][:], S_gaT[ch, :, :], reads=[B_d["gaT"]], writes=[Bgsb[s]])
                            for (t0, tn) in TG4:
                                pbk, Bp = G.mm_fm(slot, sub * 128, D1_attnT, D1_B, t0, tn)
                                em.op("dve", lambda: nc.vector.tensor_tensor(out=zst[s][:, t0:t0 + tn], in0=pbk[:, 0:tn], in1=gsb[s][:, t0:t0 + tn], op=ALU.mult),
                                      reads=[Bp, Bgsb[s]], writes=[Bzst[s]])
                            em.dma("sp", f"st_z{s}", S_z1T[ch, :, :], zst[s][:], reads=[Bzst[s]], writes=[B_d["z1T"]])
                    blocks.append(dict(load=load, run=run))
                G.run(blocks)
                em.barrier()

        checkpoint("D1")
        with ExitStack() as st:
            convT = SB(st, "convT", [128, 16, T], BF16)
            BconvT = Buf("convT")
            lg = SB(st, "lg", [128, 16], F32); lb = SB(st, "lb", [128, 16], F32); Blg = Buf("lg")
            em.dma("sp", "ld_lg", lg[:], ln_g[:, :], writes=[Blg])
            em.dma("sp", "ld_lg", lb[:], ln_b[:, :], writes=[Blg])
            mu = SB(st, "mu", [128, T], F32); rs = SB(st, "rs", [128, T], F32)
            Bmu = Buf("mu"); Brs = Buf("rs")
            em.dma("sp", "ld_mu", mu[:], S_mu[:, :], reads=[B_d["mu"]], writes=[Bmu])
            em.dma("sp", "ld_rs", rs[:], S_rs[:, :], reads=[B_d["mu"]], writes=[Brs])
            with ExitStack() as st2:
                cl = [SB(st2, f"cl{i}", [128, T], F32) for i in range(2)]
                Bcl = [Buf("cl0"), Buf("cl1")]
                for cc in range(16):
                    s = cc % 2
                    em.dma("sp", f"ld_cl{s}", cl[s][:], S_cT[cc, :, :], reads=[B_d["cT"]], writes=[Bcl[s]])
                    em.op("dve", lambda: nc.vector.tensor_tensor(out=cl[s][:], in0=cl[s][:], in1=mu[:], op=ALU.subtract), reads=[Bcl[s], Bmu], writes=[Bcl[s]])
                    em.op("dve", lambda: nc.vector.tensor_tensor(out=cl[s][:], in0=cl[s][:], in1=rs[:], op=ALU.mult), reads=[Bcl[s], Brs], writes=[Bcl[s]])
                    em.op("act", lambda: nc.scalar.activation(out=convT[:, cc, :], in_=cl[s][:], func=AF.Silu, scale=lg[:, cc:cc + 1], bias=lb[:, cc:cc + 1]),
                          reads=[Bcl[s], Blg], writes=[BconvT])
                if debug and "S_convT" in debug:
                    for cc in range(16):
                        em.dma("sp", "st_dbg", S_convT[cc, :, :], convT[:, cc, :], reads=[BconvT], writes=[Buf("dbg")])
                em.barrier()
            with ExitStack() as st2:
                G = Gemm(st2, "d2")
                gsb = [SB(st2, f"gsc{i}", [128, T], F32) for i in range(2)]
                Bgsb = [Buf("gsc0"), Buf("gsc1")]
                z1b = [SB(st2, f"z1b{i}", [128, T], F32) for i in range(2)]
                Bz1b = [Buf("z1b0"), Buf("z1b1")]
                zst = [SB(st2, f"zsb{i}", [128, T], BF16) for i in range(2)]
                Bzst = [Buf("zsb0"), Buf("zsb1")]
                tmpz = [SB(st2, f"tmpz{i}", [128, 512], F32) for i in range(2)]
                Btz = [Buf("tmpz0"), Buf("tmpz1")]
                blocks = []
                cnt = [0, 0]
                for ob in range(4):
                    def load(slot, ob=ob):
                        G.wload(slot, 0, 512, w_conv[:, ob * 512:(ob + 1) * 512])

                    def run(slot, ob=ob):
                        for sub in range(4):
                            ch = ob * 4 + sub
                            s = cnt[0] % 2
                            cnt[0] += 1
                            em.dma("sp", f"ld_gs{s}", gsb[s][:], S_gcT[ch, :, :], reads=[B_d["gcT"]], writes=[Bgsb[s]])
                            em.dma("sp", f"ld_z1{s}", z1b[s][:], S_z1T[ch, :, :], reads=[B_d["z1T"]], writes=[Bz1b[s]])
                            for (t0, tn) in TG4:
                                pbk, Bp = G.mm_fm(slot, sub * 128, convT, BconvT, t0, tn)
                                u = cnt[1] % 2
                                cnt[1] += 1
                                em.op("dve", lambda: nc.vector.tensor_tensor(out=tmpz[u][:, 0:tn], in0=pbk[:, 0:tn], in1=gsb[s][:, t0:t0 + tn], op=ALU.mult),
                                      reads=[Bp, Bgsb[s]], writes=[Btz[u]])
                                em.op("dve", lambda: nc.vector.tensor_tensor(out=zst[s][:, t0:t0 + tn], in0=tmpz[u][:, 0:tn], in1=z1b[s][:, t0:t0 + tn], op=ALU.add),
                                      reads=[Btz[u], Bz1b[s]], writes=[Bzst[s]])
                            em.dma("sp", f"st_z{s}", S_zT[ch, :, :], zst[s][:], reads=[Bzst[s]], writes=[B_d["zT"]])
                    blocks.append(dict(load=load, run=run))
                G.run(blocks)
                em.barrier()

        checkpoint("B3")
        def resid_gemm(st, tag, actT, Bact, w_ap, res_src, res_buf, dst, dst_key):
            G = Gemm(st, tag)
            xsl = [SB(st, f"xsl{tag}{i}", [128, 512], F32) for i in range(3)]
            Bxsl = [Buf(f"xsl{i}") for i in range(3)]
            hst = [SB(st, f"hst{tag}{i}", [128, 512], F32) for i in range(2)]
            Bhst = [Buf("hst0"), Buf("hst1")]
            blocks = []
            cnt = [0]
            for ob in range(4):
                def load(slot, ob=ob):
                    G.wload(slot, 0, 512, w_ap[:, ob * 512:(ob + 1) * 512])

                def run(slot, ob=ob):
                    def ldx(t):
                        em.dma("sp", f"ld_xsl{t % 3}", xsl[t % 3][:], res_src(t, ob), reads=res_buf, writes=[Bxsl[t % 3]])
                    ldx(0)
                    ldx(1)
                    for t in range(16):
                        if t + 2 < 16:
                            ldx(t + 2)
                        pbk, Bp = G.mm_tm(slot, actT, Bact, t)
                        s = cnt[0] % 2
                        cnt[0] += 1
                        em.op("dve", lambda: nc.vector.tensor_tensor(out=hst[s][:], in0=pbk[:, :], in1=xsl[t % 3][:], op=ALU.add),
                              reads=[Bp, Bxsl[t % 3]], writes=[Bhst[s]])
                        em.dma("sp", f"st_h{s}", dst[t * 128:(t + 1) * 128, ob * 512:(ob + 1) * 512], hst[s][:], reads=[Bhst[s]], writes=[B_d[dst_key]])
                blocks.append(dict(load=load, run=run))
            G.run(blocks)

        with ExitStack() as st:
            zT = SB(st, "zTa", [128, 16, T], BF16)
            BzT = Buf("zTa")
            em.dma("sp", "ld_act", zT[:, :, :], S_zT.rearrange("c p t -> p c t"), reads=[B_d["zT"]], writes=[BzT])
            resid_gemm(st, "e", zT, BzT, w_o, lambda t, ob: xa[t * 128:(t + 1) * 128, ob * 512:(ob + 1) * 512], [], S_h1, "h1")
            em.barrier()

        checkpoint("E")
        with ExitStack() as st:
            x2T = SB(st, "x2T", [128, 16, T], BF16)
            Bx2T = Buf("x2T")
            combT = SB(st, "combT", [16, T], F32)
            BcombT = Buf("combT")
            sel_sb = SB(st, "sel_sb", [16, D], F32)
            Bsl = Buf("sel_sb")
            em.dma("sp", "ld_sl", sel_sb[:], sel16_d[:, :], writes=[Bsl])
            with ExitStack() as s1:
                src = lambda t: S_h1[t * 128:(t + 1) * 128, :]
                rstd, Bss = rms_stats(s1, src, 16, "f")
                gB = SB(s1, "gBf", [128, D], F32); BgB = Buf("gBf")
                em.dma("sp", "ld_g", gB[:], g_ffn.partition_broadcast(128), writes=[BgB])
                wr = SB(s1, "wr", [128, 16, 20], F32); Bwr = Buf("wr")
                em.dma("sp", "ld_wr", wr[:], w_r.rearrange("(k p) n -> p k n", p=128), writes=[Bwr])
                brb = SB(s1, "brb", [128, 20], F32)
                em.dma("sp", "ld_wr", brb[:], b_r.partition_broadcast(128), writes=[Bwr])
                xt = [SB(s1, f"xtf{i}", [128, D], F32) for i in range(2)]
                Bx = [Buf("xtf0"), Buf("xtf1")]
                xn = [SB(s1, f"xnf{i}", [128, D], F32) for i in range(2)]
                Bxn = [Buf("xnf0"), Buf("xnf1")]
                xf = [SB(s1, f"xf{i}", [128, 16, 128], F32) for i in range(2)]
                Bxf = [Buf("xf0"), Buf("xf1")]
                pF = [PS(s1, f"pF{i}", [128, 4, 128], F32) for i in range(4)]
                BpF = [Buf(f"pF{i}") for i in range(4)]
                pR = PS(s1, "pR", [128, 512], F32); BpR = Buf("pR")
                pC = PS(s1, "pC", [128, 512], F32); BpC = Buf("pC")
                comb = SB(s1, "comb", [128, 16, 16], F32); Bcomb = Buf("comb")
                R = {k: SB(s1, "r_" + k, [128, n], F32) for k, n in
                     dict(L=20, cmax=1, ncmax=1, ohg=4, ecl=4, esum=1, pg=1, fsel=4, v1=1, m1=4, fs2=4, v2=1, m2=4, d=1, e=1, den=1,
                          w1=1, w2=1, t1=4, fine=4, pf=4).items()}
                BR = Buf("router_scratch")

                def dv(fn, reads=(), writes=()):
                    em.op("dve", fn, reads=[BR] + list(reads), writes=[BR] + list(writes))

                for t in range(16):
                    s = t % 2
                    em.dma("sp", f"ld_xt{s}", xt[s][:], S_h1[t * 128:(t + 1) * 128, :], reads=[B_d["h1"]], writes=[Bx[s]])
                    em.op("dve", lambda: nc.vector.scalar_tensor_tensor(out=xn[s][:], in0=xt[s][:], scalar=rstd[:, t:t + 1], in1=gB[:], op0=ALU.mult, op1=ALU.mult),
                          reads=[Bx[s], Bss, BgB], writes=[Bxn[s]])
                    for k in range(4):
                        def f():
                            for j in range(4):
                                c = k * 4 + j
                                ins = nc.tensor.transpose(out=pF[k][:, j, :], in_=xn[s][:, c * 128:(c + 1) * 128], identity=idf[:])
                            return ins
                        em.op("pe", f, reads=[Bxn[s], B_c], writes=[BpF[k]])
                        em.op("dve", lambda: nc.vector.tensor_copy(out=xf[s][:, k * 4:(k + 1) * 4, :], in_=pF[k][:, :, :]), reads=[BpF[k]], writes=[Bxf[s]])
                        em.op("act", lambda: nc.scalar.copy(out=x2T[:, k * 4:(k + 1) * 4, t * 128:(t + 1) * 128], in_=xf[s][:, k * 4:(k + 1) * 4, :]), reads=[Bxf[s]], writes=[Bx2T])

                    def f():
                        for c in range(16):
                            ins = nc.tensor.matmul(pR[:, 0:20], lhsT=xf[s][:, c, :], rhs=wr[:, c, :], start=(c == 0), stop=(c == 15))
                        return ins
                    em.op("pe", f, reads=[Bxf[s], Bwr], writes=[BpR])
                    L = R["L"]
                    dv(lambda: nc.vector.tensor_tensor(out=L[:], in0=pR[:, 0:20], in1=brb[:], op=ALU.add), reads=[BpR, Bwr])
                    dv(lambda: nc.vector.tensor_reduce(out=R["cmax"][:], in_=L[:, 0:4], axis=AX.X, op=ALU.max))
                    dv(lambda: nc.vector.tensor_scalar(out=R["ncmax"][:], in0=R["cmax"][:], scalar1=-1.0, scalar2=None, op0=ALU.mult))
                    dv(lambda: nc.vector.tensor_scalar(out=R["ohg"][:], in0=L[:, 0:4], scalar1=R["cmax"][:, 0:1], scalar2=None, op0=ALU.is_ge))
                    em.op("act", lambda: nc.scalar.activation(out=R["ecl"][:], in_=L[:, 0:4], func=AF.Exp, bias=R["ncmax"][:, 0:1], scale=1.0),
                          reads=[BR], writes=[BR])
                    dv(lambda: nc.vector.tensor_reduce(out=R["esum"][:], in_=R["ecl"][:], axis=AX.X, op=ALU.add))
                    dv(lambda: nc.vector.reciprocal(out=R["pg"][:], in_=R["esum"][:]))
                    dv(lambda: nc.vector.tensor_scalar(out=R["fsel"][:], in0=L[:, 4:8], scalar1=R["ohg"][:, 0:1], scalar2=None, op0=ALU.mult))
                    for g in range(1, 4):
                        dv(lambda: nc.vector.scalar_tensor_tensor(out=R["fsel"][:], in0=L[:, 4 + 4 * g:8 + 4 * g], scalar=R["ohg"][:, g:g + 1], in1=R["fsel"][:],
                                                                  op0=ALU.mult, op1=ALU.add))
                    dv(lambda: nc.vector.tensor_reduce(out=R["v1"][:], in_=R["fsel"][:], axis=AX.X, op=ALU.max))
                    dv(lambda: nc.vector.tensor_scalar(out=R["m1"][:], in0=R["fsel"][:], scalar1=R["v1"][:, 0:1], scalar2=None, op0=ALU.is_ge))
                    dv(lambda: nc.vector.scalar_tensor_tensor(out=R["fs2"][:], in0=R["m1"][:], scalar=NEG, in1=R["fsel"][:], op0=ALU.mult, op1=ALU.add))
                    dv(lambda: nc.vector.tensor_reduce(out=R["v2"][:], in_=R["fs2"][:], axis=AX.X, op=ALU.max))
                    dv(lambda: nc.vector.tensor_scalar(out=R["m2"][:], in0=R["fs2"][:], scalar1=R["v2"][:, 0:1], scalar2=None, op0=ALU.is_ge))
                    dv(lambda: nc.vector.tensor_tensor(out=R["d"][:], in0=R["v2"][:], in1=R["v1"][:], op=ALU.subtract))
                    em.op("act", lambda: nc.scalar.activation(out=R["e"][:], in_=R["d"][:], func=AF.Exp), reads=[BR], writes=[BR])
                    dv(lambda: nc.vector.tensor_scalar(out=R["den"][:], in0=R["e"][:], scalar1=1.0, scalar2=None, op0=ALU.add))
                    dv(lambda: nc.vector.reciprocal(out=R["w1"][:], in_=R["den"][:]))
                    dv(lambda: nc.vector.tensor_tensor(out=R["w2"][:], in0=R["e"][:], in1=R["w1"][:], op=ALU.mult))
                    dv(lambda: nc.vector.tensor_scalar(out=R["t1"][:], in0=R["m1"][:], scalar1=R["w1"][:, 0:1], scalar2=None, op0=ALU.mult))
                    dv(lambda: nc.vector.scalar_tensor_tensor(out=R["fine"][:], in0=R["m2"][:], scalar=R["w2"][:, 0:1], in1=R["t1"][:], op0=ALU.mult, op1=ALU.add))
                    dv(lambda: nc.vector.tensor_scalar(out=R["pf"][:], in0=R["fine"][:], scalar1=R["pg"][:, 0:1], scalar2=None, op0=ALU.mult))
                    for g in range(4):
                        dv(lambda: nc.vector.tensor_scalar(out=comb[:, t, 4 * g:4 * g + 4], in0=R["pf"][:], scalar1=R["ohg"][:, g:g + 1], scalar2=None, op0=ALU.mult),
                           writes=[Bcomb])
                    em.op("pe", lambda: nc.tensor.matmul(pC[0:16, 0:128], lhsT=comb[:, t, :], rhs=idf[:], start=True, stop=True), reads=[Bcomb, B_c], writes=[BpC])
                    em.op("act", lambda: nc.scalar.copy(out=combT[:, t * 128:(t + 1) * 128], in_=pC[0:16, 0:128]), reads=[BpC], writes=[BcombT])
                if debug and "S_comb" in debug:
                    em.dma("sp", "st_dbg", S_comb[:, :, :], comb[:, :, :], reads=[Bcomb], writes=[Buf("dbg")])
                em.barrier()
                checkpoint("F1")
            with ExitStack() as s1:
                G = Gemm(s1, "f1")
                pCB = PS(s1, "pCB", [128, 512], F32); BpCB = Buf("pCB")
                combB = [SB(s1, f"combB{i}", [128, T], F32) for i in range(2)]
                BcombB = [Buf("combB0"), Buf("combB1")]
                sg = [SB(s1, f"sg{i}", [128, T], F32) for i in range(4)]
                Bsg = [Buf(f"sg{i}") for i in range(4)]
                hst = [SB(s1, f"hstb{i}", [128, T], BF16) for i in range(2)]
                Bhst = [Buf("hstb0"), Buf("hstb1")]
                tmph = [SB(s1, f"tmph{i}", [128, 512], F32) for i in range(2)]
                Btmph = [Buf("tmph0"), Buf("tmph1")]
                blocks = []
                cnt = [0, 0]
                for e in range(16):
                    for fp in range(2):
                        def load(slot, e=e, fp=fp):
                            G.wload(slot, 0, 256, w_eg[e, :, fp * 256:(fp + 1) * 256])
                            G.wload(slot, 256, 512, w_eu[e, :, fp * 256:(fp + 1) * 256])

                        def run(slot, e=e, fp=fp):
                            cbs = combB[e % 2]
                            if fp == 0:
                                for (t0, tn) in TG4:
                                    em.op("pe", lambda: nc.tensor.matmul(pCB[:, 0:tn], lhsT=sel_sb[:, e * 128:(e + 1) * 128], rhs=combT[:, t0:t0 + tn], start=True, stop=True),
                                          reads=[Bsl, BcombT], writes=[BpCB])
                                    em.op("act", lambda: nc.scalar.copy(out=cbs[:, t0:t0 + tn], in_=pCB[:, 0:tn]), reads=[BpCB], writes=[BcombB[e % 2]])
                            par = (e * 2 + fp) % 2
                            for fl in range(2):
                                for (t0, tn) in TG4:
                                    pbk, Bp = G.mm_fm(slot, fl * 128, x2T, Bx2T, t0, tn)
                                    em.op("act", lambda: nc.scalar.activation(out=sg[par * 2 + fl][:, t0:t0 + tn], in_=pbk[:, 0:tn], func=AF.Silu),
                                          reads=[Bp], writes=[Bsg[par * 2 + fl]])
                            for fl in range(2):
                                s = cnt[0] % 2
                                cnt[0] += 1
                                for (t0, tn) in TG4:
                                    pbk, Bp = G.mm_fm(slot, 256 + fl * 128, x2T, Bx2T, t0, tn)
                                    u = cnt[1] % 2
                                    cnt[1] += 1
                                    em.op("dve", lambda: nc.vector.tensor_tensor(out=tmph[u][:, 0:tn], in0=pbk[:, 0:tn], in1=sg[par * 2 + fl][:, t0:t0 + tn], op=ALU.mult),
                                          reads=[Bp, Bsg[par * 2 + fl]], writes=[Btmph[u]])
                                    em.op("dve", lambda: nc.vector.tensor_tensor(out=hst[s][:, t0:t0 + tn], in0=tmph[u][:, 0:tn], in1=cbs[:, t0:t0 + tn], op=ALU.mult),
                                          reads=[Btmph[u], BcombB[e % 2]], writes=[Bhst[s]])
                                em.dma("sp", f"st_H{s}", S_HT[e * 4 + fp * 2 + fl, :, :], hst[s][:], reads=[Bhst[s]], writes=[B_d["HT"]])
                        blocks.append(dict(load=load, run=run))
                G.run(blocks)
                em.barrier()
                checkpoint("F2")
        with ExitStack() as st:
            G = Gemm(st, "f2", nbanks=8, nw=3)
            HTq = [SB(st, f"HTq{i}", [128, 16, 512], BF16) for i in range(4)]
            BHTq = [Buf(f"HTq{i}") for i in range(4)]
            xsl = [SB(st, f"xslf{i}", [128, 512], F32) for i in range(4)]
            Bxsl = [Buf(f"xslf{i}") for i in range(4)]
            hst = [SB(st, f"hstf{i}", [128, 512], F32) for i in range(2)]
            Bhst = [Buf("hstf0"), Buf("hstf1")]
            S_HTv = S_HT.rearrange("c p t -> p c t")
            blocks = []
            cnt = [0]

            def ldH(Gi, kq):
                em.dma("sp", f"ld_HT{kq}", HTq[kq][:, :, :], S_HTv[:, kq * 16:(kq + 1) * 16, Gi * 512:(Gi + 1) * 512], reads=[B_d["HT"]], writes=[BHTq[kq]])
            for kq in range(4):
                ldH(0, kq)
            for Gi in range(4):
                for cb in range(4):
                    for kq in range(4):
                        def load(slot, cb=cb, kq=kq):
                            G.wload(slot, 0, 512, w_ed[kq * 2048:(kq + 1) * 2048, cb * 512:(cb + 1) * 512])

                        def run(slot, Gi=Gi, cb=cb, kq=kq):
                            base = (cb % 2) * 4
                            if kq == 0:
                                for tl in range(4):
                                    em.dma("sp", f"ld_xslf{tl}", xsl[tl][:], S_h1[(Gi * 4 + tl) * 128:(Gi * 4 + tl + 1) * 128, cb * 512:(cb + 1) * 512],
                                           reads=[B_d["h1"]], writes=[Bxsl[tl]])
                            for tl in range(4):
                                G.mm_tm(slot, HTq[kq], BHTq[kq], tl, bk=base + tl, first=(kq == 0), last=(kq == 3))
                            if cb == 3 and Gi + 1 < 4:
                                ldH(Gi + 1, kq)
                            if kq == 3:
                                for tl in range(4):
                                    s = cnt[0] % 2
                                    cnt[0] += 1
                                    em.op("dve", lambda: nc.vector.tensor_tensor(out=hst[s][:], in0=G.pb[base + tl][:, :], in1=xsl[tl][:], op=ALU.add),
                                          reads=[G.Bp[base + tl], Bxsl[tl]], writes=[Bhst[s]])
                                    em.dma("sp", f"st_h{s}", S_h2[(Gi * 4 + tl) * 128:(Gi * 4 + tl + 1) * 128, cb * 512:(cb + 1) * 512], hst[s][:],
                                           reads=[Bhst[s]], writes=[B_d["h2"]])
                        blocks.append(dict(load=load, run=run))
            G.run(blocks)
            em.barrier()

        checkpoint("F")
        with ExitStack() as st:
            x3T = SB(st, "x3T", [128, 16, T], BF16)
            Bx3T = Buf("x3T")
            norm_T(st, lambda t: S_h2[t * 128:(t + 1) * 128, :], 16, g_ple, x3T, Bx3T, "g")
            pT = SB(st, "pT", [128, 2, T], BF16); BpT = Buf("pT")
            wp = SB(st, "wp", [128, 2, D], BF16); Bwp = Buf("wp")
            em.dma("pool", "ld_wp", wp[:, :, :], w_pp.rearrange("(k p) n -> p k n", p=128), writes=[Bwp])
            with ExitStack() as s1:
                pl = [SB(s1, f"pl{i}", [128, 256], F32) for i in range(2)]
                Bpl = [Buf("pl0"), Buf("pl1")]
                plb = [SB(s1, f"plb{i}", [128, 256], BF16) for i in range(2)]
                Bplb = [Buf("plb0"), Buf("plb1")]
                ptp = PS(s1, "ptp", [128, 8, 128], BF16); Bptp = Buf("ptp")
                for t in range(16):
                    s = t % 2
                    em.dma("sp", f"ld_pl{s}", pl[s][:], pp[t * 128:(t + 1) * 128, :], writes=[Bpl[s]])
                    em.op("dve", lambda: nc.vector.tensor_copy(out=plb[s][:], in_=pl[s][:]), reads=[Bpl[s]], writes=[Bplb[s]])

                    def f():
                        for j in range(2):
                            ins = nc.tensor.transpose(out=ptp[:, j, :], in_=plb[s][:, j * 128:(j + 1) * 128], identity=idb[:])
                        return ins
                    em.op("pe", f, reads=[Bplb[s], B_c], writes=[Bptp])
                    em.op("act", lambda: nc.scalar.copy(out=pT[:, :, t * 128:(t + 1) * 128], in_=ptp[:, 0:2, :]), reads=[Bptp], writes=[BpT])
                em.barrier()
            G = Gemm(st, "g")
            pP = [PS(st, f"pP{i}", [128, 512], F32) for i in range(2)]
            BpP = [Buf("pP0"), Buf("pP1")]
            xsl = [SB(st, f"xslg{i}", [128, 512], F32) for i in range(3)]
            Bxsl = [Buf(f"xslg{i}") for i in range(3)]
            sgg = [SB(st, f"sgg{i}", [128, 512], F32) for i in range(2)]
            Bsgg = [Buf("sgg0"), Buf("sgg1")]
            hst = [SB(st, f"hstg{i}", [128, 512], F32) for i in range(2)]
            Bhst = [Buf("hstg0"), Buf("hstg1")]
            blocks = []
            cnt = [0]
            for ob in range(4):
                def load(slot, ob=ob):
                    G.wload(slot, 0, 512, w_pg[:, ob * 512:(ob + 1) * 512])

                def run(slot, ob=ob):
                    def ldx(t):
                        em.dma("sp", f"ld_xsl{t % 3}", xsl[t % 3][:], S_h2[t * 128:(t + 1) * 128, ob * 512:(ob + 1) * 512], reads=[B_d["h2"]], writes=[Bxsl[t % 3]])
                    ldx(0)
                    ldx(1)
                    for t in range(16):
                        if t + 2 < 16:
                            ldx(t + 2)
                        pbk, Bp = G.mm_tm(slot, x3T, Bx3T, t)
                        s = cnt[0] % 2
                        cnt[0] += 1

                        def f():
                            for kc in range(2):
                                ins = nc.tensor.matmul(pP[s][:, :], lhsT=pT[:, kc, t * 128:(t + 1) * 128], rhs=wp[:, kc, ob * 512:(ob + 1) * 512],
                                                       start=(kc == 0), stop=(kc == 1))
                            return ins
                        em.op("pe", f, reads=[BpT, Bwp], writes=[BpP[s]])
                        em.op("act", lambda: nc.scalar.activation(out=sgg[s][:], in_=pbk[:, :], func=AF.Sigmoid), reads=[Bp], writes=[Bsgg[s]])
                        em.op("dve", lambda: nc.vector.tensor_tensor(out=hst[s][:], in0=pP[s][:, :], in1=sgg[s][:], op=ALU.mult),
                              reads=[BpP[s], Bsgg[s]], writes=[Bhst[s]])
                        em.op("dve", lambda: nc.vector.tensor_tensor(out=hst[s][:], in0=hst[s][:], in1=xsl[t % 3][:], op=ALU.add),
                              reads=[Bxsl[t % 3], Bhst[s]], writes=[Bhst[s]])
                        em.dma("sp", f"st_h{s}", S_h3[t * 128:(t + 1) * 128, ob * 512:(ob + 1) * 512], hst[s][:], reads=[Bhst[s]], writes=[B_d["h3"]])
                blocks.append(dict(load=load, run=run))
            G.run(blocks)
            em.barrier()

        checkpoint("G")
        with ExitStack() as st:
            src = lambda t: S_h3[t * 128:(t + 1) * 128, :]
            rstd, Bss = rms_stats(st, src, 16, "h")
            gB = SB(st, "gBh", [128, D], F32); BgB = Buf("gBh")
            em.dma("sp", "ld_g", gB[:], g_fin.partition_broadcast(128), writes=[BgB])
            xt = [SB(st, f"xth{i}", [128, D], F32) for i in range(2)]
            Bx = [Buf("xth0"), Buf("xth1")]
            yo = [SB(st, f"yo{i}", [128, D], F32) for i in range(2)]
            Byo = [Buf("yo0"), Buf("yo1")]
            for t in range(16):
                s = t % 2
                em.dma("sp", f"ld_xt{s}", xt[s][:], src(t), reads=[B_d["h3"]], writes=[Bx[s]])
                em.op("dve", lambda: nc.vector.scalar_tensor_tensor(out=yo[s][:], in0=xt[s][:], scalar=rstd[:, t:t + 1], in1=gB[:], op0=ALU.mult, op1=ALU.mult),
                      reads=[Bx[s], Bss, BgB], writes=[Byo[s]])
                em.dma("sp", f"st_o{s}", out_d[t * 128:(t + 1) * 128, :], yo[s][:], reads=[Byo[s]], writes=[B_d["out"]])
            em.barrier()
        es.close()
      except _Stop:
        em.barrier()
        try:
            es.close()
        except AssertionError:
            pass
    return nc


def _rel_bucket(dist):
    n = np.maximum(dist, 0)
    max_exact = 16
    nf = np.maximum(n, 1).astype(np.float32)
    large = max_exact + (np.log(nf / np.float32(max_exact)) / np.float32(math.log(128 / max_exact)) * np.float32(32 - max_exact)).astype(np.int32)
    large = np.minimum(large, 31)
    return np.where(n < max_exact, n, large)


def make_in_maps(inp):
    f = lambda k: np.ascontiguousarray(np.asarray(inp[k], dtype=np.float32))
    x = f("x"); p = f("p")[0]
    rel_bias = f("rel_bias")
    kk = np.arange(256)[:, None]
    qq = np.arange(256)[None, :]
    bs = rel_bias[_rel_bucket(qq - kk)]
    bs = np.where((qq >= kk)[:, :, None], bs, np.float32(NEG)).astype(np.float32)
    ba = rel_bias[_rel_bucket(qq + 256 - kk)].astype(np.float32)

    def lay(b):
        b = b.transpose(2, 0, 1).reshape(16, 2, 128, 256).transpose(0, 2, 1, 3).reshape(16, 128, 512)
        return np.ascontiguousarray(b)
    bias_self = lay(bs); bias_adj = lay(ba)
    t31 = np.ascontiguousarray(np.broadcast_to(rel_bias[31][None, :], (128, 16))).astype(np.float32)
    colvec = lambda v: np.ascontiguousarray(v.reshape(16, 128).T)
    shared = {
        "w_in": f("w_in")[0], "w_attn_br": f("w_attn_br")[0], "w_conv_br": f("w_conv_br")[0], "w_o": f("w_o")[0],
        "w_ple_gate": f("w_ple_gate")[0], "w_ple_proj": f("w_ple_proj")[0],
        "w_e_gate": f("w_e_gate")[0], "w_e_up": f("w_e_up")[0], "w_e_down": f("w_e_down")[0].reshape(16 * 512, D),
        "w_r": np.ascontiguousarray(np.concatenate([f("w_router_g")[0], f("w_router_e")[0]], axis=1)),
        "b_r": np.ascontiguousarray(np.concatenate([f("b_router_g")[0], f("b_router_e")[0]])[None, :]),
        "g_mix": f("g_mix"), "g_ffn": f("g_ffn"), "g_ple": f("g_ple"), "g_final": f("g_final")[None, :],
        "conv_wT": np.ascontiguousarray(f("conv_w")[0].T.reshape(16, 128, 31).transpose(1, 0, 2)),
        "conv_b": colvec(f("conv_b")[0]), "ln_g": colvec(f("ln_g")[0]), "ln_b": colvec(f("ln_b")[0]),
        "ident": np.eye(128, dtype=np.float32),
        "sel16": np.ascontiguousarray(np.repeat(np.eye(16, dtype=np.float32), 128, axis=1)),
        "bias_self": bias_self, "bias_adj": bias_adj, "t31": t31,
    }
    maps = []
    for c in range(8):
        b, hf = c // 2, c % 2
        own = x[b, hf * T:(hf + 1) * T]
        oth = x[b, (1 - hf) * T:(2 - hf) * T]
        halo = np.zeros((128, D), np.float32)
        if hf == 1:
            halo[96:128] = x[b, T - 32:T]
        xa = np.concatenate([own, oth, halo], axis=0)
        gbl = np.concatenate([np.arange(8) + 8 * hf, np.arange(8) + 8 * (1 - hf)])
        past = (gbl[None, :] < (np.arange(8) + 8 * hf)[:, None])
        past_q = np.repeat(past, 2, axis=0)
        pastm = np.broadcast_to(past_q.astype(np.float32).reshape(1, 256), (128, 256))
        pastb = np.where(pastm > 0, np.float32(0), np.float32(NEG)).astype(np.float32)
        m = dict(shared)
        m.update({"xa": np.ascontiguousarray(xa), "pp": np.ascontiguousarray(p[b, hf * T:(hf + 1) * T]),
                  "pastm": np.ascontiguousarray(pastm), "pastb": np.ascontiguousarray(pastb)})
        maps.append(m)
    return maps


_NC = {}


def kernel(**inputs):
    if "nc" not in _NC:
        _NC["nc"] = build(False)
    maps = make_in_maps(inputs)
    res = run_bass_kernel_spmd(_NC["nc"], maps, core_ids=list(range(8)))
    out = np.empty((4, 4096, D), np.float32)
    for c in range(8):
        b, hf = c // 2, c % 2
        out[b, hf * T:(hf + 1) * T] = np.asarray(res.results[c]["out"], dtype=np.float32)
    return out
```

```python
import math
from contextlib import ExitStack
import numpy as np
import concourse.bass as bass
import concourse.mybir as mybir
from concourse.bass_utils import run_bass_kernel_spmd

F32 = mybir.dt.float32
BF16 = mybir.dt.bfloat16
AF = mybir.ActivationFunctionType
ALU = mybir.AluOpType
AX = mybir.AxisListType

D = 2048
T = 2048
NEG = -1e30
EPS = 1e-6
SCALE = 128 ** -0.5


class Buf:
    __slots__ = ("name", "w", "r")

    def __init__(self, name):
        self.name = name
        self.w = None
        self.r = {}


class Eng:
    def __init__(self, name, handle, sem):
        self.name = name
        self.h = handle
        self.sem = sem
        self.count = 0
        self.waited = {}


class Emitter:
    def __init__(self, nc, es):
        self.nc = nc
        self.es = es
        self.sems = {}
        self.engs = {}
        for name, h in (("pe", nc.tensor), ("dve", nc.vector), ("act", nc.scalar),
                        ("pool", nc.gpsimd), ("sp", nc.sync)):
            self.sems["sem_" + name] = es.enter_context(nc.semaphore("sem_" + name))
            self.engs[name] = Eng(name, h, "sem_" + name)
        self.dma_cnt = {}
        self.keymap = {}
        self.pool_keys = []

    def _wait(self, e, deps, skip_self=False):
        for key, val in deps:
            if skip_self and key == e.sem:
                continue
            if e.waited.get(key, 0) < val:
                e.h.wait_ge(self.sems[key], val)
                e.waited[key] = val

    @staticmethod
    def _deps(reads, writes):
        deps = {}
        for b in reads:
            if b.w is not None:
                k, v = b.w
                if deps.get(k, 0) < v:
                    deps[k] = v
        for b in writes:
            if b.w is not None:
                k, v = b.w
                if deps.get(k, 0) < v:
                    deps[k] = v
            for k, v in b.r.items():
                if deps.get(k, 0) < v:
                    deps[k] = v
        return list(deps.items())

    @staticmethod
    def _mark(ev, reads, writes):
        k, v = ev
        for b in reads:
            if b.r.get(k, 0) < v:
                b.r[k] = v
        for b in writes:
            b.w = ev
            b.r = {}

    def op(self, eng, fn, reads=(), writes=()):
        e = self.engs[eng]
        self._wait(e, self._deps(reads, writes), skip_self=(eng == "pe"))
        ins = fn()
        e.count += 1
        ins.then_inc(self.sems[e.sem], 1)
        self._mark((e.sem, e.count), reads, writes)
        return ins

    def dma(self, q, semkey, out, in_, reads=(), writes=()):
        e = self.engs[q]
        if semkey not in self.keymap:
            idx = len(self.keymap)
            if idx >= len(self.pool_keys):
                k = f"dq{idx}"
                self.sems[k] = self.es.enter_context(self.nc.semaphore(k))
                self.dma_cnt[k] = 0
                self.pool_keys.append(k)
            self.keymap[semkey] = self.pool_keys[idx]
        semkey = self.keymap[semkey]
        self._wait(e, self._deps(reads, writes))
        ins = e.h.dma_start(out=out, in_=in_)
        self.dma_cnt[semkey] += 16
        ins.then_inc(self.sems[semkey], 16)
        self._mark((semkey, self.dma_cnt[semkey]), reads, writes)
        return ins

    def barrier(self):
        evs = [(e.sem, e.count) for e in self.engs.values() if e.count > 0]
        evs += [(k, v) for k, v in self.dma_cnt.items() if v > 0]
        for e in self.engs.values():
            self._wait(e, evs)
        self.keymap = {}


class _Stop(Exception):
    pass


def build(debug=False, stop=None):
    nc = bass.Bass("TRN2", target_bir_lowering=False)

    def checkpoint(name):
        if stop == name:
            raise _Stop()

    def din(name, shape, dt=F32):
        return nc.dram_tensor(name, shape, dt, kind="ExternalInput").ap()

    def dscr(name, shape, dt):
        return nc.dram_tensor(name, shape, dt, kind=("ExternalOutput" if (debug and name in debug) else "Internal")).ap()

    xa = din("xa", [4096 + 128, D])
    pp = din("pp", [T, 256])
    w_in = din("w_in", [D, 14336])
    w_attn = din("w_attn_br", [D, D])
    w_conv = din("w_conv_br", [D, D])
    w_o = din("w_o", [D, D])
    w_pg = din("w_ple_gate", [D, D])
    w_pp = din("w_ple_proj", [256, D])
    w_eg = din("w_e_gate", [16, D, 512])
    w_eu = din("w_e_up", [16, D, 512])
    w_ed = din("w_e_down", [16 * 512, D])
    w_r = din("w_r", [D, 20])
    b_r = din("b_r", [1, 20])
    g_mix = din("g_mix", [1, D]); g_ffn = din("g_ffn", [1, D]); g_ple = din("g_ple", [1, D]); g_fin = din("g_final", [1, D])
    conv_wT = din("conv_wT", [128, 16, 31])
    conv_b = din("conv_b", [128, 16]); ln_g = din("ln_g", [128, 16]); ln_b = din("ln_b", [128, 16])
    ident_d = din("ident", [128, 128])
    sel16_d = din("sel16", [16, D])
    bias_self = din("bias_self", [16, 128, 512])
    bias_adj = din("bias_adj", [16, 128, 512])
    t31_d = din("t31", [128, 16])
    pastb_d = din("pastb", [128, 256])
    pastm_d = din("pastm", [128, 256])
    out_d = nc.dram_tensor("out", [T, D], F32, kind="ExternalOutput").ap()

    S_qT = dscr("S_qT", [16, 128, T], BF16)
    S_kT = dscr("S_kT", [16, 128, 4096], BF16)
    S_V = dscr("S_V", [4096, D], BF16)
    S_cT = dscr("S_cT", [16, 128, T], F32)
    S_gaT = dscr("S_gaT", [16, 128, T], F32)
    S_gcT = dscr("S_gcT", [16, 128, T], F32)
    S_z1T = dscr("S_z1T", [16, 128, T], F32)
    S_zT = dscr("S_zT", [16, 128, T], BF16)
    S_h1 = dscr("S_h1", [T, D], F32)
    S_h2 = dscr("S_h2", [T, D], F32)
    S_h3 = dscr("S_h3", [T, D], F32)
    S_HT = dscr("S_HT", [64, 128, T], BF16)
    S_attnT = dscr("S_attnT", [16, 128, T], BF16)
    S_mu = dscr("S_mu", [128, T], F32)
    S_rs = dscr("S_rs", [128, T], F32)
    S_convT = dscr("S_convT", [16, 128, T], BF16)
    S_comb = dscr("S_comb", [128, 16, 16], F32)
    B_d = {k: Buf(k) for k in "qT kT V cT gaT gcT z1T zT h1 h2 h3 HT out attnT mu".split()}

    es = ExitStack()
    if True:
      em = Emitter(nc, es)
      try:

        uid = [0]

        def SB(st, name, shape, dt):
            uid[0] += 1
            return st.enter_context(nc.sbuf_tensor(f"s{uid[0]}_{name}", shape, dt))

        def PS(st, name, shape, dt):
            uid[0] += 1
            return st.enter_context(nc.psum_tensor(f"p{uid[0]}_{name}", shape, dt))

        idf = SB(es, "idf", [128, 128], F32)
        idb = SB(es, "idb", [128, 128], BF16)
        onesf = SB(es, "onesf", [128, 128], F32)
        B_c = Buf("consts")
        em.dma("sp", "ld_c0", idf[:], ident_d[:, :], writes=[B_c])
        em.op("dve", lambda: nc.vector.tensor_copy(out=idb[:], in_=idf[:]), reads=[B_c], writes=[B_c])
        em.op("dve", lambda: nc.vector.memset(onesf[:], 1.0), writes=[B_c])

        def rms_stats(st, src_tile, ntiles, tag):
            xt = [SB(st, f"xs{tag}{i}", [128, D], F32) for i in range(2)]
            Bx = [Buf("xs0"), Buf("xs1")]
            junk = SB(st, f"junk{tag}", [128, D], BF16)
            Bj = Buf("junk")
            ss = SB(st, f"ss{tag}", [128, ntiles], F32)
            rstd = SB(st, f"rstd{tag}", [128, ntiles], F32)
            Bss = Buf("ss")
            em.op("dve", lambda: nc.vector.memset(ss[:], 0.0), writes=[Bss])
            for t in range(ntiles):
                s = t % 2
                em.dma("sp", f"ld_xs{s}", xt[s][:], src_tile(t), writes=[Bx[s]])
                em.op("act", lambda: nc.scalar.activation(out=junk[:], in_=xt[s][:], func=AF.Square, accum_out=ss[:, t:t + 1]),
                      reads=[Bx[s]], writes=[Bj, Bss])
            em.op("dve", lambda: nc.vector.tensor_scalar(out=rstd[:], in0=ss[:], scalar1=1.0 / D, scalar2=EPS, op0=ALU.mult, op1=ALU.add),
                  reads=[Bss], writes=[Bss])
            em.op("act", lambda: nc.scalar.activation(out=rstd[:], in_=rstd[:], func=AF.Sqrt), reads=[Bss], writes=[Bss])
            em.op("dve", lambda: nc.vector.reciprocal(out=rstd[:], in_=rstd[:]), reads=[Bss], writes=[Bss])
            return rstd, Bss

        def norm_T(st, src_tile, ntiles, g_ap, actT, Bact, tag):
            with ExitStack() as s1:
                rstd, Bss = rms_stats(s1, src_tile, ntiles, tag)
                gB = SB(s1, f"gB{tag}", [128, D], F32)
                BgB = Buf("gB")
                em.dma("sp", "ld_g", gB[:], g_ap.partition_broadcast(128), writes=[BgB])
                xt = [SB(s1, f"xt{tag}{i}", [128, D], F32) for i in range(2)]
                Bx = [Buf("xt0"), Buf("xt1")]
                xn = [SB(s1, f"xn{tag}{i}", [128, D], BF16) for i in range(2)]
                Bxn = [Buf("xn0"), Buf("xn1")]
                pt = [PS(s1, f"pt{tag}{i}", [128, 8, 128], BF16) for i in range(2)]
                Bpt = [Buf("pt0"), Buf("pt1")]
                for t in range(ntiles):
                    s = t % 2
                    em.dma("sp", f"ld_xt{s}", xt[s][:], src_tile(t), writes=[Bx[s]])
                    em.op("dve", lambda: nc.vector.scalar_tensor_tensor(out=xn[s][:], in0=xt[s][:], scalar=rstd[:, t:t + 1], in1=gB[:],
                                                                        op0=ALU.mult, op1=ALU.mult),
                          reads=[Bx[s], Bss, BgB], writes=[Bxn[s]])
                    for half in range(2):
                        def f():
                            for j in range(8):
                                c = half * 8 + j
                                ins = nc.tensor.transpose(out=pt[half][:, j, :], in_=xn[s][:, c * 128:(c + 1) * 128], identity=idb[:])
                            return ins
                        em.op("pe", f, reads=[Bxn[s], B_c], writes=[Bpt[half]])
                        dst = actT[:, half * 8:(half + 1) * 8, t * 128:(t + 1) * 128]
                        if half == 0:
                            em.op("act", lambda: nc.scalar.copy(out=dst, in_=pt[half][:, :, :]), reads=[Bpt[half]], writes=[Bact])
                        else:
                            em.op("dve", lambda: nc.vector.tensor_copy(out=dst, in_=pt[half][:, :, :]), reads=[Bpt[half]], writes=[Bact])
                em.barrier()

        class Gemm:
            def __init__(self, st, tag, nbanks=4, nw=3):
                self.wb = [SB(st, f"wb{tag}{i}", [128, 16, 512], BF16) for i in range(nw)]
                self.Bw = [[Buf(f"wb{i}a"), Buf(f"wb{i}b")] for i in range(nw)]
                self.pb = [PS(st, f"pb{tag}{i}", [128, 512], F32) for i in range(nbanks)]
                self.Bp = [Buf(f"pb{i}") for i in range(nbanks)]
                self.nw = nw
                self.bank = 0

            def next_bank(self):
                b = self.bank
                self.bank = (self.bank + 1) % len(self.pb)
                return b

            def wload(self, slot, c0, c1, src):
                part = 0 if c0 == 0 else 1
                em.dma("pool", f"ld_w{slot}_{part}", self.wb[slot][:, :, c0:c1], src.rearrange("(k p) n -> p k n", p=128), writes=[self.Bw[slot][part]])

            def run(self, blocks):
                n = len(blocks)
                for b in range(min(self.nw - 1, n)):
                    blocks[b]["load"](b % self.nw)
                for b in range(n):
                    if b + self.nw - 1 < n:
                        blocks[b + self.nw - 1]["load"]((b + self.nw - 1) % self.nw)
                    blocks[b]["run"](b % self.nw)

            def mm_fm(self, slot, c0, actT, Bact, t0, tn, nk=16):
                bk = self.next_bank()
                pbk = self.pb[bk]
                wbs = self.wb[slot]

                def f():
                    for kc in range(nk):
                        ins = nc.tensor.matmul(pbk[:, 0:tn], lhsT=wbs[:, kc, c0:c0 + 128], rhs=actT[:, kc, t0:t0 + tn],
                                               start=(kc == 0), stop=(kc == nk - 1))
                    return ins
                em.op("pe", f, reads=self.Bw[slot] + [Bact], writes=[self.Bp[bk]])
                return pbk, self.Bp[bk]

            def mm_tm(self, slot, actT, Bact, t, ncols=512, bk=None, first=True, last=True, nk=16, kofs=0):
                if bk is None:
                    bk = self.next_bank()
                pbk = self.pb[bk]
                wbs = self.wb[slot]

                def f():
                    for kc in range(nk):
                        ins = nc.tensor.matmul(pbk[:, 0:ncols], lhsT=actT[:, kofs + kc, t * 128:(t + 1) * 128], rhs=wbs[:, kc, 0:ncols],
                                               start=(first and kc == 0), stop=(last and kc == nk - 1))
                    return ins
                em.op("pe", f, reads=self.Bw[slot] + [Bact], writes=[self.Bp[bk]])
                return pbk, self.Bp[bk]

        cpy_ctr = [0]
        act_only = [False]

        def evac_copy(out, in_, reads, writes):
            cpy_ctr[0] += 1
            if act_only[0] or cpy_ctr[0] % 2:
                em.op("act", lambda: nc.scalar.copy(out=out, in_=in_), reads=reads, writes=writes)
            else:
                em.op("dve", lambda: nc.vector.tensor_copy(out=out, in_=in_), reads=reads, writes=writes)

        TG4 = [(i * 512, 512) for i in range(4)]

        def kv_blocks(G, st, actT, Bact, tok_off, tgroups, tag):
            kst = [SB(st, f"kst{tag}{i}", [128, 512], BF16) for i in range(2)]
            Bkst = [Buf("kst0"), Buf("kst1")]
            vst = [SB(st, f"vst{tag}{i}", [128, 512], BF16) for i in range(2)]
            Bvst = [Buf("vst0"), Buf("vst1")]
            blocks = []
            cnt = [0, 0]
            for kb in range(4):
                def load(slot, kb=kb):
                    G.wload(slot, 0, 512, w_in[:, 2048 + kb * 512: 2048 + (kb + 1) * 512])

                def run(slot, kb=kb):
                    for sub in range(4):
                        h = kb * 4 + sub
                        for (t0, tn) in tgroups:
                            s = cnt[0] % 2
                            cnt[0] += 1
                            pbk, Bp = G.mm_fm(slot, sub * 128, actT, Bact, t0, tn)
                            evac_copy(kst[s][:, 0:tn], pbk[:, 0:tn], [Bp], [Bkst[s]])
                            em.dma("sp", f"st_k{s}", S_kT[h, :, tok_off + t0:tok_off + t0 + tn], kst[s][:, 0:tn], reads=[Bkst[s]], writes=[B_d["kT"]])
                blocks.append(dict(load=load, run=run))
            for vb in range(4):
                def load(slot, vb=vb):
                    G.wload(slot, 0, 512, w_in[:, 4096 + vb * 512: 4096 + (vb + 1) * 512])

                def run(slot, vb=vb):
                    for t in range(16):
                        s = cnt[1] % 2
                        cnt[1] += 1
                        pbk, Bp = G.mm_tm(slot, actT, Bact, t)
                        evac_copy(vst[s][:], pbk[:, :], [Bp], [Bvst[s]])
                        em.dma("sp", f"st_v{s}", S_V[tok_off + t * 128: tok_off + (t + 1) * 128, vb * 512:(vb + 1) * 512], vst[s][:],
                               reads=[Bvst[s]], writes=[B_d["V"]])
                blocks.append(dict(load=load, run=run))
            return blocks

        with ExitStack() as st:
            actT = SB(st, "actT0", [128, 16, T], BF16)
            Bact = Buf("actT0")
            norm_T(st, lambda t: xa[2048 + t * 128: 2048 + (t + 1) * 128, :], 16, g_mix, actT, Bact, "a")
            G = Gemm(st, "a")
            G.run(kv_blocks(G, st, actT, Bact, 2048, TG4, "a"))
            em.barrier()

        checkpoint("B0")
        with ExitStack() as st:
            TA = T + 128
            actT = SB(st, "actT1", [128, 16, TA], BF16)
            Bact = Buf("actT1")

            def src1(t):
                if t < 16:
                    return xa[t * 128:(t + 1) * 128, :]
                return xa[4096:4096 + 128, :]
            norm_T(st, src1, 17, g_mix, actT, Bact, "b")
            act_only[0] = True
            G = Gemm(st, "b")
            cw = SB(st, "cw", [128, 16, 31], F32); cb = SB(st, "cb", [128, 16], F32)
            Bcw = Buf("cw")
            em.dma("sp", "ld_cw", cw[:], conv_wT[:, :, :], writes=[Bcw])
            em.dma("sp", "ld_cw", cb[:], conv_b[:, :], writes=[Bcw])
            csum = SB(st, "csum", [128, T], F32); csq = SB(st, "csq", [128, T], F32)
            Bcs = Buf("csum"); Bcq = Buf("csq")
            em.op("pool", lambda: nc.gpsimd.memset(csum[:], 0.0), writes=[Bcs])
            em.op("pool", lambda: nc.gpsimd.memset(csq[:], 0.0), writes=[Bcq])

            blocks_other = []
            qst = [SB(st, f"qst{i}", [128, 512], BF16) for i in range(2)]
            Bqst = [Buf("qst0"), Buf("qst1")]
            qcnt = [0]
            for qb in range(4):
                def load(slot, qb=qb):
                    G.wload(slot, 0, 512, w_in[:, qb * 512:(qb + 1) * 512])

                def run(slot, qb=qb):
                    for sub in range(4):
                        h = qb * 4 + sub
                        for (t0, tn) in TG4:
                            s = qcnt[0] % 2
                            qcnt[0] += 1
                            pbk, Bp = G.mm_fm(slot, sub * 128, actT, Bact, t0, tn)
                            evac_copy(qst[s][:, 0:tn], pbk[:, 0:tn], [Bp], [Bqst[s]])
                            em.dma("sp", f"st_q{s}", S_qT[h, :, t0:t0 + tn], qst[s][:, 0:tn], reads=[Bqst[s]], writes=[B_d["qT"]])
                blocks_other.append(dict(load=load, run=run))
            blocks_other += kv_blocks(G, st, actT, Bact, 0, TG4, "b")
            gst = [SB(st, f"gst{i}", [128, 512], F32) for i in range(2)]
            Bgst = [Buf("gst0"), Buf("gst1")]
            gcnt = [0]
            for gb in range(8):
                def load(slot, gb=gb):
                    G.wload(slot, 0, 512, w_in[:, 10240 + gb * 512: 10240 + (gb + 1) * 512])

                def run(slot, gb=gb):
                    for sub in range(4):
                        ch = (gb % 4) * 4 + sub
                        dst = S_gaT if gb < 4 else S_gcT
                        for (t0, tn) in TG4:
                            s = gcnt[0] % 2
                            gcnt[0] += 1
                            pbk, Bp = G.mm_fm(slot, sub * 128, actT, Bact, t0, tn)
                            em.op("act", lambda: nc.scalar.activation(out=gst[s][:, 0:tn], in_=pbk[:, 0:tn], func=AF.Sigmoid),
                                  reads=[Bp], writes=[Bgst[s]])
                            em.dma("sp", f"st_g{s}", dst[ch, :, t0:t0 + tn], gst[s][:, 0:tn], reads=[Bgst[s]], writes=[B_d["gaT" if gb < 4 else "gcT"]])
                blocks_other.append(dict(load=load, run=run))
            A_sb = SB(st, "A_sb", [128, TA], F32); BA = Buf("A")
            Us = [SB(st, f"U{i}", [128, 32 + T], F32) for i in range(2)]; BUs = [Buf("U0"), Buf("U1")]
            U = Us[0]; BU = BUs[0]
            acc = [SB(st, f"cacc{i}", [128, T], F32) for i in range(2)]
            Bacc = [Buf("cacc0"), Buf("cacc1")]
            sqb = SB(st, "sqb", [128, T], F32); Bsq = Buf("sqb")
            TG5 = TG4 + [(T, 128)]
            pending_stats = []

            def flush_stats():
                while pending_stats:
                    pending_stats.pop(0)()
            blocks_conv = []
            for cc in range(16):
                def load(slot, cc=cc):
                    G.wload(slot, 0, 128, w_in[:, 6144 + cc * 128: 6144 + (cc + 1) * 128])
                    G.wload(slot, 128, 256, w_in[:, 8192 + cc * 128: 8192 + (cc + 1) * 128])

                def run(slot, cc=cc):
                    flush_stats()
                    U = Us[cc % 2]
                    BU = BUs[cc % 2]
                    for (t0, tn) in TG5:
                        pbk, Bp = G.mm_fm(slot, 0, actT, Bact, t0, tn)
                        em.op("act", lambda: nc.scalar.copy(out=A_sb[:, t0:t0 + tn], in_=pbk[:, 0:tn]), reads=[Bp], writes=[BA])
                    for (t0, tn) in TG5:
                        pbk, Bp = G.mm_fm(slot, 128, actT, Bact, t0, tn)
                        if t0 < T:
                            em.op("act", lambda: nc.scalar.activation(out=U[:, 32 + t0:32 + t0 + tn], in_=pbk[:, 0:tn], func=AF.Sigmoid),
                                  reads=[Bp], writes=[BU])
                        else:
                            em.op("act", lambda: nc.scalar.activation(out=U[:, 0:32], in_=pbk[:, 96:128], func=AF.Sigmoid),
                                  reads=[Bp], writes=[BU])
                    em.op("dve", lambda: nc.vector.tensor_tensor(out=U[:, 0:32], in0=U[:, 0:32], in1=A_sb[:, T + 96:T + 128], op=ALU.mult),
                          reads=[BA, BU], writes=[BU])
                    em.op("dve", lambda: nc.vector.tensor_tensor(out=U[:, 32:32 + T], in0=U[:, 32:32 + T], in1=A_sb[:, 0:T], op=ALU.mult),
                          reads=[BA, BU], writes=[BU])
                    a = acc[cc % 2]
                    Ba = Bacc[cc % 2]
                    em.op("dve", lambda: nc.vector.tensor_scalar(out=a[:], in0=U[:, 2:2 + T], scalar1=cw[:, cc, 0:1], scalar2=cb[:, cc:cc + 1],
                                                                 op0=ALU.mult, op1=ALU.add), reads=[BU, Bcw], writes=[Ba])
                    for j in range(1, 31):
                        em.op("dve", lambda: nc.vector.scalar_tensor_tensor(out=a[:], in0=U[:, 2 + j:2 + j + T], scalar=cw[:, cc, j:j + 1], in1=a[:],
                                                                            op0=ALU.mult, op1=ALU.add), reads=[BU, Bcw, Ba], writes=[Ba])

                    def stats(a=a, Ba=Ba, cc=cc):
                        em.dma("sp", f"st_c{cc % 2}", S_cT[cc, :, :], a[:], reads=[Ba], writes=[B_d["cT"]])
                        em.op("pool", lambda: nc.gpsimd.tensor_tensor(out=sqb[:], in0=a[:], in1=a[:], op=ALU.mult), reads=[Ba], writes=[Bsq])
                        em.op("pool", lambda: nc.gpsimd.tensor_tensor(out=csum[:], in0=csum[:], in1=a[:], op=ALU.add), reads=[Ba, Bcs], writes=[Bcs])
                        em.op("pool", lambda: nc.gpsimd.tensor_tensor(out=csq[:], in0=csq[:], in1=sqb[:], op=ALU.add), reads=[Bsq, Bcq], writes=[Bcq])
                    pending_stats.append(stats)
                blocks_conv.append(dict(load=load, run=run))
            order = []
            for n in range(len(blocks_other)):
                order.append(blocks_other[n])
                if n < 16:
                    order.append(blocks_conv[n])
            G.run(order)
            flush_stats()
            act_only[0] = False
            mu = A_sb[:, 0:T]; rs = U[:, 0:T]
            Bmu = BA; Brs = BU
            for (t0, tn) in TG4:
                bk = G.next_bank()
                em.op("pe", lambda: nc.tensor.matmul(G.pb[bk][:, :], lhsT=onesf[:], rhs=csum[:, t0:t0 + tn], start=True, stop=True),
                      reads=[Bcs, B_c], writes=[G.Bp[bk]])
                em.op("dve", lambda: nc.vector.tensor_scalar(out=mu[:, t0:t0 + tn], in0=G.pb[bk][:, :], scalar1=1.0 / D, scalar2=None, op0=ALU.mult),
                      reads=[G.Bp[bk]], writes=[Bmu])
                bk = G.next_bank()
                em.op("pe", lambda: nc.tensor.matmul(G.pb[bk][:, :], lhsT=onesf[:], rhs=csq[:, t0:t0 + tn], start=True, stop=True),
                      reads=[Bcq, B_c], writes=[G.Bp[bk]])
                em.op("dve", lambda: nc.vector.tensor_scalar(out=rs[:, t0:t0 + tn], in0=G.pb[bk][:, :], scalar1=1.0 / D, scalar2=EPS, op0=ALU.mult, op1=ALU.add),
                      reads=[G.Bp[bk]], writes=[Brs])
            em.op("dve", lambda: nc.vector.tensor_tensor(out=sqb[:], in0=mu, in1=mu, op=ALU.mult), reads=[Bmu], writes=[Bsq])
            em.op("dve", lambda: nc.vector.tensor_tensor(out=rs, in0=rs, in1=sqb[:], op=ALU.subtract), reads=[Brs, Bsq], writes=[Brs])
            em.op("act", lambda: nc.scalar.activation(out=rs, in_=rs, func=AF.Sqrt), reads=[Brs], writes=[Brs])
            em.op("dve", lambda: nc.vector.reciprocal(out=rs, in_=rs), reads=[Brs], writes=[Brs])
            em.dma("sp", "st_mu", S_mu[:, :], mu, reads=[Bmu], writes=[B_d["mu"]])
            em.dma("sp", "st_mu", S_rs[:, :], rs, reads=[Brs], writes=[B_d["mu"]])
            em.barrier()

        checkpoint("B1")
        with ExitStack() as st:
            attnT = SB(st, "attnT", [128, 16, T], BF16)
            BattnT = Buf("attnT")
            qs = [SB(st, f"qs{i}", [128, 8, 256], BF16) for i in range(2)]
            ks = [SB(st, f"ks{i}", [128, 16, 256], BF16) for i in range(2)]
            vs = [SB(st, f"vs{i}", [128, 32, 136], BF16) for i in range(2)]
            bsf = [SB(st, f"bsf{i}", [128, 512], F32) for i in range(2)]
            baj = [SB(st, f"baj{i}", [128, 512], F32) for i in range(2)]
            Bhq = [Buf("hq0"), Buf("hq1")]; Bhk = [Buf("hk0"), Buf("hk1")]; Bhv = [Buf("hv0"), Buf("hv1")]; Bhb = [Buf("hb0"), Buf("hb1")]
            t31 = SB(st, "t31", [128, 16], F32)
            pastb = SB(st, "pastb", [128, 16, 16], F32)
            pastm = SB(st, "pastm", [128, 16, 16], F32)
            Bmk = Buf("masks")
            em.dma("sp", "ld_mk", t31[:], t31_d[:, :], writes=[Bmk])
            em.dma("sp", "ld_mk", pastb[:, :, :], pastb_d.rearrange("p (a b) -> p a b", b=16), writes=[Bmk])
            em.dma("sp", "ld_mk", pastm[:, :, :], pastm_d.rearrange("p (a b) -> p a b", b=16), writes=[Bmk])
            for i in range(2):
                em.op("dve", lambda: nc.vector.memset(vs[i][:, :, 128:136], 0.0), writes=[Bhv[i]])
                em.op("dve", lambda: nc.vector.memset(vs[i][:, :, 128:129], 1.0), writes=[Bhv[i]])
            km = SB(st, "km", [128, 16], F32); kmb = SB(st, "kmb", [128, 16], BF16); Bkm = Buf("km")
            gate = SB(st, "gate", [128, 16, 16], F32); Bgate = Buf("gate")
            m8 = SB(st, "m8", [128, 16, 8], F32); Bm8 = Buf("m8")
            selm = [SB(st, f"selm{i}", [128, 16, 16], F32) for i in range(2)]
            Bsel = [Buf("sel0"), Buf("sel1")]
            tmpb = [SB(st, f"tmpb{i}", [128, 512], F32) for i in range(2)]
            Btmp = [Buf("tmpb0"), Buf("tmpb1")]
            PT = [SB(st, f"PT{i}", [128, 512], BF16) for i in range(3)]
            BPT = [Buf(f"PT{i}") for i in range(3)]
            oacc = [SB(st, f"oacc{i}", [128, 2, 129], F32) for i in range(2)]
            Boacc = [Buf("oacc0"), Buf("oacc1")]
            rden = SB(st, "rden", [128, 2], F32); Brden = Buf("rden")
            obf = SB(st, "obf", [128, 2, 128], BF16); Bobf = Buf("obf")
            pS = [PS(st, f"pS{i}", [128, 512], F32) for i in range(3)]
            BpS = [Buf(f"pS{i}") for i in range(3)]
            pO = [PS(st, f"pO{i}", [128, 2, 256], F32) for i in range(3)]
            BpO = [Buf(f"pO{i}") for i in range(3)]
            pG = PS(st, "pG", [128, 32, 16], F32); BpG = Buf("pG")
            pTr = PS(st, "pTr", [128, 1024], BF16); BpTr = Buf("pTr")

            def head_load(h):
                s = h % 2
                em.dma("sp", f"ld_hq{s}", qs[s][:, :, :], S_qT[h].rearrange("p (j t) -> p j t", t=256), reads=[B_d["qT"]], writes=[Bhq[s]])
                em.dma("sp", f"ld_hk{s}", ks[s][:, :, :], S_kT[h].rearrange("p (j t) -> p j t", t=256), reads=[B_d["kT"]], writes=[Bhk[s]])
                em.dma("sp", f"ld_hv{s}", vs[s][:, :, 0:128], S_V[:, h * 128:(h + 1) * 128].rearrange("(kt p) d -> p kt d", p=128),
                       reads=[B_d["V"]], writes=[Bhv[s]])
                em.dma("sp", f"ld_hb{s}", bsf[s][:], bias_self[h], writes=[Bhb[s]])
                em.dma("sp", f"ld_hc{s}", baj[s][:], bias_adj[h], writes=[Bhb[s]])

            def head_prologue(h):
                s = h % 2
                em.op("dve", lambda: nc.vector.tensor_reduce(out=km[:], in_=ks[s][:, :, :], axis=AX.X, op=ALU.add), reads=[Bhk[s]], writes=[Bkm])
                em.op("dve", lambda: nc.vector.tensor_scalar(out=kmb[:], in0=km[:], scalar1=1.0 / 256, scalar2=None, op0=ALU.mult), reads=[Bkm], writes=[Bkm])

                def f():
                    for qt in range(16):
                        ins = nc.tensor.matmul(pG[:, qt, :], lhsT=qs[s][:, qt // 2, (qt % 2) * 128:(qt % 2 + 1) * 128], rhs=kmb[:, :], start=True, stop=True)
                    return ins
                em.op("pe", f, reads=[Bhq[s], Bkm], writes=[BpG])
                em.op("dve", lambda: nc.vector.tensor_tensor(out=gate[:, :, :], in0=pG[:, 0:16, :], in1=pastb[:, :, :], op=ALU.add),
                      reads=[BpG, Bmk], writes=[Bgate])
                for qt in range(16):
                    em.op("dve", lambda: nc.vector.max(out=m8[:, qt, :], in_=gate[:, qt, :]), reads=[Bgate], writes=[Bm8])
                sm = selm[s]
                for qt in range(16):
                    em.op("dve", lambda: nc.vector.tensor_scalar(out=sm[:, qt, :], in0=gate[:, qt, :], scalar1=m8[:, qt, 2:3], scalar2=None, op0=ALU.is_ge),
                          reads=[Bgate, Bm8], writes=[Bsel[s]])
                em.op("dve", lambda: nc.vector.tensor_tensor(out=sm[:, :, :], in0=sm[:, :, :], in1=pastm[:, :, :], op=ALU.mult),
                      reads=[Bsel[s], Bmk], writes=[Bsel[s]])

            pairs = []
            for h in range(16):
                for i in range(8):
                    lst = [(h, i, i, 0)]
                    for j in range(i):
                        lst.append((h, i, j, 1 if j == i - 1 else 2))
                    for k in range(8):
                        lst.append((h, i, 8 + k, 1 if (i == 0 and k == 7) else 2))
                    for n, p in enumerate(lst):
                        pairs.append(p + (n == 0, n == len(lst) - 1))

            def emit_S(n):
                h, i, j, kind, first, last = pairs[n]
                s = h % 2
                b = n % 3

                def f():
                    for kt in range(2):
                        ins = nc.tensor.matmul(pS[b][:, kt * 256:(kt + 1) * 256], lhsT=ks[s][:, j, kt * 128:(kt + 1) * 128], rhs=qs[s][:, i, :],
                                               start=True, stop=True)
                    return ins
                em.op("pe", f, reads=[Bhq[s], Bhk[s]], writes=[BpS[b]])

            head_load(0)
            head_prologue(0)
            emit_S(0)
            emit_S(1)
            for n in range(len(pairs)):
                h, i, j, kind, first, last = pairs[n]
                s = h % 2
                b = n % 3
                if first and i == 0 and h + 1 < 16:
                    head_load(h + 1)
                if n + 2 < len(pairs):
                    if pairs[n + 2][0] != pairs[n + 1][0]:
                        head_prologue(pairs[n + 2][0])
                    emit_S(n + 2)
                if kind == 2:
                    em.op("act", lambda: nc.scalar.activation(out=PT[b][:], in_=pS[b][:], func=AF.Exp, bias=t31[:, h:h + 1], scale=SCALE),
                          reads=[BpS[b], Bmk], writes=[BPT[b]])
                else:
                    tb = n % 2
                    btile = bsf[s] if kind == 0 else baj[s]
                    em.op("dve", lambda: nc.vector.scalar_tensor_tensor(out=tmpb[tb][:], in0=pS[b][:], scalar=SCALE, in1=btile[:], op0=ALU.mult, op1=ALU.add),
                          reads=[BpS[b], Bhb[s]], writes=[Btmp[tb]])
                    em.op("act", lambda: nc.scalar.activation(out=PT[b][:], in_=tmpb[tb][:], func=AF.Exp), reads=[Btmp[tb]], writes=[BPT[b]])

                def f():
                    for q2 in range(2):
                        for kt in range(2):
                            ins = nc.tensor.matmul(pO[b][:, q2, 0:132], lhsT=PT[b][:, kt * 256 + q2 * 128: kt * 256 + (q2 + 1) * 128],
                                                   rhs=vs[s][:, j * 2 + kt, 0:132], start=(kt == 0), stop=(kt == 1))
                    return ins
                em.op("pe", f, reads=[BPT[b], Bhv[s]], writes=[BpO[b]])
                oa = oacc[i % 2]
                Boa = Boacc[i % 2]
                if first:
                    em.op("dve", lambda: nc.vector.tensor_copy(out=oa[:, :, :], in_=pO[b][:, :, 0:129]), reads=[BpO[b]], writes=[Boa])
                else:
                    for q2 in range(2):
                        em.op("dve", lambda: nc.vector.scalar_tensor_tensor(out=oa[:, q2, :], in0=pO[b][:, q2, 0:129], scalar=selm[s][:, i * 2 + q2, j:j + 1],
                                                                            in1=oa[:, q2, :], op0=ALU.mult, op1=ALU.add),
                              reads=[BpO[b], Bsel[s], Boa], writes=[Boa])
                if last:
                    em.op("dve", lambda: nc.vector.reciprocal(out=rden[:, :], in_=oa[:, :, 128]), reads=[Boa], writes=[Brden])
                    for q2 in range(2):
                        em.op("dve", lambda: nc.vector.tensor_scalar(out=obf[:, q2, :], in0=oa[:, q2, 0:128], scalar1=rden[:, q2:q2 + 1], scalar2=None, op0=ALU.mult),
                              reads=[Boa, Brden], writes=[Bobf])

                    def f():
                        for q2 in range(2):
                            ins = nc.tensor.transpose(out=pTr[:, q2 * 128:(q2 + 1) * 128], in_=obf[:, q2, :], identity=idb[:])
                        return ins
                    em.op("pe", f, reads=[Bobf, B_c], writes=[BpTr])
                    em.op("act", lambda: nc.scalar.copy(out=attnT[:, h, i * 256:(i + 1) * 256], in_=pTr[:, 0:256]), reads=[BpTr], writes=[BattnT])
            em.dma("sp", "st_at", S_attnT.rearrange("c p t -> p c t"), attnT[:, :, :], reads=[BattnT], writes=[B_d["attnT"]])
            em.barrier()

        checkpoint("C")
        with ExitStack() as st:
            D1_attnT = SB(st, "attnT1", [128, 16, T], BF16)
            D1_B = Buf("attnT1")
            em.dma("sp", "ld_act", D1_attnT[:, :, :], S_attnT.rearrange("c p t -> p c t"), reads=[B_d["attnT"]], writes=[D1_B])
            if True:
                st2 = st
                G = Gemm(st2, "d1")
                gsb = [SB(st2, f"gsb{i}", [128, T], F32) for i in range(2)]
                Bgsb = [Buf("gsb0"), Buf("gsb1")]
                zst = [SB(st2, f"zst{i}", [128, T], F32) for i in range(2)]
                Bzst = [Buf("zst0"), Buf("zst1")]
                blocks = []
                cnt = [0]
                for ob in range(4):
                    def load(slot, ob=ob):
                        G.wload(slot, 0, 512, w_attn[:, ob * 512:(ob + 1) * 512])

                    def run(slot, ob=ob):
                        for sub in range(4):
                            ch = ob * 4 + sub
                            s = cnt[0] % 2
                            cnt[0] += 1
                            em.dma("sp", f"ld_gs{s}", gsb[s][:], S_gaT[ch, :, :], reads=[B_d["gaT"]], writes=[Bgsb[s]])
                            for (t0, tn) in TG4:
                                pbk, Bp = G.mm_fm(slot, sub * 128, D1_attnT, D1_B, t0, tn)
                                em.op("dve", lambda: nc.vector.tensor_tensor(out=zst[s][:, t0:t0 + tn], in0=pbk[:, 0:tn], in1=gsb[s][:, t0:t0 + tn], op=ALU.mult),
                                      reads=[Bp, Bgsb[s]], writes=[Bzst[s]])
                            em.dma("sp", f"st_z{s}", S_z1T[ch, :, :], zst[s][:], reads=[Bzst[s]], writes=[B_d["z1T"]])
                    blocks.append(dict(load=load, run=run))
                G.run(blocks)
                em.barrier()

        checkpoint("D1")
        with ExitStack() as st:
            convT = SB(st, "convT", [128, 16, T], BF16)
            BconvT = Buf("convT")
            lg = SB(st, "lg", [128, 16], F32); lb = SB(st, "lb", [128, 16], F32); Blg = Buf("lg")
            em.dma("sp", "ld_lg", lg[:], ln_g[:, :], writes=[Blg])
            em.dma("sp", "ld_lg", lb[:], ln_b[:, :], writes=[Blg])
            mu = SB(st, "mu", [128, T], F32); rs = SB(st, "rs", [128, T], F32)
            Bmu = Buf("mu"); Brs = Buf("rs")
            em.dma("sp", "ld_mu", mu[:], S_mu[:, :], reads=[B_d["mu"]], writes=[Bmu])
            em.dma("sp", "ld_rs", rs[:], S_rs[:, :], reads=[B_d["mu"]], writes=[Brs])
            with ExitStack() as st2:
                cl = [SB(st2, f"cl{i}", [128, T], F32) for i in range(2)]
                Bcl = [Buf("cl0"), Buf("cl1")]
                for cc in range(16):
                    s = cc % 2
                    em.dma("sp", f"ld_cl{s}", cl[s][:], S_cT[cc, :, :], reads=[B_d["cT"]], writes=[Bcl[s]])
                    em.op("dve", lambda: nc.vector.tensor_tensor(out=cl[s][:], in0=cl[s][:], in1=mu[:], op=ALU.subtract), reads=[Bcl[s], Bmu], writes=[Bcl[s]])
                    em.op("dve", lambda: nc.vector.tensor_tensor(out=cl[s][:], in0=cl[s][:], in1=rs[:], op=ALU.mult), reads=[Bcl[s], Brs], writes=[Bcl[s]])
                    em.op("act", lambda: nc.scalar.activation(out=convT[:, cc, :], in_=cl[s][:], func=AF.Silu, scale=lg[:, cc:cc + 1], bias=lb[:, cc:cc + 1]),
                          reads=[Bcl[s], Blg], writes=[BconvT])
                if debug and "S_convT" in debug:
                    for cc in range(16):
                        em.dma("sp", "st_dbg", S_convT[cc, :, :], convT[:, cc, :], reads=[BconvT], writes=[Buf("dbg")])
                em.barrier()
            with ExitStack() as st2:
                G = Gemm(st2, "d2")
                gsb = [SB(st2, f"gsc{i}", [128, T], F32) for i in range(2)]
                Bgsb = [Buf("gsc0"), Buf("gsc1")]
                z1b = [SB(st2, f"z1b{i}", [128, T], F32) for i in range(2)]
                Bz1b = [Buf("z1b0"), Buf("z1b1")]
                zst = [SB(st2, f"zsb{i}", [128, T], BF16) for i in range(2)]
                Bzst = [Buf("zsb0"), Buf("zsb1")]
                tmpz = [SB(st2, f"tmpz{i}", [128, 512], F32) for i in range(2)]
                Btz = [Buf("tmpz0"), Buf("tmpz1")]
                blocks = []
                cnt = [0, 0]
                for ob in range(4):
                    def load(slot, ob=ob):
                        G.wload(slot, 0, 512, w_conv[:, ob * 512:(ob + 1) * 512])

                    def run(slot, ob=ob):
                        for sub in range(4):
                            ch = ob * 4 + sub
                            s = cnt[0] % 2
                            cnt[0] += 1
                            em.dma("sp", f"ld_gs{s}", gsb[s][:], S_gcT[ch, :, :], reads=[B_d["gcT"]], writes=[Bgsb[s]])
                            em.dma("sp", f"ld_z1{s}", z1b[s][:], S_z1T[ch, :, :], reads=[B_d["z1T"]], writes=[Bz1b[s]])
                            for (t0, tn) in TG4:
                                pbk, Bp = G.mm_fm(slot, sub * 128, convT, BconvT, t0, tn)
                                u = cnt[1] % 2
                                cnt[1] += 1
                                em.op("dve", lambda: nc.vector.tensor_tensor(out=tmpz[u][:, 0:tn], in0=pbk[:, 0:tn], in1=gsb[s][:, t0:t0 + tn], op=ALU.mult),
                                      reads=[Bp, Bgsb[s]], writes=[Btz[u]])
                                em.op("dve", lambda: nc.vector.tensor_tensor(out=zst[s][:, t0:t0 + tn], in0=tmpz[u][:, 0:tn], in1=z1b[s][:, t0:t0 + tn], op=ALU.add),
                                      reads=[Btz[u], Bz1b[s]], writes=[Bzst[s]])
                            em.dma("sp", f"st_z{s}", S_zT[ch, :, :], zst[s][:], reads=[Bzst[s]], writes=[B_d["zT"]])
                    blocks.append(dict(load=load, run=run))
                G.run(blocks)
                em.barrier()

        checkpoint("B3")
        def resid_gemm(st, tag, actT, Bact, w_ap, res_src, res_buf, dst, dst_key):
            G = Gemm(st, tag)
            xsl = [SB(st, f"xsl{tag}{i}", [128, 512], F32) for i in range(3)]
            Bxsl = [Buf(f"xsl{i}") for i in range(3)]
            hst = [SB(st, f"hst{tag}{i}", [128, 512], F32) for i in range(2)]
            Bhst = [Buf("hst0"), Buf("hst1")]
            blocks = []
            cnt = [0]
            for ob in range(4):
                def load(slot, ob=ob):
                    G.wload(slot, 0, 512, w_ap[:, ob * 512:(ob + 1) * 512])

                def run(slot, ob=ob):
                    def ldx(t):
                        em.dma("sp", f"ld_xsl{t % 3}", xsl[t % 3][:], res_src(t, ob), reads=res_buf, writes=[Bxsl[t % 3]])
                    ldx(0)
                    ldx(1)
                    for t in range(16):
                        if t + 2 < 16:
                            ldx(t + 2)
                        pbk, Bp = G.mm_tm(slot, actT, Bact, t)
                        s = cnt[0] % 2
                        cnt[0] += 1
                        em.op("dve", lambda: nc.vector.tensor_tensor(out=hst[s][:], in0=pbk[:, :], in1=xsl[t % 3][:], op=ALU.add),
                              reads=[Bp, Bxsl[t % 3]], writes=[Bhst[s]])
                        em.dma("sp", f"st_h{s}", dst[t * 128:(t + 1) * 128, ob * 512:(ob + 1) * 512], hst[s][:], reads=[Bhst[s]], writes=[B_d[dst_key]])
                blocks.append(dict(load=load, run=run))
            G.run(blocks)

        with ExitStack() as st:
            zT = SB(st, "zTa", [128, 16, T], BF16)
            BzT = Buf("zTa")
            em.dma("sp", "ld_act", zT[:, :, :], S_zT.rearrange("c p t -> p c t"), reads=[B_d["zT"]], writes=[BzT])
            resid_gemm(st, "e", zT, BzT, w_o, lambda t, ob: xa[t * 128:(t + 1) * 128, ob * 512:(ob + 1) * 512], [], S_h1, "h1")
            em.barrier()

        checkpoint("E")
        with ExitStack() as st:
            x2T = SB(st, "x2T", [128, 16, T], BF16)
            Bx2T = Buf("x2T")
            combT = SB(st, "combT", [16, T], F32)
            BcombT = Buf("combT")
            sel_sb = SB(st, "sel_sb", [16, D], F32)
            Bsl = Buf("sel_sb")
            em.dma("sp", "ld_sl", sel_sb[:], sel16_d[:, :], writes=[Bsl])
            with ExitStack() as s1:
                src = lambda t: S_h1[t * 128:(t + 1) * 128, :]
                rstd, Bss = rms_stats(s1, src, 16, "f")
                gB = SB(s1, "gBf", [128, D], F32); BgB = Buf("gBf")
                em.dma("sp", "ld_g", gB[:], g_ffn.partition_broadcast(128), writes=[BgB])
                wr = SB(s1, "wr", [128, 16, 20], F32); Bwr = Buf("wr")
                em.dma("sp", "ld_wr", wr[:], w_r.rearrange("(k p) n -> p k n", p=128), writes=[Bwr])
                brb = SB(s1, "brb", [128, 20], F32)
                em.dma("sp", "ld_wr", brb[:], b_r.partition_broadcast(128), writes=[Bwr])
                xt = [SB(s1, f"xtf{i}", [128, D], F32) for i in range(2)]
                Bx = [Buf("xtf0"), Buf("xtf1")]
                xn = [SB(s1, f"xnf{i}", [128, D], F32) for i in range(2)]
                Bxn = [Buf("xnf0"), Buf("xnf1")]
                xf = [SB(s1, f"xf{i}", [128, 16, 128], F32) for i in range(2)]
                Bxf = [Buf("xf0"), Buf("xf1")]
                pF = [PS(s1, f"pF{i}", [128, 4, 128], F32) for i in range(4)]
                BpF = [Buf(f"pF{i}") for i in range(4)]
                pR = PS(s1, "pR", [128, 512], F32); BpR = Buf("pR")
                pC = PS(s1, "pC", [128, 512], F32); BpC = Buf("pC")
                comb = SB(s1, "comb", [128, 16, 16], F32); Bcomb = Buf("comb")
                R = {k: SB(s1, "r_" + k, [128, n], F32) for k, n in
                     dict(L=20, cmax=1, ncmax=1, ohg=4, ecl=4, esum=1, pg=1, fsel=4, v1=1, m1=4, fs2=4, v2=1, m2=4, d=1, e=1, den=1,
                          w1=1, w2=1, t1=4, fine=4, pf=4).items()}
                BR = Buf("router_scratch")

                def dv(fn, reads=(), writes=()):
                    em.op("dve", fn, reads=[BR] + list(reads), writes=[BR] + list(writes))

                for t in range(16):
                    s = t % 2
                    em.dma("sp", f"ld_xt{s}", xt[s][:], S_h1[t * 128:(t + 1) * 128, :], reads=[B_d["h1"]], writes=[Bx[s]])
                    em.op("dve", lambda: nc.vector.scalar_tensor_tensor(out=xn[s][:], in0=xt[s][:], scalar=rstd[:, t:t + 1], in1=gB[:], op0=ALU.mult, op1=ALU.mult),
                          reads=[Bx[s], Bss, BgB], writes=[Bxn[s]])
                    for k in range(4):
                        def f():
                            for j in range(4):
                                c = k * 4 + j
                                ins = nc.tensor.transpose(out=pF[k][:, j, :], in_=xn[s][:, c * 128:(c + 1) * 128], identity=idf[:])
                            return ins
                        em.op("pe", f, reads=[Bxn[s], B_c], writes=[BpF[k]])
                        em.op("dve", lambda: nc.vector.tensor_copy(out=xf[s][:, k * 4:(k + 1) * 4, :], in_=pF[k][:, :, :]), reads=[BpF[k]], writes=[Bxf[s]])
                        em.op("act", lambda: nc.scalar.copy(out=x2T[:, k * 4:(k + 1) * 4, t * 128:(t + 1) * 128], in_=xf[s][:, k * 4:(k + 1) * 4, :]), reads=[Bxf[s]], writes=[Bx2T])

                    def f():
                        for c in range(16):
                            ins = nc.tensor.matmul(pR[:, 0:20], lhsT=xf[s][:, c, :], rhs=wr[:, c, :], start=(c == 0), stop=(c == 15))
                        return ins
                    em.op("pe", f, reads=[Bxf[s], Bwr], writes=[BpR])
                    L = R["L"]
                    dv(lambda: nc.vector.tensor_tensor(out=L[:], in0=pR[:, 0:20], in1=brb[:], op=ALU.add), reads=[BpR, Bwr])
                    dv(lambda: nc.vector.tensor_reduce(out=R["cmax"][:], in_=L[:, 0:4], axis=AX.X, op=ALU.max))
                    dv(lambda: nc.vector.tensor_scalar(out=R["ncmax"][:], in0=R["cmax"][:], scalar1=-1.0, scalar2=None, op0=ALU.mult))
                    dv(lambda: nc.vector.tensor_scalar(out=R["ohg"][:], in0=L[:, 0:4], scalar1=R["cmax"][:, 0:1], scalar2=None, op0=ALU.is_ge))
                    em.op("act", lambda: nc.scalar.activation(out=R["ecl"][:], in_=L[:, 0:4], func=AF.Exp, bias=R["ncmax"][:, 0:1], scale=1.0),
                          reads=[BR], writes=[BR])
                    dv(lambda: nc.vector.tensor_reduce(out=R["esum"][:], in_=R["ecl"][:], axis=AX.X, op=ALU.add))
                    dv(lambda: nc.vector.reciprocal(out=R["pg"][:], in_=R["esum"][:]))
                    dv(lambda: nc.vector.tensor_scalar(out=R["fsel"][:], in0=L[:, 4:8], scalar1=R["ohg"][:, 0:1], scalar2=None, op0=ALU.mult))
                    for g in range(1, 4):
                        dv(lambda: nc.vector.scalar_tensor_tensor(out=R["fsel"][:], in0=L[:, 4 + 4 * g:8 + 4 * g], scalar=R["ohg"][:, g:g + 1], in1=R["fsel"][:],
                                                                  op0=ALU.mult, op1=ALU.add))
                    dv(lambda: nc.vector.tensor_reduce(out=R["v1"][:], in_=R["fsel"][:], axis=AX.X, op=ALU.max))
                    dv(lambda: nc.vector.tensor_scalar(out=R["m1"][:], in0=R["fsel"][:], scalar1=R["v1"][:, 0:1], scalar2=None, op0=ALU.is_ge))
                    dv(lambda: nc.vector.scalar_tensor_tensor(out=R["fs2"][:], in0=R["m1"][:], scalar=NEG, in1=R["fsel"][:], op0=ALU.mult, op1=ALU.add))
                    dv(lambda: nc.vector.tensor_reduce(out=R["v2"][:], in_=R["fs2"][:], axis=AX.X, op=ALU.max))
                    dv(lambda: nc.vector.tensor_scalar(out=R["m2"][:], in0=R["fs2"][:], scalar1=R["v2"][:, 0:1], scalar2=None, op0=ALU.is_ge))
                    dv(lambda: nc.vector.tensor_tensor(out=R["d"][:], in0=R["v2"][:], in1=R["v1"][:], op=ALU.subtract))
                    em.op("act", lambda: nc.scalar.activation(out=R["e"][:], in_=R["d"][:], func=AF.Exp), reads=[BR], writes=[BR])
                    dv(lambda: nc.vector.tensor_scalar(out=R["den"][:], in0=R["e"][:], scalar1=1.0, scalar2=None, op0=ALU.add))
                    dv(lambda: nc.vector.reciprocal(out=R["w1"][:], in_=R["den"][:]))
                    dv(lambda: nc.vector.tensor_tensor(out=R["w2"][:], in0=R["e"][:], in1=R["w1"][:], op=ALU.mult))
                    dv(lambda: nc.vector.tensor_scalar(out=R["t1"][:], in0=R["m1"][:], scalar1=R["w1"][:, 0:1], scalar2=None, op0=ALU.mult))
                    dv(lambda: nc.vector.scalar_tensor_tensor(out=R["fine"][:], in0=R["m2"][:], scalar=R["w2"][:, 0:1], in1=R["t1"][:], op0=ALU.mult, op1=ALU.add))
                    dv(lambda: nc.vector.tensor_scalar(out=R["pf"][:], in0=R["fine"][:], scalar1=R["pg"][:, 0:1], scalar2=None, op0=ALU.mult))
                    for g in range(4):
                        dv(lambda: nc.vector.tensor_scalar(out=comb[:, t, 4 * g:4 * g + 4], in0=R["pf"][:], scalar1=R["ohg"][:, g:g + 1], scalar2=None, op0=ALU.mult),
                           writes=[Bcomb])
                    em.op("pe", lambda: nc.tensor.matmul(pC[0:16, 0:128], lhsT=comb[:, t, :], rhs=idf[:], start=True, stop=True), reads=[Bcomb, B_c], writes=[BpC])
                    em.op("act", lambda: nc.scalar.copy(out=combT[:, t * 128:(t + 1) * 128], in_=pC[0:16, 0:128]), reads=[BpC], writes=[BcombT])
                if debug and "S_comb" in debug:
                    em.dma("sp", "st_dbg", S_comb[:, :, :], comb[:, :, :], reads=[Bcomb], writes=[Buf("dbg")])
                em.barrier()
                checkpoint("F1")
            with ExitStack() as s1:
                G = Gemm(s1, "f1")
                pCB = PS(s1, "pCB", [128, 512], F32); BpCB = Buf("pCB")
                combB = [SB(s1, f"combB{i}", [128, T], F32) for i in range(2)]
                BcombB = [Buf("combB0"), Buf("combB1")]
                sg = [SB(s1, f"sg{i}", [128, T], F32) for i in range(4)]
                Bsg = [Buf(f"sg{i}") for i in range(4)]
                hst = [SB(s1, f"hstb{i}", [128, T], BF16) for i in range(2)]
                Bhst = [Buf("hstb0"), Buf("hstb1")]
                tmph = [SB(s1, f"tmph{i}", [128, 512], F32) for i in range(2)]
                Btmph = [Buf("tmph0"), Buf("tmph1")]
                blocks = []
                cnt = [0, 0]
                for e in range(16):
                    for fp in range(2):
                        def load(slot, e=e, fp=fp):
                            G.wload(slot, 0, 256, w_eg[e, :, fp * 256:(fp + 1) * 256])
                            G.wload(slot, 256, 512, w_eu[e, :, fp * 256:(fp + 1) * 256])

                        def run(slot, e=e, fp=fp):
                            cbs = combB[e % 2]
                            if fp == 0:
                                for (t0, tn) in TG4:
                                    em.op("pe", lambda: nc.tensor.matmul(pCB[:, 0:tn], lhsT=sel_sb[:, e * 128:(e + 1) * 128], rhs=combT[:, t0:t0 + tn], start=True, stop=True),
                                          reads=[Bsl, BcombT], writes=[BpCB])
                                    em.op("act", lambda: nc.scalar.copy(out=cbs[:, t0:t0 + tn], in_=pCB[:, 0:tn]), reads=[BpCB], writes=[BcombB[e % 2]])
                            par = (e * 2 + fp) % 2
                            for fl in range(2):
                                for (t0, tn) in TG4:
                                    pbk, Bp = G.mm_fm(slot, fl * 128, x2T, Bx2T, t0, tn)
                                    em.op("act", lambda: nc.scalar.activation(out=sg[par * 2 + fl][:, t0:t0 + tn], in_=pbk[:, 0:tn], func=AF.Silu),
                                          reads=[Bp], writes=[Bsg[par * 2 + fl]])
                            for fl in range(2):
                                s = cnt[0] % 2
                                cnt[0] += 1
                                for (t0, tn) in TG4:
                                    pbk, Bp = G.mm_fm(slot, 256 + fl * 128, x2T, Bx2T, t0, tn)
                                    u = cnt[1] % 2
                                    cnt[1] += 1
                                    em.op("dve", lambda: nc.vector.tensor_tensor(out=tmph[u][:, 0:tn], in0=pbk[:, 0:tn], in1=sg[par * 2 + fl][:, t0:t0 + tn], op=ALU.mult),
                                          reads=[Bp, Bsg[par * 2 + fl]], writes=[Btmph[u]])
                                    em.op("dve", lambda: nc.vector.tensor_tensor(out=hst[s][:, t0:t0 + tn], in0=tmph[u][:, 0:tn], in1=cbs[:, t0:t0 + tn], op=ALU.mult),
                                          reads=[Btmph[u], BcombB[e % 2]], writes=[Bhst[s]])
                                em.dma("sp", f"st_H{s}", S_HT[e * 4 + fp * 2 + fl, :, :], hst[s][:], reads=[Bhst[s]], writes=[B_d["HT"]])
                        blocks.append(dict(load=load, run=run))
                G.run(blocks)
                em.barrier()
                checkpoint("F2")
        with ExitStack() as st:
            G = Gemm(st, "f2", nbanks=8, nw=3)
            HTq = [SB(st, f"HTq{i}", [128, 16, 512], BF16) for i in range(4)]
            BHTq = [Buf(f"HTq{i}") for i in range(4)]
            xsl = [SB(st, f"xslf{i}", [128, 512], F32) for i in range(4)]
            Bxsl = [Buf(f"xslf{i}") for i in range(4)]
            hst = [SB(st, f"hstf{i}", [128, 512], F32) for i in range(2)]
            Bhst = [Buf("hstf0"), Buf("hstf1")]
            S_HTv = S_HT.rearrange("c p t -> p c t")
            blocks = []
            cnt = [0]

            def ldH(Gi, kq):
                em.dma("sp", f"ld_HT{kq}", HTq[kq][:, :, :], S_HTv[:, kq * 16:(kq + 1) * 16, Gi * 512:(Gi + 1) * 512], reads=[B_d["HT"]], writes=[BHTq[kq]])
            for kq in range(4):
                ldH(0, kq)
            for Gi in range(4):
                for cb in range(4):
                    for kq in range(4):
                        def load(slot, cb=cb, kq=kq):
                            G.wload(slot, 0, 512, w_ed[kq * 2048:(kq + 1) * 2048, cb * 512:(cb + 1) * 512])

                        def run(slot, Gi=Gi, cb=cb, kq=kq):
                            base = (cb % 2) * 4
                            if kq == 0:
                                for tl in range(4):
                                    em.dma("sp", f"ld_xslf{tl}", xsl[tl][:], S_h1[(Gi * 4 + tl) * 128:(Gi * 4 + tl + 1) * 128, cb * 512:(cb + 1) * 512],
                                           reads=[B_d["h1"]], writes=[Bxsl[tl]])
                            for tl in range(4):
                                G.mm_tm(slot, HTq[kq], BHTq[kq], tl, bk=base + tl, first=(kq == 0), last=(kq == 3))
                            if cb == 3 and Gi + 1 < 4:
                                ldH(Gi + 1, kq)
                            if kq == 3:
                                for tl in range(4):
                                    s = cnt[0] % 2
                                    cnt[0] += 1
                                    em.op("dve", lambda: nc.vector.tensor_tensor(out=hst[s][:], in0=G.pb[base + tl][:, :], in1=xsl[tl][:], op=ALU.add),
                                          reads=[G.Bp[base + tl], Bxsl[tl]], writes=[Bhst[s]])
                                    em.dma("sp", f"st_h{s}", S_h2[(Gi * 4 + tl) * 128:(Gi * 4 + tl + 1) * 128, cb * 512:(cb + 1) * 512], hst[s][:],
                                           reads=[Bhst[s]], writes=[B_d["h2"]])
                        blocks.append(dict(load=load, run=run))
            G.run(blocks)
            em.barrier()

        checkpoint("F")
        with ExitStack() as st:
            x3T = SB(st, "x3T", [128, 16, T], BF16)
            Bx3T = Buf("x3T")
            norm_T(st, lambda t: S_h2[t * 128:(t + 1) * 128, :], 16, g_ple, x3T, Bx3T, "g")
            pT = SB(st, "pT", [128, 2, T], BF16); BpT = Buf("pT")
            wp = SB(st, "wp", [128, 2, D], BF16); Bwp = Buf("wp")
            em.dma("pool", "ld_wp", wp[:, :, :], w_pp.rearrange("(k p) n -> p k n", p=128), writes=[Bwp])
            with ExitStack() as s1:
                pl = [SB(s1, f"pl{i}", [128, 256], F32) for i in range(2)]
                Bpl = [Buf("pl0"), Buf("pl1")]
                plb = [SB(s1, f"plb{i}", [128, 256], BF16) for i in range(2)]
                Bplb = [Buf("plb0"), Buf("plb1")]
                ptp = PS(s1, "ptp", [128, 8, 128], BF16); Bptp = Buf("ptp")
                for t in range(16):
                    s = t % 2
                    em.dma("sp", f"ld_pl{s}", pl[s][:], pp[t * 128:(t + 1) * 128, :], writes=[Bpl[s]])
                    em.op("dve", lambda: nc.vector.tensor_copy(out=plb[s][:], in_=pl[s][:]), reads=[Bpl[s]], writes=[Bplb[s]])

                    def f():
                        for j in range(2):
                            ins = nc.tensor.transpose(out=ptp[:, j, :], in_=plb[s][:, j * 128:(j + 1) * 128], identity=idb[:])
                        return ins
                    em.op("pe", f, reads=[Bplb[s], B_c], writes=[Bptp])
                    em.op("act", lambda: nc.scalar.copy(out=pT[:, :, t * 128:(t + 1) * 128], in_=ptp[:, 0:2, :]), reads=[Bptp], writes=[BpT])
                em.barrier()
            G = Gemm(st, "g")
            pP = [PS(st, f"pP{i}", [128, 512], F32) for i in range(2)]
            BpP = [Buf("pP0"), Buf("pP1")]
            xsl = [SB(st, f"xslg{i}", [128, 512], F32) for i in range(3)]
            Bxsl = [Buf(f"xslg{i}") for i in range(3)]
            sgg = [SB(st, f"sgg{i}", [128, 512], F32) for i in range(2)]
            Bsgg = [Buf("sgg0"), Buf("sgg1")]
            hst = [SB(st, f"hstg{i}", [128, 512], F32) for i in range(2)]
            Bhst = [Buf("hstg0"), Buf("hstg1")]
            blocks = []
            cnt = [0]
            for ob in range(4):
                def load(slot, ob=ob):
                    G.wload(slot, 0, 512, w_pg[:, ob * 512:(ob + 1) * 512])

                def run(slot, ob=ob):
                    def ldx(t):
                        em.dma("sp", f"ld_xsl{t % 3}", xsl[t % 3][:], S_h2[t * 128:(t + 1) * 128, ob * 512:(ob + 1) * 512], reads=[B_d["h2"]], writes=[Bxsl[t % 3]])
                    ldx(0)
                    ldx(1)
                    for t in range(16):
                        if t + 2 < 16:
                            ldx(t + 2)
                        pbk, Bp = G.mm_tm(slot, x3T, Bx3T, t)
                        s = cnt[0] % 2
                        cnt[0] += 1

                        def f():
                            for kc in range(2):
                                ins = nc.tensor.matmul(pP[s][:, :], lhsT=pT[:, kc, t * 128:(t + 1) * 128], rhs=wp[:, kc, ob * 512:(ob + 1) * 512],
                                                       start=(kc == 0), stop=(kc == 1))
                            return ins
                        em.op("pe", f, reads=[BpT, Bwp], writes=[BpP[s]])
                        em.op("act", lambda: nc.scalar.activation(out=sgg[s][:], in_=pbk[:, :], func=AF.Sigmoid), reads=[Bp], writes=[Bsgg[s]])
                        em.op("dve", lambda: nc.vector.tensor_tensor(out=hst[s][:], in0=pP[s][:, :], in1=sgg[s][:], op=ALU.mult),
                              reads=[BpP[s], Bsgg[s]], writes=[Bhst[s]])
                        em.op("dve", lambda: nc.vector.tensor_tensor(out=hst[s][:], in0=hst[s][:], in1=xsl[t % 3][:], op=ALU.add),
                              reads=[Bxsl[t % 3], Bhst[s]], writes=[Bhst[s]])
                        em.dma("sp", f"st_h{s}", S_h3[t * 128:(t + 1) * 128, ob * 512:(ob + 1) * 512], hst[s][:], reads=[Bhst[s]], writes=[B_d["h3"]])
                blocks.append(dict(load=load, run=run))
            G.run(blocks)
            em.barrier()

        checkpoint("G")
        with ExitStack() as st:
            src = lambda t: S_h3[t * 128:(t + 1) * 128, :]
            rstd, Bss = rms_stats(st, src, 16, "h")
            gB = SB(st, "gBh", [128, D], F32); BgB = Buf("gBh")
            em.dma("sp", "ld_g", gB[:], g_fin.partition_broadcast(128), writes=[BgB])
            xt = [SB(st, f"xth{i}", [128, D], F32) for i in range(2)]
            Bx = [Buf("xth0"), Buf("xth1")]
            yo = [SB(st, f"yo{i}", [128, D], F32) for i in range(2)]
            Byo = [Buf("yo0"), Buf("yo1")]
            for t in range(16):
                s = t % 2
                em.dma("sp", f"ld_xt{s}", xt[s][:], src(t), reads=[B_d["h3"]], writes=[Bx[s]])
                em.op("dve", lambda: nc.vector.scalar_tensor_tensor(out=yo[s][:], in0=xt[s][:], scalar=rstd[:, t:t + 1], in1=gB[:], op0=ALU.mult, op1=ALU.mult),
                      reads=[Bx[s], Bss, BgB], writes=[Byo[s]])
                em.dma("sp", f"st_o{s}", out_d[t * 128:(t + 1) * 128, :], yo[s][:], reads=[Byo[s]], writes=[B_d["out"]])
            em.barrier()
        es.close()
      except _Stop:
        em.barrier()
        try:
            es.close()
        except AssertionError:
            pass
    return nc


def _rel_bucket(dist):
    n = np.maximum(dist, 0)
    max_exact = 16
    nf = np.maximum(n, 1).astype(np.float32)
    large = max_exact + (np.log(nf / np.float32(max_exact)) / np.float32(math.log(128 / max_exact)) * np.float32(32 - max_exact)).astype(np.int32)
    large = np.minimum(large, 31)
    return np.where(n < max_exact, n, large)


def make_in_maps(inp):
    f = lambda k: np.ascontiguousarray(np.asarray(inp[k], dtype=np.float32))
    x = f("x"); p = f("p")[0]
    rel_bias = f("rel_bias")
    kk = np.arange(256)[:, None]
    qq = np.arange(256)[None, :]
    bs = rel_bias[_rel_bucket(qq - kk)]
    bs = np.where((qq >= kk)[:, :, None], bs, np.float32(NEG)).astype(np.float32)
    ba = rel_bias[_rel_bucket(qq + 256 - kk)].astype(np.float32)

    def lay(b):
        b = b.transpose(2, 0, 1).reshape(16, 2, 128, 256).transpose(0, 2, 1, 3).reshape(16, 128, 512)
        return np.ascontiguousarray(b)
    bias_self = lay(bs); bias_adj = lay(ba)
    t31 = np.ascontiguousarray(np.broadcast_to(rel_bias[31][None, :], (128, 16))).astype(np.float32)
    colvec = lambda v: np.ascontiguousarray(v.reshape(16, 128).T)
    shared = {
        "w_in": f("w_in")[0], "w_attn_br": f("w_attn_br")[0], "w_conv_br": f("w_conv_br")[0], "w_o": f("w_o")[0],
        "w_ple_gate": f("w_ple_gate")[0], "w_ple_proj": f("w_ple_proj")[0],
        "w_e_gate": f("w_e_gate")[0], "w_e_up": f("w_e_up")[0], "w_e_down": f("w_e_down")[0].reshape(16 * 512, D),
        "w_r": np.ascontiguousarray(np.concatenate([f("w_router_g")[0], f("w_router_e")[0]], axis=1)),
        "b_r": np.ascontiguousarray(np.concatenate([f("b_router_g")[0], f("b_router_e")[0]])[None, :]),
        "g_mix": f("g_mix"), "g_ffn": f("g_ffn"), "g_ple": f("g_ple"), "g_final": f("g_final")[None, :],
        "conv_wT": np.ascontiguousarray(f("conv_w")[0].T.reshape(16, 128, 31).transpose(1, 0, 2)),
        "conv_b": colvec(f("conv_b")[0]), "ln_g": colvec(f("ln_g")[0]), "ln_b": colvec(f("ln_b")[0]),
        "ident": np.eye(128, dtype=np.float32),
        "sel16": np.ascontiguousarray(np.repeat(np.eye(16, dtype=np.float32), 128, axis=1)),
        "bias_self": bias_self, "bias_adj": bias_adj, "t31": t31,
    }
    maps = []
    for c in range(8):
        b, hf = c // 2, c % 2
        own = x[b, hf * T:(hf + 1) * T]
        oth = x[b, (1 - hf) * T:(2 - hf) * T]
        halo = np.zeros((128, D), np.float32)
        if hf == 1:
            halo[96:128] = x[b, T - 32:T]
        xa = np.concatenate([own, oth, halo], axis=0)
        gbl = np.concatenate([np.arange(8) + 8 * hf, np.arange(8) + 8 * (1 - hf)])
        past = (gbl[None, :] < (np.arange(8) + 8 * hf)[:, None])
        past_q = np.repeat(past, 2, axis=0)
        pastm = np.broadcast_to(past_q.astype(np.float32).reshape(1, 256), (128, 256))
        pastb = np.where(pastm > 0, np.float32(0), np.float32(NEG)).astype(np.float32)
        m = dict(shared)
        m.update({"xa": np.ascontiguousarray(xa), "pp": np.ascontiguousarray(p[b, hf * T:(hf + 1) * T]),
                  "pastm": np.ascontiguousarray(pastm), "pastb": np.ascontiguousarray(pastb)})
        maps.append(m)
    return maps


_NC = {}


def kernel(**inputs):
    if "nc" not in _NC:
        _NC["nc"] = build(False)
    maps = make_in_maps(inputs)
    res = run_bass_kernel_spmd(_NC["nc"], maps, core_ids=list(range(8)))
    out = np.empty((4, 4096, D), np.float32)
    for c in range(8):
        b, hf = c // 2, c % 2
        out[b, hf * T:(hf + 1) * T] = np.asarray(res.results[c]["out"], dtype=np.float32)
    return out
```

```python
import math
from contextlib import ExitStack
import numpy as np
import concourse.bass as bass
import concourse.mybir as mybir
from concourse.bass_utils import run_bass_kernel_spmd

F32 = mybir.dt.float32
BF16 = mybir.dt.bfloat16
AF = mybir.ActivationFunctionType
ALU = mybir.AluOpType
AX = mybir.AxisListType

D = 2048
T = 2048
NEG = -1e30
EPS = 1e-6
SCALE = 128 ** -0.5


class Buf:
    __slots__ = ("name", "w", "r")

    def __init__(self, name):
        self.name = name
        self.w = None
        self.r = {}


class Eng:
    def __init__(self, name, handle, sem):
        self.name = name
        self.h = handle
        self.sem = sem
        self.count = 0
        self.waited = {}


class Emitter:
    def __init__(self, nc, es):
        self.nc = nc
        self.es = es
        self.sems = {}
        self.engs = {}
        for name, h in (("pe", nc.tensor), ("dve", nc.vector), ("act", nc.scalar),
                        ("pool", nc.gpsimd), ("sp", nc.sync)):
            self.sems["sem_" + name] = es.enter_context(nc.semaphore("sem_" + name))
            self.engs[name] = Eng(name, h, "sem_" + name)
        self.dma_cnt = {}
        self.keymap = {}
        self.pool_keys = []

    def _wait(self, e, deps, skip_self=False):
        for key, val in deps:
            if skip_self and key == e.sem:
                continue
            if e.waited.get(key, 0) < val:
                e.h.wait_ge(self.sems[key], val)
                e.waited[key] = val

    @staticmethod
    def _deps(reads, writes):
        deps = {}
        for b in reads:
            if b.w is not None:
                k, v = b.w
                if deps.get(k, 0) < v:
                    deps[k] = v
        for b in writes:
            if b.w is not None:
                k, v = b.w
                if deps.get(k, 0) < v:
                    deps[k] = v
            for k, v in b.r.items():
                if deps.get(k, 0) < v:
                    deps[k] = v
        return list(deps.items())

    @staticmethod
    def _mark(ev, reads, writes):
        k, v = ev
        for b in reads:
            if b.r.get(k, 0) < v:
                b.r[k] = v
        for b in writes:
            b.w = ev
            b.r = {}

    def op(self, eng, fn, reads=(), writes=()):
        e = self.engs[eng]
        self._wait(e, self._deps(reads, writes), skip_self=(eng == "pe"))
        ins = fn()
        e.count += 1
        ins.then_inc(self.sems[e.sem], 1)
        self._mark((e.sem, e.count), reads, writes)
        return ins

    def dma(self, q, semkey, out, in_, reads=(), writes=()):
        e = self.engs[q]
        if semkey not in self.keymap:
            idx = len(self.keymap)
            if idx >= len(self.pool_keys):
                k = f"dq{idx}"
                self.sems[k] = self.es.enter_context(self.nc.semaphore(k))
                self.dma_cnt[k] = 0
                self.pool_keys.append(k)
            self.keymap[semkey] = self.pool_keys[idx]
        semkey = self.keymap[semkey]
        self._wait(e, self._deps(reads, writes))
        ins = e.h.dma_start(out=out, in_=in_)
        self.dma_cnt[semkey] += 16
        ins.then_inc(self.sems[semkey], 16)
        self._mark((semkey, self.dma_cnt[semkey]), reads, writes)
        return ins

    def barrier(self):
        evs = [(e.sem, e.count) for e in self.engs.values() if e.count > 0]
        evs += [(k, v) for k, v in self.dma_cnt.items() if v > 0]
        for e in self.engs.values():
            self._wait(e, evs)
        self.keymap = {}


class _Stop(Exception):
    pass


def build(debug=False, stop=None):
    nc = bass.Bass("TRN2", target_bir_lowering=False)

    def checkpoint(name):
        if stop == name:
            raise _Stop()

    def din(name, shape, dt=F32):
        return nc.dram_tensor(name, shape, dt, kind="ExternalInput").ap()

    def dscr(name, shape, dt):
        return nc.dram_tensor(name, shape, dt, kind=("ExternalOutput" if (debug and name in debug) else "Internal")).ap()

    xa = din("xa", [4096 + 128, D])
    pp = din("pp", [T, 256])
    w_in = din("w_in", [D, 14336])
    w_attn = din("w_attn_br", [D, D])
    w_conv = din("w_conv_br", [D, D])
    w_o = din("w_o", [D, D])
    w_pg = din("w_ple_gate", [D, D])
    w_pp = din("w_ple_proj", [256, D])
    w_eg = din("w_e_gate", [16, D, 512])
    w_eu = din("w_e_up", [16, D, 512])
    w_ed = din("w_e_down", [16 * 512, D])
    w_r = din("w_r", [D, 20])
    b_r = din("b_r", [1, 20])
    g_mix = din("g_mix", [1, D]); g_ffn = din("g_ffn", [1, D]); g_ple = din("g_ple", [1, D]); g_fin = din("g_final", [1, D])
    conv_wT = din("conv_wT", [128, 16, 31])
    conv_b = din("conv_b", [128, 16]); ln_g = din("ln_g", [128, 16]); ln_b = din("ln_b", [128, 16])
    ident_d = din("ident", [128, 128])
    sel16_d = din("sel16", [16, D])
    bias_self = din("bias_self", [16, 128, 512])
    bias_adj = din("bias_adj", [16, 128, 512])
    t31_d = din("t31", [128, 16])
    pastb_d = din("pastb", [128, 256])
    pastm_d = din("pastm", [128, 256])
    out_d = nc.dram_tensor("out", [T, D], F32, kind="ExternalOutput").ap()

    S_qT = dscr("S_qT", [16, 128, T], BF16)
    S_kT = dscr("S_kT", [16, 128, 4096], BF16)
    S_V = dscr("S_V", [4096, D], BF16)
    S_cT = dscr("S_cT", [16, 128, T], F32)
    S_gaT = dscr("S_gaT", [16, 128, T], F32)
    S_gcT = dscr("S_gcT", [16, 128, T], F32)
    S_z1T = dscr("S_z1T", [16, 128, T], F32)
    S_zT = dscr("S_zT", [16, 128, T], BF16)
    S_h1 = dscr("S_h1", [T, D], F32)
    S_h2 = dscr("S_h2", [T, D], F32)
    S_h3 = dscr("S_h3", [T, D], F32)
    S_HT = dscr("S_HT", [64, 128, T], BF16)
    S_attnT = dscr("S_attnT", [16, 128, T], BF16)
    S_mu = dscr("S_mu", [128, T], F32)
    S_rs = dscr("S_rs", [128, T], F32)
    S_convT = dscr("S_convT", [16, 128, T], BF16)
    S_comb = dscr("S_comb", [128, 16, 16], F32)
    B_d = {k: Buf(k) for k in "qT kT V cT gaT gcT z1T zT h1 h2 h3 HT out attnT mu".split()}

    es = ExitStack()
    if True:
      em = Emitter(nc, es)
      try:

        uid = [0]

        def SB(st, name, shape, dt):
            uid[0] += 1
            return st.enter_context(nc.sbuf_tensor(f"s{uid[0]}_{name}", shape, dt))

        def PS(st, name, shape, dt):
            uid[0] += 1
            return st.enter_context(nc.psum_tensor(f"p{uid[0]}_{name}", shape, dt))

        idf = SB(es, "idf", [128, 128], F32)
        idb = SB(es, "idb", [128, 128], BF16)
        onesf = SB(es, "onesf", [128, 128], F32)
        B_c = Buf("consts")
        em.dma("sp", "ld_c0", idf[:], ident_d[:, :], writes=[B_c])
        em.op("dve", lambda: nc.vector.tensor_copy(out=idb[:], in_=idf[:]), reads=[B_c], writes=[B_c])
        em.op("dve", lambda: nc.vector.memset(onesf[:], 1.0), writes=[B_c])

        def rms_stats(st, src_tile, ntiles, tag):
            xt = [SB(st, f"xs{tag}{i}", [128, D], F32) for i in range(2)]
            Bx = [Buf("xs0"), Buf("xs1")]
            junk = SB(st, f"junk{tag}", [128, D], BF16)
            Bj = Buf("junk")
            ss = SB(st, f"ss{tag}", [128, ntiles], F32)
            rstd = SB(st, f"rstd{tag}", [128, ntiles], F32)
            Bss = Buf("ss")
            em.op("dve", lambda: nc.vector.memset(ss[:], 0.0), writes=[Bss])
            for t in range(ntiles):
                s = t % 2
                em.dma("sp", f"ld_xs{s}", xt[s][:], src_tile(t), writes=[Bx[s]])
                em.op("act", lambda: nc.scalar.activation(out=junk[:], in_=xt[s][:], func=AF.Square, accum_out=ss[:, t:t + 1]),
                      reads=[Bx[s]], writes=[Bj, Bss])
            em.op("dve", lambda: nc.vector.tensor_scalar(out=rstd[:], in0=ss[:], scalar1=1.0 / D, scalar2=EPS, op0=ALU.mult, op1=ALU.add),
                  reads=[Bss], writes=[Bss])
            em.op("act", lambda: nc.scalar.activation(out=rstd[:], in_=rstd[:], func=AF.Sqrt), reads=[Bss], writes=[Bss])
            em.op("dve", lambda: nc.vector.reciprocal(out=rstd[:], in_=rstd[:]), reads=[Bss], writes=[Bss])
            return rstd, Bss

        def norm_T(st, src_tile, ntiles, g_ap, actT, Bact, tag):
            with ExitStack() as s1:
                rstd, Bss = rms_stats(s1, src_tile, ntiles, tag)
                gB = SB(s1, f"gB{tag}", [128, D], F32)
                BgB = Buf("gB")
                em.dma("sp", "ld_g", gB[:], g_ap.partition_broadcast(128), writes=[BgB])
                xt = [SB(s1, f"xt{tag}{i}", [128, D], F32) for i in range(2)]
                Bx = [Buf("xt0"), Buf("xt1")]
                xn = [SB(s1, f"xn{tag}{i}", [128, D], BF16) for i in range(2)]
                Bxn = [Buf("xn0"), Buf("xn1")]
                pt = [PS(s1, f"pt{tag}{i}", [128, 8, 128], BF16) for i in range(2)]
                Bpt = [Buf("pt0"), Buf("pt1")]
                for t in range(ntiles):
                    s = t % 2
                    em.dma("sp", f"ld_xt{s}", xt[s][:], src_tile(t), writes=[Bx[s]])
                    em.op("dve", lambda: nc.vector.scalar_tensor_tensor(out=xn[s][:], in0=xt[s][:], scalar=rstd[:, t:t + 1], in1=gB[:],
                                                                        op0=ALU.mult, op1=ALU.mult),
                          reads=[Bx[s], Bss, BgB], writes=[Bxn[s]])
                    for half in range(2):
                        def f():
                            for j in range(8):
                                c = half * 8 + j
                                ins = nc.tensor.transpose(out=pt[half][:, j, :], in_=xn[s][:, c * 128:(c + 1) * 128], identity=idb[:])
                            return ins
                        em.op("pe", f, reads=[Bxn[s], B_c], writes=[Bpt[half]])
                        dst = actT[:, half * 8:(half + 1) * 8, t * 128:(t + 1) * 128]
                        if half == 0:
                            em.op("act", lambda: nc.scalar.copy(out=dst, in_=pt[half][:, :, :]), reads=[Bpt[half]], writes=[Bact])
                        else:
                            em.op("dve", lambda: nc.vector.tensor_copy(out=dst, in_=pt[half][:, :, :]), reads=[Bpt[half]], writes=[Bact])
                em.barrier()

        class Gemm:
            def __init__(self, st, tag, nbanks=4, nw=3):
                self.wb = [SB(st, f"wb{tag}{i}", [128, 16, 512], BF16) for i in range(nw)]
                self.Bw = [[Buf(f"wb{i}a"), Buf(f"wb{i}b")] for i in range(nw)]
                self.pb = [PS(st, f"pb{tag}{i}", [128, 512], F32) for i in range(nbanks)]
                self.Bp = [Buf(f"pb{i}") for i in range(nbanks)]
                self.nw = nw
                self.bank = 0

            def next_bank(self):
                b = self.bank
                self.bank = (self.bank + 1) % len(self.pb)
                return b

            def wload(self, slot, c0, c1, src):
                part = 0 if c0 == 0 else 1
                em.dma("pool", f"ld_w{slot}_{part}", self.wb[slot][:, :, c0:c1], src.rearrange("(k p) n -> p k n", p=128), writes=[self.Bw[slot][part]])

            def run(self, blocks):
                n = len(blocks)
                for b in range(min(self.nw - 1, n)):
                    blocks[b]["load"](b % self.nw)
                for b in range(n):
                    if b + self.nw - 1 < n:
                        blocks[b + self.nw - 1]["load"]((b + self.nw - 1) % self.nw)
                    blocks[b]["run"](b % self.nw)

            def mm_fm(self, slot, c0, actT, Bact, t0, tn, nk=16):
                bk = self.next_bank()
                pbk = self.pb[bk]
                wbs = self.wb[slot]

                def f():
                    for kc in range(nk):
                        ins = nc.tensor.matmul(pbk[:, 0:tn], lhsT=wbs[:, kc, c0:c0 + 128], rhs=actT[:, kc, t0:t0 + tn],
                                               start=(kc == 0), stop=(kc == nk - 1))
                    return ins
                em.op("pe", f, reads=self.Bw[slot] + [Bact], writes=[self.Bp[bk]])
                return pbk, self.Bp[bk]

            def mm_tm(self, slot, actT, Bact, t, ncols=512, bk=None, first=True, last=True, nk=16, kofs=0):
                if bk is None:
                    bk = self.next_bank()
                pbk = self.pb[bk]
                wbs = self.wb[slot]

                def f():
                    for kc in range(nk):
                        ins = nc.tensor.matmul(pbk[:, 0:ncols], lhsT=actT[:, kofs + kc, t * 128:(t + 1) * 128], rhs=wbs[:, kc, 0:ncols],
                                               start=(first and kc == 0), stop=(last and kc == nk - 1))
                    return ins
                em.op("pe", f, reads=self.Bw[slot] + [Bact], writes=[self.Bp[bk]])
                return pbk, self.Bp[bk]

        cpy_ctr = [0]
        act_only = [False]

        def evac_copy(out, in_, reads, writes):
            cpy_ctr[0] += 1
            if act_only[0] or cpy_ctr[0] % 2:
                em.op("act", lambda: nc.scalar.copy(out=out, in_=in_), reads=reads, writes=writes)
            else:
                em.op("dve", lambda: nc.vector.tensor_copy(out=out, in_=in_), reads=reads, writes=writes)

        TG4 = [(i * 512, 512) for i in range(4)]

        def kv_blocks(G, st, actT, Bact, tok_off, tgroups, tag):
            kst = [SB(st, f"kst{tag}{i}", [128, 512], BF16) for i in range(2)]
            Bkst = [Buf("kst0"), Buf("kst1")]
            vst = [SB(st, f"vst{tag}{i}", [128, 512], BF16) for i in range(2)]
            Bvst = [Buf("vst0"), Buf("vst1")]
            blocks = []
            cnt = [0, 0]
            for kb in range(4):
                def load(slot, kb=kb):
                    G.wload(slot, 0, 512, w_in[:, 2048 + kb * 512: 2048 + (kb + 1) * 512])

                def run(slot, kb=kb):
                    for sub in range(4):
                        h = kb * 4 + sub
                        for (t0, tn) in tgroups:
                            s = cnt[0] % 2
                            cnt[0] += 1
                            pbk, Bp = G.mm_fm(slot, sub * 128, actT, Bact, t0, tn)
                            evac_copy(kst[s][:, 0:tn], pbk[:, 0:tn], [Bp], [Bkst[s]])
                            em.dma("sp", f"st_k{s}", S_kT[h, :, tok_off + t0:tok_off + t0 + tn], kst[s][:, 0:tn], reads=[Bkst[s]], writes=[B_d["kT"]])
                blocks.append(dict(load=load, run=run))
            for vb in range(4):
                def load(slot, vb=vb):
                    G.wload(slot, 0, 512, w_in[:, 4096 + vb * 512: 4096 + (vb + 1) * 512])

                def run(slot, vb=vb):
                    for t in range(16):
                        s = cnt[1] % 2
                        cnt[1] += 1
                        pbk, Bp = G.mm_tm(slot, actT, Bact, t)
                        evac_copy(vst[s][:], pbk[:, :], [Bp], [Bvst[s]])
                        em.dma("sp", f"st_v{s}", S_V[tok_off + t * 128: tok_off + (t + 1) * 128, vb * 512:(vb + 1) * 512], vst[s][:],
                               reads=[Bvst[s]], writes=[B_d["V"]])
                blocks.append(dict(load=load, run=run))
            return blocks

        with ExitStack() as st:
            actT = SB(st, "actT0", [128, 16, T], BF16)
            Bact = Buf("actT0")
            norm_T(st, lambda t: xa[2048 + t * 128: 2048 + (t + 1) * 128, :], 16, g_mix, actT, Bact, "a")
            G = Gemm(st, "a")
            G.run(kv_blocks(G, st, actT, Bact, 2048, TG4, "a"))
            em.barrier()

        checkpoint("B0")
        with ExitStack() as st:
            TA = T + 128
            actT = SB(st, "actT1", [128, 16, TA], BF16)
            Bact = Buf("actT1")

            def src1(t):
                if t < 16:
                    return xa[t * 128:(t + 1) * 128, :]
                return xa[4096:4096 + 128, :]
            norm_T(st, src1, 17, g_mix, actT, Bact, "b")
            act_only[0] = True
            G = Gemm(st, "b")
            cw = SB(st, "cw", [128, 16, 31], F32); cb = SB(st, "cb", [128, 16], F32)
            Bcw = Buf("cw")
            em.dma("sp", "ld_cw", cw[:], conv_wT[:, :, :], writes=[Bcw])
            em.dma("sp", "ld_cw", cb[:], conv_b[:, :], writes=[Bcw])
            csum = SB(st, "csum", [128, T], F32); csq = SB(st, "csq", [128, T], F32)
            Bcs = Buf("csum"); Bcq = Buf("csq")
            em.op("pool", lambda: nc.gpsimd.memset(csum[:], 0.0), writes=[Bcs])
            em.op("pool", lambda: nc.gpsimd.memset(csq[:], 0.0), writes=[Bcq])

            blocks_other = []
            qst = [SB(st, f"qst{i}", [128, 512], BF16) for i in range(2)]
            Bqst = [Buf("qst0"), Buf("qst1")]
            qcnt = [0]
            for qb in range(4):
                def load(slot, qb=qb):
                    G.wload(slot, 0, 512, w_in[:, qb * 512:(qb + 1) * 512])

                def run(slot, qb=qb):
                    for sub in range(4):
                        h = qb * 4 + sub
                        for (t0, tn) in TG4:
                            s = qcnt[0] % 2
                            qcnt[0] += 1
                            pbk, Bp = G.mm_fm(slot, sub * 128, actT, Bact, t0, tn)
                            evac_copy(qst[s][:, 0:tn], pbk[:, 0:tn], [Bp], [Bqst[s]])
                            em.dma("sp", f"st_q{s}", S_qT[h, :, t0:t0 + tn], qst[s][:, 0:tn], reads=[Bqst[s]], writes=[B_d["qT"]])
                blocks_other.append(dict(load=load, run=run))
            blocks_other += kv_blocks(G, st, actT, Bact, 0, TG4, "b")
            gst = [SB(st, f"gst{i}", [128, 512], F32) for i in range(2)]
            Bgst = [Buf("gst0"), Buf("gst1")]
            gcnt = [0]
            for gb in range(8):
                def load(slot, gb=gb):
                    G.wload(slot, 0, 512, w_in[:, 10240 + gb * 512: 10240 + (gb + 1) * 512])

                def run(slot, gb=gb):
                    for sub in range(4):
                        ch = (gb % 4) * 4 + sub
                        dst = S_gaT if gb < 4 else S_gcT
                        for (t0, tn) in TG4:
                            s = gcnt[0] % 2
                            gcnt[0] += 1
                            pbk, Bp = G.mm_fm(slot, sub * 128, actT, Bact, t0, tn)
                            em.op("act", lambda: nc.scalar.activation(out=gst[s][:, 0:tn], in_=pbk[:, 0:tn], func=AF.Sigmoid),
                                  reads=[Bp], writes=[Bgst[s]])
                            em.dma("sp", f"st_g{s}", dst[ch, :, t0:t0 + tn], gst[s][:, 0:tn], reads=[Bgst[s]], writes=[B_d["gaT" if gb < 4 else "gcT"]])
                blocks_other.append(dict(load=load, run=run))
            A_sb = SB(st, "A_sb", [128, TA], F32); BA = Buf("A")
            Us = [SB(st, f"U{i}", [128, 32 + T], F32) for i in range(2)]; BUs = [Buf("U0"), Buf("U1")]
            U = Us[0]; BU = BUs[0]
            acc = [SB(st, f"cacc{i}", [128, T], F32) for i in range(2)]
            Bacc = [Buf("cacc0"), Buf("cacc1")]
            sqb = SB(st, "sqb", [128, T], F32); Bsq = Buf("sqb")
            TG5 = TG4 + [(T, 128)]
            pending_stats = []

            def flush_stats():
                while pending_stats:
                    pending_stats.pop(0)()
            blocks_conv = []
            for cc in range(16):
                def load(slot, cc=cc):
                    G.wload(slot, 0, 128, w_in[:, 6144 + cc * 128: 6144 + (cc + 1) * 128])
                    G.wload(slot, 128, 256, w_in[:, 8192 + cc * 128: 8192 + (cc + 1) * 128])

                def run(slot, cc=cc):
                    flush_stats()
                    U = Us[cc % 2]
                    BU = BUs[cc % 2]
                    for (t0, tn) in TG5:
                        pbk, Bp = G.mm_fm(slot, 0, actT, Bact, t0, tn)
                        em.op("act", lambda: nc.scalar.copy(out=A_sb[:, t0:t0 + tn], in_=pbk[:, 0:tn]), reads=[Bp], writes=[BA])
                    for (t0, tn) in TG5:
                        pbk, Bp = G.mm_fm(slot, 128, actT, Bact, t0, tn)
                        if t0 < T:
                            em.op("act", lambda: nc.scalar.activation(out=U[:, 32 + t0:32 + t0 + tn], in_=pbk[:, 0:tn], func=AF.Sigmoid),
                                  reads=[Bp], writes=[BU])
                        else:
                            em.op("act", lambda: nc.scalar.activation(out=U[:, 0:32], in_=pbk[:, 96:128], func=AF.Sigmoid),
                                  reads=[Bp], writes=[BU])
                    em.op("dve", lambda: nc.vector.tensor_tensor(out=U[:, 0:32], in0=U[:, 0:32], in1=A_sb[:, T + 96:T + 128], op=ALU.mult),
                          reads=[BA, BU], writes=[BU])
                    em.op("dve", lambda: nc.vector.tensor_tensor(out=U[:, 32:32 + T], in0=U[:, 32:32 + T], in1=A_sb[:, 0:T], op=ALU.mult),
                          reads=[BA, BU], writes=[BU])
                    a = acc[cc % 2]
                    Ba = Bacc[cc % 2]
                    em.op("dve", lambda: nc.vector.tensor_scalar(out=a[:], in0=U[:, 2:2 + T], scalar1=cw[:, cc, 0:1], scalar2=cb[:, cc:cc + 1],
                                                                 op0=ALU.mult, op1=ALU.add), reads=[BU, Bcw], writes=[Ba])
                    for j in range(1, 31):
                        em.op("dve", lambda: nc.vector.scalar_tensor_tensor(out=a[:], in0=U[:, 2 + j:2 + j + T], scalar=cw[:, cc, j:j + 1], in1=a[:],
                                                                            op0=ALU.mult, op1=ALU.add), reads=[BU, Bcw, Ba], writes=[Ba])

                    def stats(a=a, Ba=Ba, cc=cc):
                        em.dma("sp", f"st_c{cc % 2}", S_cT[cc, :, :], a[:], reads=[Ba], writes=[B_d["cT"]])
                        em.op("pool", lambda: nc.gpsimd.tensor_tensor(out=sqb[:], in0=a[:], in1=a[:], op=ALU.mult), reads=[Ba], writes=[Bsq])
                        em.op("pool", lambda: nc.gpsimd.tensor_tensor(out=csum[:], in0=csum[:], in1=a[:], op=ALU.add), reads=[Ba, Bcs], writes=[Bcs])
                        em.op("pool", lambda: nc.gpsimd.tensor_tensor(out=csq[:], in0=csq[:], in1=sqb[:], op=ALU.add), reads=[Bsq, Bcq], writes=[Bcq])
                    pending_stats.append(stats)
                blocks_conv.append(dict(load=load, run=run))
            order = []
            for n in range(len(blocks_other)):
                order.append(blocks_other[n])
                if n < 16:
                    order.append(blocks_conv[n])
            G.run(order)
            flush_stats()
            act_only[0] = False
            mu = A_sb[:, 0:T]; rs = U[:, 0:T]
            Bmu = BA; Brs = BU
            for (t0, tn) in TG4:
                bk = G.next_bank()
                em.op("pe", lambda: nc.tensor.matmul(G.pb[bk][:, :], lhsT=onesf[:], rhs=csum[:, t0:t0 + tn], start=True, stop=True),
                      reads=[Bcs, B_c], writes=[G.Bp[bk]])
                em.op("dve", lambda: nc.vector.tensor_scalar(out=mu[:, t0:t0 + tn], in0=G.pb[bk][:, :], scalar1=1.0 / D, scalar2=None, op0=ALU.mult),
                      reads=[G.Bp[bk]], writes=[Bmu])
                bk = G.next_bank()
                em.op("pe", lambda: nc.tensor.matmul(G.pb[bk][:, :], lhsT=onesf[:], rhs=csq[:, t0:t0 + tn], start=True, stop=True),
                      reads=[Bcq, B_c], writes=[G.Bp[bk]])
                em.op("dve", lambda: nc.vector.tensor_scalar(out=rs[:, t0:t0 + tn], in0=G.pb[bk][:, :], scalar1=1.0 / D, scalar2=EPS, op0=ALU.mult, op1=ALU.add),
                      reads=[G.Bp[bk]], writes=[Brs])
            em.op("dve", lambda: nc.vector.tensor_tensor(out=sqb[:], in0=mu, in1=mu, op=ALU.mult), reads=[Bmu], writes=[Bsq])
            em.op("dve", lambda: nc.vector.tensor_tensor(out=rs, in0=rs, in1=sqb[:], op=ALU.subtract), reads=[Brs, Bsq], writes=[Brs])
            em.op("act", lambda: nc.scalar.activation(out=rs, in_=rs, func=AF.Sqrt), reads=[Brs], writes=[Brs])
            em.op("dve", lambda: nc.vector.reciprocal(out=rs, in_=rs), reads=[Brs], writes=[Brs])
            em.dma("sp", "st_mu", S_mu[:, :], mu, reads=[Bmu], writes=[B_d["mu"]])
            em.dma("sp", "st_mu", S_rs[:, :], rs, reads=[Brs], writes=[B_d["mu"]])
            em.barrier()

        checkpoint("B1")
        with ExitStack() as st:
            attnT = SB(st, "attnT", [128, 16, T], BF16)
            BattnT = Buf("attnT")
            qs = [SB(st, f"qs{i}", [128, 8, 256], BF16) for i in range(2)]
            ks = [SB(st, f"ks{i}", [128, 16, 256], BF16) for i in range(2)]
            vs = [SB(st, f"vs{i}", [128, 32, 136], BF16) for i in range(2)]
            bsf = [SB(st, f"bsf{i}", [128, 512], F32) for i in range(2)]
            baj = [SB(st, f"baj{i}", [128, 512], F32) for i in range(2)]
            Bhq = [Buf("hq0"), Buf("hq1")]; Bhk = [Buf("hk0"), Buf("hk1")]; Bhv = [Buf("hv0"), Buf("hv1")]; Bhb = [Buf("hb0"), Buf("hb1")]
            t31 = SB(st, "t31", [128, 16], F32)
            pastb = SB(st, "pastb", [128, 16, 16], F32)
            pastm = SB(st, "pastm", [128, 16, 16], F32)
            Bmk = Buf("masks")
            em.dma("sp", "ld_mk", t31[:], t31_d[:, :], writes=[Bmk])
            em.dma("sp", "ld_mk", pastb[:, :, :], pastb_d.rearrange("p (a b) -> p a b", b=16), writes=[Bmk])
            em.dma("sp", "ld_mk", pastm[:, :, :], pastm_d.rearrange("p (a b) -> p a b", b=16), writes=[Bmk])
            for i in range(2):
                em.op("dve", lambda: nc.vector.memset(vs[i][:, :, 128:136], 0.0), writes=[Bhv[i]])
                em.op("dve", lambda: nc.vector.memset(vs[i][:, :, 128:129], 1.0), writes=[Bhv[i]])
            km = SB(st, "km", [128, 16], F32); kmb = SB(st, "kmb", [128, 16], BF16); Bkm = Buf("km")
            gate = SB(st, "gate", [128, 16, 16], F32); Bgate = Buf("gate")
            m8 = SB(st, "m8", [128, 16, 8], F32); Bm8 = Buf("m8")
            selm = [SB(st, f"selm{i}", [128, 16, 16], F32) for i in range(2)]
            Bsel = [Buf("sel0"), Buf("sel1")]
            tmpb = [SB(st, f"tmpb{i}", [128, 512], F32) for i in range(2)]
            Btmp = [Buf("tmpb0"), Buf("tmpb1")]
            PT = [SB(st, f"PT{i}", [128, 512], BF16) for i in range(3)]
            BPT = [Buf(f"PT{i}") for i in range(3)]
            oacc = [SB(st, f"oacc{i}", [128, 2, 129], F32) for i in range(2)]
            Boacc = [Buf("oacc0"), Buf("oacc1")]
            rden = SB(st, "rden", [128, 2], F32); Brden = Buf("rden")
            obf = SB(st, "obf", [128, 2, 128], BF16); Bobf = Buf("obf")
            pS = [PS(st, f"pS{i}", [128, 512], F32) for i in range(3)]
            BpS = [Buf(f"pS{i}") for i in range(3)]
            pO = [PS(st, f"pO{i}", [128, 2, 256], F32) for i in range(3)]
            BpO = [Buf(f"pO{i}") for i in range(3)]
            pG = PS(st, "pG", [128, 32, 16], F32); BpG = Buf("pG")
            pTr = PS(st, "pTr", [128, 1024], BF16); BpTr = Buf("pTr")

            def head_load(h):
                s = h % 2
                em.dma("sp", f"ld_hq{s}", qs[s][:, :, :], S_qT[h].rearrange("p (j t) -> p j t", t=256), reads=[B_d["qT"]], writes=[Bhq[s]])
                em.dma("sp", f"ld_hk{s}", ks[s][:, :, :], S_kT[h].rearrange("p (j t) -> p j t", t=256), reads=[B_d["kT"]], writes=[Bhk[s]])
                em.dma("sp", f"ld_hv{s}", vs[s][:, :, 0:128], S_V[:, h * 128:(h + 1) * 128].rearrange("(kt p) d -> p kt d", p=128),
                       reads=[B_d["V"]], writes=[Bhv[s]])
                em.dma("sp", f"ld_hb{s}", bsf[s][:], bias_self[h], writes=[Bhb[s]])
                em.dma("sp", f"ld_hc{s}", baj[s][:], bias_adj[h], writes=[Bhb[s]])

            def head_prologue(h):
                s = h % 2
                em.op("dve", lambda: nc.vector.tensor_reduce(out=km[:], in_=ks[s][:, :, :], axis=AX.X, op=ALU.add), reads=[Bhk[s]], writes=[Bkm])
                em.op("dve", lambda: nc.vector.tensor_scalar(out=kmb[:], in0=km[:], scalar1=1.0 / 256, scalar2=None, op0=ALU.mult), reads=[Bkm], writes=[Bkm])

                def f():
                    for qt in range(16):
                        ins = nc.tensor.matmul(pG[:, qt, :], lhsT=qs[s][:, qt // 2, (qt % 2) * 128:(qt % 2 + 1) * 128], rhs=kmb[:, :], start=True, stop=True)
                    return ins
                em.op("pe", f, reads=[Bhq[s], Bkm], writes=[BpG])
                em.op("dve", lambda: nc.vector.tensor_tensor(out=gate[:, :, :], in0=pG[:, 0:16, :], in1=pastb[:, :, :], op=ALU.add),
                      reads=[BpG, Bmk], writes=[Bgate])
                for qt in range(16):
                    em.op("dve", lambda: nc.vector.max(out=m8[:, qt, :], in_=gate[:, qt, :]), reads=[Bgate], writes=[Bm8])
                sm = selm[s]
                for qt in range(16):
                    em.op("dve", lambda: nc.vector.tensor_scalar(out=sm[:, qt, :], in0=gate[:, qt, :], scalar1=m8[:, qt, 2:3], scalar2=None, op0=ALU.is_ge),
                          reads=[Bgate, Bm8], writes=[Bsel[s]])
                em.op("dve", lambda: nc.vector.tensor_tensor(out=sm[:, :, :], in0=sm[:, :, :], in1=pastm[:, :, :], op=ALU.mult),
                      reads=[Bsel[s], Bmk], writes=[Bsel[s]])

            pairs = []
            for h in range(16):
                for i in range(8):
                    lst = [(h, i, i, 0)]
                    for j in range(i):
                        lst.append((h, i, j, 1 if j == i - 1 else 2))
                    for k in range(8):
                        lst.append((h, i, 8 + k, 1 if (i == 0 and k == 7) else 2))
                    for n, p in enumerate(lst):
                        pairs.append(p + (n == 0, n == len(lst) - 1))

            def emit_S(n):
                h, i, j, kind, first, last = pairs[n]
                s = h % 2
                b = n % 3

                def f():
                    for kt in range(2):
                        ins = nc.tensor.matmul(pS[b][:, kt * 256:(kt + 1) * 256], lhsT=ks[s][:, j, kt * 128:(kt + 1) * 128], rhs=qs[s][:, i, :],
                                               start=True, stop=True)
                    return ins
                em.op("pe", f, reads=[Bhq[s], Bhk[s]], writes=[BpS[b]])

            PLA = 12
            head_load(0)
            head_prologue(0)
            emit_S(0)
            emit_S(1)
            for n in range(len(pairs)):
                h, i, j, kind, first, last = pairs[n]
                s = h % 2
                b = n % 3
                if first and i == 0 and h + 1 < 16:
                    head_load(h + 1)
                if n + PLA < len(pairs) and pairs[n + PLA][0] != pairs[n + PLA - 1][0]:
                    head_prologue(pairs[n + PLA][0])
                if n + 2 < len(pairs):
                    emit_S(n + 2)
                if kind == 2:
                    em.op("act", lambda: nc.scalar.activation(out=PT[b][:], in_=pS[b][:], func=AF.Exp, bias=t31[:, h:h + 1], scale=SCALE),
                          reads=[BpS[b], Bmk], writes=[BPT[b]])
                else:
                    tb = n % 2
                    btile = bsf[s] if kind == 0 else baj[s]
                    em.op("dve", lambda: nc.vector.scalar_tensor_tensor(out=tmpb[tb][:], in0=pS[b][:], scalar=SCALE, in1=btile[:], op0=ALU.mult, op1=ALU.add),
                          reads=[BpS[b], Bhb[s]], writes=[Btmp[tb]])
                    em.op("act", lambda: nc.scalar.activation(out=PT[b][:], in_=tmpb[tb][:], func=AF.Exp), reads=[Btmp[tb]], writes=[BPT[b]])

                def f():
                    for q2 in range(2):
                        for kt in range(2):
                            ins = nc.tensor.matmul(pO[b][:, q2, 0:132], lhsT=PT[b][:, kt * 256 + q2 * 128: kt * 256 + (q2 + 1) * 128],
                                                   rhs=vs[s][:, j * 2 + kt, 0:132], start=(kt == 0), stop=(kt == 1))
                    return ins
                em.op("pe", f, reads=[BPT[b], Bhv[s]], writes=[BpO[b]])
                oa = oacc[i % 2]
                Boa = Boacc[i % 2]
                if first:
                    em.op("dve", lambda: nc.vector.tensor_copy(out=oa[:, :, :], in_=pO[b][:, :, 0:129]), reads=[BpO[b]], writes=[Boa])
                else:
                    for q2 in range(2):
                        em.op("dve", lambda: nc.vector.scalar_tensor_tensor(out=oa[:, q2, :], in0=pO[b][:, q2, 0:129], scalar=selm[s][:, i * 2 + q2, j:j + 1],
                                                                            in1=oa[:, q2, :], op0=ALU.mult, op1=ALU.add),
                              reads=[BpO[b], Bsel[s], Boa], writes=[Boa])
                if last:
                    em.op("dve", lambda: nc.vector.reciprocal(out=rden[:, :], in_=oa[:, :, 128]), reads=[Boa], writes=[Brden])
                    for q2 in range(2):
                        em.op("dve", lambda: nc.vector.tensor_scalar(out=obf[:, q2, :], in0=oa[:, q2, 0:128], scalar1=rden[:, q2:q2 + 1], scalar2=None, op0=ALU.mult),
                              reads=[Boa, Brden], writes=[Bobf])

                    def f():
                        for q2 in range(2):
                            ins = nc.tensor.transpose(out=pTr[:, q2 * 128:(q2 + 1) * 128], in_=obf[:, q2, :], identity=idb[:])
                        return ins
                    em.op("pe", f, reads=[Bobf, B_c], writes=[BpTr])
                    em.op("act", lambda: nc.scalar.copy(out=attnT[:, h, i * 256:(i + 1) * 256], in_=pTr[:, 0:256]), reads=[BpTr], writes=[BattnT])
            em.dma("sp", "st_at", S_attnT.rearrange("c p t -> p c t"), attnT[:, :, :], reads=[BattnT], writes=[B_d["attnT"]])
            em.barrier()

        checkpoint("C")
        with ExitStack() as st:
            D1_attnT = SB(st, "attnT1", [128, 16, T], BF16)
            D1_B = Buf("attnT1")
            em.dma("sp", "ld_act", D1_attnT[:, :, :], S_attnT.rearrange("c p t -> p c t"), reads=[B_d["attnT"]], writes=[D1_B])
            if True:
                st2 = st
                G = Gemm(st2, "d1")
                gsb = [SB(st2, f"gsb{i}", [128, T], F32) for i in range(2)]
                Bgsb = [Buf("gsb0"), Buf("gsb1")]
                zst = [SB(st2, f"zst{i}", [128, T], F32) for i in range(2)]
                Bzst = [Buf("zst0"), Buf("zst1")]
                blocks = []
                cnt = [0]
                for ob in range(4):
                    def load(slot, ob=ob):
                        G.wload(slot, 0, 512, w_attn[:, ob * 512:(ob + 1) * 512])

                    def run(slot, ob=ob):
                        for sub in range(4):
                            ch = ob * 4 + sub
                            s = cnt[0] % 2
                            cnt[0] += 1
                            em.dma("sp", f"ld_gs{s}", gsb[s][:], S_gaT[ch, :, :], reads=[B_d["gaT"]], writes=[Bgsb[s]])
                            for (t0, tn) in TG4:
                                pbk, Bp = G.mm_fm(slot, sub * 128, D1_attnT, D1_B, t0, tn)
                                em.op("dve", lambda: nc.vector.tensor_tensor(out=zst[s][:, t0:t0 + tn], in0=pbk[:, 0:tn], in1=gsb[s][:, t0:t0 + tn], op=ALU.mult),
                                      reads=[Bp, Bgsb[s]], writes=[Bzst[s]])
                            em.dma("sp", f"st_z{s}", S_z1T[ch, :, :], zst[s][:], reads=[Bzst[s]], writes=[B_d["z1T"]])
                    blocks.append(dict(load=load, run=run))
                G.run(blocks)
                em.barrier()

        checkpoint("D1")
        with ExitStack() as st:
            convT = SB(st, "convT", [128, 16, T], BF16)
            BconvT = Buf("convT")
            lg = SB(st, "lg", [128, 16], F32); lb = SB(st, "lb", [128, 16], F32); Blg = Buf("lg")
            em.dma("sp", "ld_lg", lg[:], ln_g[:, :], writes=[Blg])
            em.dma("sp", "ld_lg", lb[:], ln_b[:, :], writes=[Blg])
            mu = SB(st, "mu", [128, T], F32); rs = SB(st, "rs", [128, T], F32)
            Bmu = Buf("mu"); Brs = Buf("rs")
            em.dma("sp", "ld_mu", mu[:], S_mu[:, :], reads=[B_d["mu"]], writes=[Bmu])
            em.dma("sp", "ld_rs", rs[:], S_rs[:, :], reads=[B_d["mu"]], writes=[Brs])
            with ExitStack() as st2:
                cl = [SB(st2, f"cl{i}", [128, T], F32) for i in range(2)]
                Bcl = [Buf("cl0"), Buf("cl1")]
                for cc in range(16):
                    s = cc % 2
                    em.dma("sp", f"ld_cl{s}", cl[s][:], S_cT[cc, :, :], reads=[B_d["cT"]], writes=[Bcl[s]])
                    em.op("dve", lambda: nc.vector.tensor_tensor(out=cl[s][:], in0=cl[s][:], in1=mu[:], op=ALU.subtract), reads=[Bcl[s], Bmu], writes=[Bcl[s]])
                    em.op("dve", lambda: nc.vector.tensor_tensor(out=cl[s][:], in0=cl[s][:], in1=rs[:], op=ALU.mult), reads=[Bcl[s], Brs], writes=[Bcl[s]])
                    em.op("act", lambda: nc.scalar.activation(out=convT[:, cc, :], in_=cl[s][:], func=AF.Silu, scale=lg[:, cc:cc + 1], bias=lb[:, cc:cc + 1]),
                          reads=[Bcl[s], Blg], writes=[BconvT])
                if debug and "S_convT" in debug:
                    for cc in range(16):
                        em.dma("sp", "st_dbg", S_convT[cc, :, :], convT[:, cc, :], reads=[BconvT], writes=[Buf("dbg")])
                em.barrier()
            with ExitStack() as st2:
                G = Gemm(st2, "d2")
                gsb = [SB(st2, f"gsc{i}", [128, T], F32) for i in range(2)]
                Bgsb = [Buf("gsc0"), Buf("gsc1")]
                z1b = [SB(st2, f"z1b{i}", [128, T], F32) for i in range(2)]
                Bz1b = [Buf("z1b0"), Buf("z1b1")]
                zst = [SB(st2, f"zsb{i}", [128, T], BF16) for i in range(2)]
                Bzst = [Buf("zsb0"), Buf("zsb1")]
                tmpz = [SB(st2, f"tmpz{i}", [128, 512], F32) for i in range(2)]
                Btz = [Buf("tmpz0"), Buf("tmpz1")]
                blocks = []
                cnt = [0, 0]
                for ob in range(4):
                    def load(slot, ob=ob):
                        G.wload(slot, 0, 512, w_conv[:, ob * 512:(ob + 1) * 512])

                    def run(slot, ob=ob):
                        for sub in range(4):
                            ch = ob * 4 + sub
                            s = cnt[0] % 2
                            cnt[0] += 1
                            em.dma("sp", f"ld_gs{s}", gsb[s][:], S_gcT[ch, :, :], reads=[B_d["gcT"]], writes=[Bgsb[s]])
                            em.dma("sp", f"ld_z1{s}", z1b[s][:], S_z1T[ch, :, :], reads=[B_d["z1T"]], writes=[Bz1b[s]])
                            for (t0, tn) in TG4:
                                pbk, Bp = G.mm_fm(slot, sub * 128, convT, BconvT, t0, tn)
                                u = cnt[1] % 2
                                cnt[1] += 1
                                em.op("dve", lambda: nc.vector.tensor_tensor(out=tmpz[u][:, 0:tn], in0=pbk[:, 0:tn], in1=gsb[s][:, t0:t0 + tn], op=ALU.mult),
                                      reads=[Bp, Bgsb[s]], writes=[Btz[u]])
                                em.op("dve", lambda: nc.vector.tensor_tensor(out=zst[s][:, t0:t0 + tn], in0=tmpz[u][:, 0:tn], in1=z1b[s][:, t0:t0 + tn], op=ALU.add),
                                      reads=[Btz[u], Bz1b[s]], writes=[Bzst[s]])
                            em.dma("sp", f"st_z{s}", S_zT[ch, :, :], zst[s][:], reads=[Bzst[s]], writes=[B_d["zT"]])
                    blocks.append(dict(load=load, run=run))
                G.run(blocks)
                em.barrier()

        checkpoint("B3")
        def resid_gemm(st, tag, actT, Bact, w_ap, res_src, res_buf, dst, dst_key):
            G = Gemm(st, tag)
            xsl = [SB(st, f"xsl{tag}{i}", [128, 512], F32) for i in range(3)]
            Bxsl = [Buf(f"xsl{i}") for i in range(3)]
            hst = [SB(st, f"hst{tag}{i}", [128, 512], F32) for i in range(2)]
            Bhst = [Buf("hst0"), Buf("hst1")]
            blocks = []
            cnt = [0]
            for ob in range(4):
                def load(slot, ob=ob):
                    G.wload(slot, 0, 512, w_ap[:, ob * 512:(ob + 1) * 512])

                def run(slot, ob=ob):
                    def ldx(t):
                        em.dma("sp", f"ld_xsl{t % 3}", xsl[t % 3][:], res_src(t, ob), reads=res_buf, writes=[Bxsl[t % 3]])
                    ldx(0)
                    ldx(1)
                    for t in range(16):
                        if t + 2 < 16:
                            ldx(t + 2)
                        pbk, Bp = G.mm_tm(slot, actT, Bact, t)
                        s = cnt[0] % 2
                        cnt[0] += 1
                        em.op("dve", lambda: nc.vector.tensor_tensor(out=hst[s][:], in0=pbk[:, :], in1=xsl[t % 3][:], op=ALU.add),
                              reads=[Bp, Bxsl[t % 3]], writes=[Bhst[s]])
                        em.dma("sp", f"st_h{s}", dst[t * 128:(t + 1) * 128, ob * 512:(ob + 1) * 512], hst[s][:], reads=[Bhst[s]], writes=[B_d[dst_key]])
                blocks.append(dict(load=load, run=run))
            G.run(blocks)

        with ExitStack() as st:
            zT = SB(st, "zTa", [128, 16, T], BF16)
            BzT = Buf("zTa")
            em.dma("sp", "ld_act", zT[:, :, :], S_zT.rearrange("c p t -> p c t"), reads=[B_d["zT"]], writes=[BzT])
            resid_gemm(st, "e", zT, BzT, w_o, lambda t, ob: xa[t * 128:(t + 1) * 128, ob * 512:(ob + 1) * 512], [], S_h1, "h1")
            em.barrier()

        checkpoint("E")
        with ExitStack() as st:
            x2T = SB(st, "x2T", [128, 16, T], BF16)
            Bx2T = Buf("x2T")
            combT = SB(st, "combT", [16, T], F32)
            BcombT = Buf("combT")
            sel_sb = SB(st, "sel_sb", [16, D], F32)
            Bsl = Buf("sel_sb")
            em.dma("sp", "ld_sl", sel_sb[:], sel16_d[:, :], writes=[Bsl])
            with ExitStack() as s1:
                src = lambda t: S_h1[t * 128:(t + 1) * 128, :]
                rstd, Bss = rms_stats(s1, src, 16, "f")
                gB = SB(s1, "gBf", [128, D], F32); BgB = Buf("gBf")
                em.dma("sp", "ld_g", gB[:], g_ffn.partition_broadcast(128), writes=[BgB])
                wr = SB(s1, "wr", [128, 16, 20], F32); Bwr = Buf("wr")
                em.dma("sp", "ld_wr", wr[:], w_r.rearrange("(k p) n -> p k n", p=128), writes=[Bwr])
                brb = SB(s1, "brb", [128, 20], F32)
                em.dma("sp", "ld_wr", brb[:], b_r.partition_broadcast(128), writes=[Bwr])
                xt = [SB(s1, f"xtf{i}", [128, D], F32) for i in range(2)]
                Bx = [Buf("xtf0"), Buf("xtf1")]
                xn = [SB(s1, f"xnf{i}", [128, D], F32) for i in range(2)]
                Bxn = [Buf("xnf0"), Buf("xnf1")]
                xf = [SB(s1, f"xf{i}", [128, 16, 128], F32) for i in range(2)]
                Bxf = [Buf("xf0"), Buf("xf1")]
                pF = [PS(s1, f"pF{i}", [128, 4, 128], F32) for i in range(4)]
                BpF = [Buf(f"pF{i}") for i in range(4)]
                pR = PS(s1, "pR", [128, 512], F32); BpR = Buf("pR")
                pC = PS(s1, "pC", [128, 512], F32); BpC = Buf("pC")
                comb = SB(s1, "comb", [128, 16, 16], F32); Bcomb = Buf("comb")
                R = {k: SB(s1, "r_" + k, [128, n], F32) for k, n in
                     dict(L=20, cmax=1, ncmax=1, ohg=4, ecl=4, esum=1, pg=1, fsel=4, v1=1, m1=4, fs2=4, v2=1, m2=4, d=1, e=1, den=1,
                          w1=1, w2=1, t1=4, fine=4, pf=4).items()}
                BR = Buf("router_scratch")

                def dv(fn, reads=(), writes=()):
                    em.op("dve", fn, reads=[BR] + list(reads), writes=[BR] + list(writes))

                for t in range(16):
                    s = t % 2
                    em.dma("sp", f"ld_xt{s}", xt[s][:], S_h1[t * 128:(t + 1) * 128, :], reads=[B_d["h1"]], writes=[Bx[s]])
                    em.op("dve", lambda: nc.vector.scalar_tensor_tensor(out=xn[s][:], in0=xt[s][:], scalar=rstd[:, t:t + 1], in1=gB[:], op0=ALU.mult, op1=ALU.mult),
                          reads=[Bx[s], Bss, BgB], writes=[Bxn[s]])
                    for k in range(4):
                        def f():
                            for j in range(4):
                                c = k * 4 + j
                                ins = nc.tensor.transpose(out=pF[k][:, j, :], in_=xn[s][:, c * 128:(c + 1) * 128], identity=idf[:])
                            return ins
                        em.op("pe", f, reads=[Bxn[s], B_c], writes=[BpF[k]])
                        em.op("dve", lambda: nc.vector.tensor_copy(out=xf[s][:, k * 4:(k + 1) * 4, :], in_=pF[k][:, :, :]), reads=[BpF[k]], writes=[Bxf[s]])
                        em.op("act", lambda: nc.scalar.copy(out=x2T[:, k * 4:(k + 1) * 4, t * 128:(t + 1) * 128], in_=xf[s][:, k * 4:(k + 1) * 4, :]), reads=[Bxf[s]], writes=[Bx2T])

                    def f():
                        for c in range(16):
                            ins = nc.tensor.matmul(pR[:, 0:20], lhsT=xf[s][:, c, :], rhs=wr[:, c, :], start=(c == 0), stop=(c == 15))
                        return ins
                    em.op("pe", f, reads=[Bxf[s], Bwr], writes=[BpR])
                    L = R["L"]
                    dv(lambda: nc.vector.tensor_tensor(out=L[:], in0=pR[:, 0:20], in1=brb[:], op=ALU.add), reads=[BpR, Bwr])
                    dv(lambda: nc.vector.tensor_reduce(out=R["cmax"][:], in_=L[:, 0:4], axis=AX.X, op=ALU.max))
                    dv(lambda: nc.vector.tensor_scalar(out=R["ncmax"][:], in0=R["cmax"][:], scalar1=-1.0, scalar2=None, op0=ALU.mult))
                    dv(lambda: nc.vector.tensor_scalar(out=R["ohg"][:], in0=L[:, 0:4], scalar1=R["cmax"][:, 0:1], scalar2=None, op0=ALU.is_ge))
                    em.op("act", lambda: nc.scalar.activation(out=R["ecl"][:], in_=L[:, 0:4], func=AF.Exp, bias=R["ncmax"][:, 0:1], scale=1.0),
                          reads=[BR], writes=[BR])
                    dv(lambda: nc.vector.tensor_reduce(out=R["esum"][:], in_=R["ecl"][:], axis=AX.X, op=ALU.add))
                    dv(lambda: nc.vector.reciprocal(out=R["pg"][:], in_=R["esum"][:]))
                    dv(lambda: nc.vector.tensor_scalar(out=R["fsel"][:], in0=L[:, 4:8], scalar1=R["ohg"][:, 0:1], scalar2=None, op0=ALU.mult))
                    for g in range(1, 4):
                        dv(lambda: nc.vector.scalar_tensor_tensor(out=R["fsel"][:], in0=L[:, 4 + 4 * g:8 + 4 * g], scalar=R["ohg"][:, g:g + 1], in1=R["fsel"][:],
                                                                  op0=ALU.mult, op1=ALU.add))
                    dv(lambda: nc.vector.tensor_reduce(out=R["v1"][:], in_=R["fsel"][:], axis=AX.X, op=ALU.max))
                    dv(lambda: nc.vector.tensor_scalar(out=R["m1"][:], in0=R["fsel"][:], scalar1=R["v1"][:, 0:1], scalar2=None, op0=ALU.is_ge))
                    dv(lambda: nc.vector.scalar_tensor_tensor(out=R["fs2"][:], in0=R["m1"][:], scalar=NEG, in1=R["fsel"][:], op0=ALU.mult, op1=ALU.add))
                    dv(lambda: nc.vector.tensor_reduce(out=R["v2"][:], in_=R["fs2"][:], axis=AX.X, op=ALU.max))
                    dv(lambda: nc.vector.tensor_scalar(out=R["m2"][:], in0=R["fs2"][:], scalar1=R["v2"][:, 0:1], scalar2=None, op0=ALU.is_ge))
                    dv(lambda: nc.vector.tensor_tensor(out=R["d"][:], in0=R["v2"][:], in1=R["v1"][:], op=ALU.subtract))
                    em.op("act", lambda: nc.scalar.activation(out=R["e"][:], in_=R["d"][:], func=AF.Exp), reads=[BR], writes=[BR])
                    dv(lambda: nc.vector.tensor_scalar(out=R["den"][:], in0=R["e"][:], scalar1=1.0, scalar2=None, op0=ALU.add))
                    dv(lambda: nc.vector.reciprocal(out=R["w1"][:], in_=R["den"][:]))
                    dv(lambda: nc.vector.tensor_tensor(out=R["w2"][:], in0=R["e"][:], in1=R["w1"][:], op=ALU.mult))
                    dv(lambda: nc.vector.tensor_scalar(out=R["t1"][:], in0=R["m1"][:], scalar1=R["w1"][:, 0:1], scalar2=None, op0=ALU.mult))
                    dv(lambda: nc.vector.scalar_tensor_tensor(out=R["fine"][:], in0=R["m2"][:], scalar=R["w2"][:, 0:1], in1=R["t1"][:], op0=ALU.mult, op1=ALU.add))
                    dv(lambda: nc.vector.tensor_scalar(out=R["pf"][:], in0=R["fine"][:], scalar1=R["pg"][:, 0:1], scalar2=None, op0=ALU.mult))
                    for g in range(4):
                        dv(lambda: nc.vector.tensor_scalar(out=comb[:, t, 4 * g:4 * g + 4], in0=R["pf"][:], scalar1=R["ohg"][:, g:g + 1], scalar2=None, op0=ALU.mult),
                           writes=[Bcomb])
                    em.op("pe", lambda: nc.tensor.matmul(pC[0:16, 0:128], lhsT=comb[:, t, :], rhs=idf[:], start=True, stop=True), reads=[Bcomb, B_c], writes=[BpC])
                    em.op("act", lambda: nc.scalar.copy(out=combT[:, t * 128:(t + 1) * 128], in_=pC[0:16, 0:128]), reads=[BpC], writes=[BcombT])
                if debug and "S_comb" in debug:
                    em.dma("sp", "st_dbg", S_comb[:, :, :], comb[:, :, :], reads=[Bcomb], writes=[Buf("dbg")])
                em.barrier()
                checkpoint("F1")
            with ExitStack() as s1:
                G = Gemm(s1, "f1")
                pCB = PS(s1, "pCB", [128, 512], F32); BpCB = Buf("pCB")
                combB = [SB(s1, f"combB{i}", [128, T], F32) for i in range(2)]
                BcombB = [Buf("combB0"), Buf("combB1")]
                sg = [SB(s1, f"sg{i}", [128, T], F32) for i in range(4)]
                Bsg = [Buf(f"sg{i}") for i in range(4)]
                hst = [SB(s1, f"hstb{i}", [128, T], BF16) for i in range(2)]
                Bhst = [Buf("hstb0"), Buf("hstb1")]
                tmph = [SB(s1, f"tmph{i}", [128, 512], F32) for i in range(2)]
                Btmph = [Buf("tmph0"), Buf("tmph1")]
                blocks = []
                cnt = [0, 0]
                for e in range(16):
                    for fp in range(2):
                        def load(slot, e=e, fp=fp):
                            G.wload(slot, 0, 256, w_eg[e, :, fp * 256:(fp + 1) * 256])
                            G.wload(slot, 256, 512, w_eu[e, :, fp * 256:(fp + 1) * 256])

                        def run(slot, e=e, fp=fp):
                            cbs = combB[e % 2]
                            if fp == 0:
                                for (t0, tn) in TG4:
                                    em.op("pe", lambda: nc.tensor.matmul(pCB[:, 0:tn], lhsT=sel_sb[:, e * 128:(e + 1) * 128], rhs=combT[:, t0:t0 + tn], start=True, stop=True),
                                          reads=[Bsl, BcombT], writes=[BpCB])
                                    em.op("act", lambda: nc.scalar.copy(out=cbs[:, t0:t0 + tn], in_=pCB[:, 0:tn]), reads=[BpCB], writes=[BcombB[e % 2]])
                            par = (e * 2 + fp) % 2
                            for fl in range(2):
                                for (t0, tn) in TG4:
                                    pbk, Bp = G.mm_fm(slot, fl * 128, x2T, Bx2T, t0, tn)
                                    em.op("act", lambda: nc.scalar.activation(out=sg[par * 2 + fl][:, t0:t0 + tn], in_=pbk[:, 0:tn], func=AF.Silu),
                                          reads=[Bp], writes=[Bsg[par * 2 + fl]])
                            for fl in range(2):
                                s = cnt[0] % 2
                                cnt[0] += 1
                                for (t0, tn) in TG4:
                                    pbk, Bp = G.mm_fm(slot, 256 + fl * 128, x2T, Bx2T, t0, tn)
                                    u = cnt[1] % 2
                                    cnt[1] += 1
                                    em.op("dve", lambda: nc.vector.tensor_tensor(out=tmph[u][:, 0:tn], in0=pbk[:, 0:tn], in1=sg[par * 2 + fl][:, t0:t0 + tn], op=ALU.mult),
                                          reads=[Bp, Bsg[par * 2 + fl]], writes=[Btmph[u]])
                                    em.op("dve", lambda: nc.vector.tensor_tensor(out=hst[s][:, t0:t0 + tn], in0=tmph[u][:, 0:tn], in1=cbs[:, t0:t0 + tn], op=ALU.mult),
                                          reads=[Btmph[u], BcombB[e % 2]], writes=[Bhst[s]])
                                em.dma("sp", f"st_H{s}", S_HT[e * 4 + fp * 2 + fl, :, :], hst[s][:], reads=[Bhst[s]], writes=[B_d["HT"]])
                        blocks.append(dict(load=load, run=run))
                G.run(blocks)
                em.barrier()
                checkpoint("F2")
        with ExitStack() as st:
            G = Gemm(st, "f2", nbanks=8, nw=3)
            HTq = [SB(st, f"HTq{i}", [128, 16, 512], BF16) for i in range(4)]
            BHTq = [Buf(f"HTq{i}") for i in range(4)]
            xsl = [SB(st, f"xslf{i}", [128, 512], F32) for i in range(4)]
            Bxsl = [Buf(f"xslf{i}") for i in range(4)]
            hst = [SB(st, f"hstf{i}", [128, 512], F32) for i in range(2)]
            Bhst = [Buf("hstf0"), Buf("hstf1")]
            S_HTv = S_HT.rearrange("c p t -> p c t")
            blocks = []
            cnt = [0]

            def ldH(Gi, kq):
                em.dma("sp", f"ld_HT{kq}", HTq[kq][:, :, :], S_HTv[:, kq * 16:(kq + 1) * 16, Gi * 512:(Gi + 1) * 512], reads=[B_d["HT"]], writes=[BHTq[kq]])
            for kq in range(4):
                ldH(0, kq)
            for Gi in range(4):
                for cb in range(4):
                    for kq in range(4):
                        def load(slot, cb=cb, kq=kq):
                            G.wload(slot, 0, 512, w_ed[kq * 2048:(kq + 1) * 2048, cb * 512:(cb + 1) * 512])

                        def run(slot, Gi=Gi, cb=cb, kq=kq):
                            base = (cb % 2) * 4
                            if kq == 0:
                                for tl in range(4):
                                    em.dma("sp", f"ld_xslf{tl}", xsl[tl][:], S_h1[(Gi * 4 + tl) * 128:(Gi * 4 + tl + 1) * 128, cb * 512:(cb + 1) * 512],
                                           reads=[B_d["h1"]], writes=[Bxsl[tl]])
                            for tl in range(4):
                                G.mm_tm(slot, HTq[kq], BHTq[kq], tl, bk=base + tl, first=(kq == 0), last=(kq == 3))
                            if cb == 3 and Gi + 1 < 4:
                                ldH(Gi + 1, kq)
                            if kq == 3:
                                for tl in range(4):
                                    s = cnt[0] % 2
                                    cnt[0] += 1
                                    em.op("dve", lambda: nc.vector.tensor_tensor(out=hst[s][:], in0=G.pb[base + tl][:, :], in1=xsl[tl][:], op=ALU.add),
                                          reads=[G.Bp[base + tl], Bxsl[tl]], writes=[Bhst[s]])
                                    em.dma("sp", f"st_h{s}", S_h2[(Gi * 4 + tl) * 128:(Gi * 4 + tl + 1) * 128, cb * 512:(cb + 1) * 512], hst[s][:],
                                           reads=[Bhst[s]], writes=[B_d["h2"]])
                        blocks.append(dict(load=load, run=run))
            G.run(blocks)
            em.barrier()

        checkpoint("F")
        with ExitStack() as st:
            x3T = SB(st, "x3T", [128, 16, T], BF16)
            Bx3T = Buf("x3T")
            norm_T(st, lambda t: S_h2[t * 128:(t + 1) * 128, :], 16, g_ple, x3T, Bx3T, "g")
            pT = SB(st, "pT", [128, 2, T], BF16); BpT = Buf("pT")
            wp = SB(st, "wp", [128, 2, D], BF16); Bwp = Buf("wp")
            em.dma("pool", "ld_wp", wp[:, :, :], w_pp.rearrange("(k p) n -> p k n", p=128), writes=[Bwp])
            with ExitStack() as s1:
                pl = [SB(s1, f"pl{i}", [128, 256], F32) for i in range(2)]
                Bpl = [Buf("pl0"), Buf("pl1")]
                plb = [SB(s1, f"plb{i}", [128, 256], BF16) for i in range(2)]
                Bplb = [Buf("plb0"), Buf("plb1")]
                ptp = PS(s1, "ptp", [128, 8, 128], BF16); Bptp = Buf("ptp")
                for t in range(16):
                    s = t % 2
                    em.dma("sp", f"ld_pl{s}", pl[s][:], pp[t * 128:(t + 1) * 128, :], writes=[Bpl[s]])
                    em.op("dve", lambda: nc.vector.tensor_copy(out=plb[s][:], in_=pl[s][:]), reads=[Bpl[s]], writes=[Bplb[s]])

                    def f():
                        for j in range(2):
                            ins = nc.tensor.transpose(out=ptp[:, j, :], in_=plb[s][:, j * 128:(j + 1) * 128], identity=idb[:])
                        return ins
                    em.op("pe", f, reads=[Bplb[s], B_c], writes=[Bptp])
                    em.op("act", lambda: nc.scalar.copy(out=pT[:, :, t * 128:(t + 1) * 128], in_=ptp[:, 0:2, :]), reads=[Bptp], writes=[BpT])
                em.barrier()
            G = Gemm(st, "g")
            pP = [PS(st, f"pP{i}", [128, 512], F32) for i in range(2)]
            BpP = [Buf("pP0"), Buf("pP1")]
            xsl = [SB(st, f"xslg{i}", [128, 512], F32) for i in range(3)]
            Bxsl = [Buf(f"xslg{i}") for i in range(3)]
            sgg = [SB(st, f"sgg{i}", [128, 512], F32) for i in range(2)]
            Bsgg = [Buf("sgg0"), Buf("sgg1")]
            hst = [SB(st, f"hstg{i}", [128, 512], F32) for i in range(2)]
            Bhst = [Buf("hstg0"), Buf("hstg1")]
            blocks = []
            cnt = [0]
            for ob in range(4):
                def load(slot, ob=ob):
                    G.wload(slot, 0, 512, w_pg[:, ob * 512:(ob + 1) * 512])

                def run(slot, ob=ob):
                    def ldx(t):
                        em.dma("sp", f"ld_xsl{t % 3}", xsl[t % 3][:], S_h2[t * 128:(t + 1) * 128, ob * 512:(ob + 1) * 512], reads=[B_d["h2"]], writes=[Bxsl[t % 3]])
                    ldx(0)
                    ldx(1)
                    for t in range(16):
                        if t + 2 < 16:
                            ldx(t + 2)
                        pbk, Bp = G.mm_tm(slot, x3T, Bx3T, t)
                        s = cnt[0] % 2
                        cnt[0] += 1

                        def f():
                            for kc in range(2):
                                ins = nc.tensor.matmul(pP[s][:, :], lhsT=pT[:, kc, t * 128:(t + 1) * 128], rhs=wp[:, kc, ob * 512:(ob + 1) * 512],
                                                       start=(kc == 0), stop=(kc == 1))
                            return ins
                        em.op("pe", f, reads=[BpT, Bwp], writes=[BpP[s]])
                        em.op("act", lambda: nc.scalar.activation(out=sgg[s][:], in_=pbk[:, :], func=AF.Sigmoid), reads=[Bp], writes=[Bsgg[s]])
                        em.op("dve", lambda: nc.vector.tensor_tensor(out=hst[s][:], in0=pP[s][:, :], in1=sgg[s][:], op=ALU.mult),
                              reads=[BpP[s], Bsgg[s]], writes=[Bhst[s]])
                        em.op("dve", lambda: nc.vector.tensor_tensor(out=hst[s][:], in0=hst[s][:], in1=xsl[t % 3][:], op=ALU.add),
                              reads=[Bxsl[t % 3], Bhst[s]], writes=[Bhst[s]])
                        em.dma("sp", f"st_h{s}", S_h3[t * 128:(t + 1) * 128, ob * 512:(ob + 1) * 512], hst[s][:], reads=[Bhst[s]], writes=[B_d["h3"]])
                blocks.append(dict(load=load, run=run))
            G.run(blocks)
            em.barrier()

        checkpoint("G")
        with ExitStack() as st:
            src = lambda t: S_h3[t * 128:(t + 1) * 128, :]
            rstd, Bss = rms_stats(st, src, 16, "h")
            gB = SB(st, "gBh", [128, D], F32); BgB = Buf("gBh")
            em.dma("sp", "ld_g", gB[:], g_fin.partition_broadcast(128), writes=[BgB])
            xt = [SB(st, f"xth{i}", [128, D], F32) for i in range(2)]
            Bx = [Buf("xth0"), Buf("xth1")]
            yo = [SB(st, f"yo{i}", [128, D], F32) for i in range(2)]
            Byo = [Buf("yo0"), Buf("yo1")]
            for t in range(16):
                s = t % 2
                em.dma("sp", f"ld_xt{s}", xt[s][:], src(t), reads=[B_d["h3"]], writes=[Bx[s]])
                em.op("dve", lambda: nc.vector.scalar_tensor_tensor(out=yo[s][:], in0=xt[s][:], scalar=rstd[:, t:t + 1], in1=gB[:], op0=ALU.mult, op1=ALU.mult),
                      reads=[Bx[s], Bss, BgB], writes=[Byo[s]])
                em.dma("sp", f"st_o{s}", out_d[t * 128:(t + 1) * 128, :], yo[s][:], reads=[Byo[s]], writes=[B_d["out"]])
            em.barrier()
        es.close()
      except _Stop:
        em.barrier()
        try:
            es.close()
        except AssertionError:
            pass
    return nc


def _rel_bucket(dist):
    n = np.maximum(dist, 0)
    max_exact = 16
    nf = np.maximum(n, 1).astype(np.float32)
    large = max_exact + (np.log(nf / np.float32(max_exact)) / np.float32(math.log(128 / max_exact)) * np.float32(32 - max_exact)).astype(np.int32)
    large = np.minimum(large, 31)
    return np.where(n < max_exact, n, large)


def make_in_maps(inp):
    f = lambda k: np.ascontiguousarray(np.asarray(inp[k], dtype=np.float32))
    x = f("x"); p = f("p")[0]
    rel_bias = f("rel_bias")
    kk = np.arange(256)[:, None]
    qq = np.arange(256)[None, :]
    bs = rel_bias[_rel_bucket(qq - kk)]
    bs = np.where((qq >= kk)[:, :, None], bs, np.float32(NEG)).astype(np.float32)
    ba = rel_bias[_rel_bucket(qq + 256 - kk)].astype(np.float32)

    def lay(b):
        b = b.transpose(2, 0, 1).reshape(16, 2, 128, 256).transpose(0, 2, 1, 3).reshape(16, 128, 512)
        return np.ascontiguousarray(b)
    bias_self = lay(bs); bias_adj = lay(ba)
    t31 = np.ascontiguousarray(np.broadcast_to(rel_bias[31][None, :], (128, 16))).astype(np.float32)
    colvec = lambda v: np.ascontiguousarray(v.reshape(16, 128).T)
    shared = {
        "w_in": f("w_in")[0], "w_attn_br": f("w_attn_br")[0], "w_conv_br": f("w_conv_br")[0], "w_o": f("w_o")[0],
        "w_ple_gate": f("w_ple_gate")[0], "w_ple_proj": f("w_ple_proj")[0],
        "w_e_gate": f("w_e_gate")[0], "w_e_up": f("w_e_up")[0], "w_e_down": f("w_e_down")[0].reshape(16 * 512, D),
        "w_r": np.ascontiguousarray(np.concatenate([f("w_router_g")[0], f("w_router_e")[0]], axis=1)),
        "b_r": np.ascontiguousarray(np.concatenate([f("b_router_g")[0], f("b_router_e")[0]])[None, :]),
        "g_mix": f("g_mix"), "g_ffn": f("g_ffn"), "g_ple": f("g_ple"), "g_final": f("g_final")[None, :],
        "conv_wT": np.ascontiguousarray(f("conv_w")[0].T.reshape(16, 128, 31).transpose(1, 0, 2)),
        "conv_b": colvec(f("conv_b")[0]), "ln_g": colvec(f("ln_g")[0]), "ln_b": colvec(f("ln_b")[0]),
        "ident": np.eye(128, dtype=np.float32),
        "sel16": np.ascontiguousarray(np.repeat(np.eye(16, dtype=np.float32), 128, axis=1)),
        "bias_self": bias_self, "bias_adj": bias_adj, "t31": t31,
    }
    maps = []
    for c in range(8):
        b, hf = c // 2, c % 2
        own = x[b, hf * T:(hf + 1) * T]
        oth = x[b, (1 - hf) * T:(2 - hf) * T]
        halo = np.zeros((128, D), np.float32)
        if hf == 1:
            halo[96:128] = x[b, T - 32:T]
        xa = np.concatenate([own, oth, halo], axis=0)
        gbl = np.concatenate([np.arange(8) + 8 * hf, np.arange(8) + 8 * (1 - hf)])
        past = (gbl[None, :] < (np.arange(8) + 8 * hf)[:, None])
        past_q = np.repeat(past, 2, axis=0)
        pastm = np.broadcast_to(past_q.astype(np.float32).reshape(1, 256), (128, 256))
        pastb = np.where(pastm > 0, np.float32(0), np.float32(NEG)).astype(np.float32)
        m = dict(shared)
        m.update({"xa": np.ascontiguousarray(xa), "pp": np.ascontiguousarray(p[b, hf * T:(hf + 1) * T]),
                  "pastm": np.ascontiguousarray(pastm), "pastb": np.ascontiguousarray(pastb)})
        maps.append(m)
    return maps


_NC = {}


def kernel(**inputs):
    if "nc" not in _NC:
        _NC["nc"] = build(False)
    maps = make_in_maps(inputs)
    res = run_bass_kernel_spmd(_NC["nc"], maps, core_ids=list(range(8)))
    out = np.empty((4, 4096, D), np.float32)
    for c in range(8):
        b, hf = c // 2, c % 2
        out[b, hf * T:(hf + 1) * T] = np.asarray(res.results[c]["out"], dtype=np.float32)
    return out
```

```python
import math
from contextlib import ExitStack
import numpy as np
import concourse.bass as bass
import concourse.mybir as mybir
from concourse.bass_utils import run_bass_kernel_spmd

F32 = mybir.dt.float32
BF16 = mybir.dt.bfloat16
AF = mybir.ActivationFunctionType
ALU = mybir.AluOpType
AX = mybir.AxisListType

D = 2048
T = 2048
NEG = -1e30
EPS = 1e-6
SCALE = 128 ** -0.5


class Buf:
    __slots__ = ("name", "w", "r")

    def __init__(self, name):
        self.name = name
        self.w = None
        self.r = {}


class Eng:
    def __init__(self, name, handle, sem):
        self.name = name
        self.h = handle
        self.sem = sem
        self.count = 0
        self.waited = {}


class Emitter:
    def __init__(self, nc, es):
        self.nc = nc
        self.es = es
        self.sems = {}
        self.engs = {}
        for name, h in (("pe", nc.tensor), ("dve", nc.vector), ("act", nc.scalar),
                        ("pool", nc.gpsimd), ("sp", nc.sync)):
            self.sems["sem_" + name] = es.enter_context(nc.semaphore("sem_" + name))
            self.engs[name] = Eng(name, h, "sem_" + name)
        self.dma_cnt = {}
        self.keymap = {}
        self.pool_keys = []

    def _wait(self, e, deps, skip_self=False):
        for key, val in deps:
            if skip_self and key == e.sem:
                continue
            if e.waited.get(key, 0) < val:
                e.h.wait_ge(self.sems[key], val)
                e.waited[key] = val

    @staticmethod
    def _deps(reads, writes):
        deps = {}
        for b in reads:
            if b.w is not None:
                k, v = b.w
                if deps.get(k, 0) < v:
                    deps[k] = v
        for b in writes:
            if b.w is not None:
                k, v = b.w
                if deps.get(k, 0) < v:
                    deps[k] = v
            for k, v in b.r.items():
                if deps.get(k, 0) < v:
                    deps[k] = v
        return list(deps.items())

    @staticmethod
    def _mark(ev, reads, writes):
        k, v = ev
        for b in reads:
            if b.r.get(k, 0) < v:
                b.r[k] = v
        for b in writes:
            b.w = ev
            b.r = {}

    def op(self, eng, fn, reads=(), writes=()):
        e = self.engs[eng]
        self._wait(e, self._deps(reads, writes), skip_self=(eng == "pe"))
        ins = fn()
        e.count += 1
        ins.then_inc(self.sems[e.sem], 1)
        self._mark((e.sem, e.count), reads, writes)
        return ins

    def dma(self, q, semkey, out, in_, reads=(), writes=()):
        e = self.engs[q]
        if semkey not in self.keymap:
            idx = len(self.keymap)
            if idx >= len(self.pool_keys):
                k = f"dq{idx}"
                self.sems[k] = self.es.enter_context(self.nc.semaphore(k))
                self.dma_cnt[k] = 0
                self.pool_keys.append(k)
            self.keymap[semkey] = self.pool_keys[idx]
        semkey = self.keymap[semkey]
        self._wait(e, self._deps(reads, writes))
        ins = e.h.dma_start(out=out, in_=in_)
        self.dma_cnt[semkey] += 16
        ins.then_inc(self.sems[semkey], 16)
        self._mark((semkey, self.dma_cnt[semkey]), reads, writes)
        return ins

    def barrier(self):
        evs = [(e.sem, e.count) for e in self.engs.values() if e.count > 0]
        evs += [(k, v) for k, v in self.dma_cnt.items() if v > 0]
        for e in self.engs.values():
            self._wait(e, evs)
        self.keymap = {}


class _Stop(Exception):
    pass


def build(debug=False, stop=None):
    nc = bass.Bass("TRN2", target_bir_lowering=False)

    def checkpoint(name):
        if stop == name:
            raise _Stop()

    def din(name, shape, dt=F32):
        return nc.dram_tensor(name, shape, dt, kind="ExternalInput").ap()

    def dscr(name, shape, dt):
        return nc.dram_tensor(name, shape, dt, kind=("ExternalOutput" if (debug and name in debug) else "Internal")).ap()

    xa = din("xa", [4096 + 128, D])
    pp = din("pp", [T, 256])
    w_in = din("w_in", [D, 14336])
    w_attn = din("w_attn_br", [D, D])
    w_conv = din("w_conv_br", [D, D])
    w_o = din("w_o", [D, D])
    w_pg = din("w_ple_gate", [D, D])
    w_pp = din("w_ple_proj", [256, D])
    w_eg = din("w_e_gate", [16, D, 512])
    w_eu = din("w_e_up", [16, D, 512])
    w_ed = din("w_e_down", [16 * 512, D])
    w_r = din("w_r", [D, 20])
    b_r = din("b_r", [1, 20])
    g_mix = din("g_mix", [1, D]); g_ffn = din("g_ffn", [1, D]); g_ple = din("g_ple", [1, D]); g_fin = din("g_final", [1, D])
    conv_wT = din("conv_wT", [128, 16, 31])
    conv_b = din("conv_b", [128, 16]); ln_g = din("ln_g", [128, 16]); ln_b = din("ln_b", [128, 16])
    ident_d = din("ident", [128, 128])
    sel16_d = din("sel16", [16, D])
    bias_self = din("bias_self", [16, 128, 512])
    bias_adj = din("bias_adj", [16, 128, 512])
    t31_d = din("t31", [128, 16])
    pastb_d = din("pastb", [128, 256])
    pastm_d = din("pastm", [128, 256])
    out_d = nc.dram_tensor("out", [T, D], F32, kind="ExternalOutput").ap()

    S_qT = dscr("S_qT", [16, 128, T], BF16)
    S_kT = dscr("S_kT", [16, 128, 4096], BF16)
    S_V = dscr("S_V", [4096, D], BF16)
    S_cT = dscr("S_cT", [16, 128, T], F32)
    S_gaT = dscr("S_gaT", [16, 128, T], F32)
    S_gcT = dscr("S_gcT", [16, 128, T], F32)
    S_z1T = dscr("S_z1T", [16, 128, T], F32)
    S_zT = dscr("S_zT", [16, 128, T], BF16)
    S_h1 = dscr("S_h1", [T, D], F32)
    S_h2 = dscr("S_h2", [T, D], F32)
    S_h3 = dscr("S_h3", [T, D], F32)
    S_HT = dscr("S_HT", [64, 128, T], BF16)
    S_attnT = dscr("S_attnT", [16, 128, T], BF16)
    S_mu = dscr("S_mu", [128, T], F32)
    S_rs = dscr("S_rs", [128, T], F32)
    S_convT = dscr("S_convT", [16, 128, T], BF16)
    S_comb = dscr("S_comb", [128, 16, 16], F32)
    B_d = {k: Buf(k) for k in "qT kT V cT gaT gcT z1T zT h1 h2 h3 HT out attnT mu".split()}

    es = ExitStack()
    if True:
      em = Emitter(nc, es)
      try:

        uid = [0]

        def SB(st, name, shape, dt):
            uid[0] += 1
            return st.enter_context(nc.sbuf_tensor(f"s{uid[0]}_{name}", shape, dt))

        def PS(st, name, shape, dt):
            uid[0] += 1
            return st.enter_context(nc.psum_tensor(f"p{uid[0]}_{name}", shape, dt))

        idf = SB(es, "idf", [128, 128], F32)
        idb = SB(es, "idb", [128, 128], BF16)
        onesf = SB(es, "onesf", [128, 128], F32)
        B_c = Buf("consts")
        em.dma("sp", "ld_c0", idf[:], ident_d[:, :], writes=[B_c])
        em.op("dve", lambda: nc.vector.tensor_copy(out=idb[:], in_=idf[:]), reads=[B_c], writes=[B_c])
        em.op("dve", lambda: nc.vector.memset(onesf[:], 1.0), writes=[B_c])

        def rms_stats(st, src_tile, ntiles, tag):
            xt = [SB(st, f"xs{tag}{i}", [128, D], F32) for i in range(2)]
            Bx = [Buf("xs0"), Buf("xs1")]
            junk = SB(st, f"junk{tag}", [128, D], BF16)
            Bj = Buf("junk")
            ss = SB(st, f"ss{tag}", [128, ntiles], F32)
            rstd = SB(st, f"rstd{tag}", [128, ntiles], F32)
            Bss = Buf("ss")
            em.op("dve", lambda: nc.vector.memset(ss[:], 0.0), writes=[Bss])
            for t in range(ntiles):
                s = t % 2
                em.dma("sp", f"ld_xs{s}", xt[s][:], src_tile(t), writes=[Bx[s]])
                em.op("act", lambda: nc.scalar.activation(out=junk[:], in_=xt[s][:], func=AF.Square, accum_out=ss[:, t:t + 1]),
                      reads=[Bx[s]], writes=[Bj, Bss])
            em.op("dve", lambda: nc.vector.tensor_scalar(out=rstd[:], in0=ss[:], scalar1=1.0 / D, scalar2=EPS, op0=ALU.mult, op1=ALU.add),
                  reads=[Bss], writes=[Bss])
            em.op("act", lambda: nc.scalar.activation(out=rstd[:], in_=rstd[:], func=AF.Sqrt), reads=[Bss], writes=[Bss])
            em.op("dve", lambda: nc.vector.reciprocal(out=rstd[:], in_=rstd[:]), reads=[Bss], writes=[Bss])
            return rstd, Bss

        def norm_T(st, src_tile, ntiles, g_ap, actT, Bact, tag):
            with ExitStack() as s1:
                rstd, Bss = rms_stats(s1, src_tile, ntiles, tag)
                gB = SB(s1, f"gB{tag}", [128, D], F32)
                BgB = Buf("gB")
                em.dma("sp", "ld_g", gB[:], g_ap.partition_broadcast(128), writes=[BgB])
                xt = [SB(s1, f"xt{tag}{i}", [128, D], F32) for i in range(2)]
                Bx = [Buf("xt0"), Buf("xt1")]
                xn = [SB(s1, f"xn{tag}{i}", [128, D], BF16) for i in range(2)]
                Bxn = [Buf("xn0"), Buf("xn1")]
                pt = [PS(s1, f"pt{tag}{i}", [128, 8, 128], BF16) for i in range(2)]
                Bpt = [Buf("pt0"), Buf("pt1")]
                for t in range(ntiles):
                    s = t % 2
                    em.dma("sp", f"ld_xt{s}", xt[s][:], src_tile(t), writes=[Bx[s]])
                    em.op("dve", lambda: nc.vector.scalar_tensor_tensor(out=xn[s][:], in0=xt[s][:], scalar=rstd[:, t:t + 1], in1=gB[:],
                                                                        op0=ALU.mult, op1=ALU.mult),
                          reads=[Bx[s], Bss, BgB], writes=[Bxn[s]])
                    for half in range(2):
                        def f():
                            for j in range(8):
                                c = half * 8 + j
                                ins = nc.tensor.transpose(out=pt[half][:, j, :], in_=xn[s][:, c * 128:(c + 1) * 128], identity=idb[:])
                            return ins
                        em.op("pe", f, reads=[Bxn[s], B_c], writes=[Bpt[half]])
                        dst = actT[:, half * 8:(half + 1) * 8, t * 128:(t + 1) * 128]
                        if half == 0:
                            em.op("act", lambda: nc.scalar.copy(out=dst, in_=pt[half][:, :, :]), reads=[Bpt[half]], writes=[Bact])
                        else:
                            em.op("dve", lambda: nc.vector.tensor_copy(out=dst, in_=pt[half][:, :, :]), reads=[Bpt[half]], writes=[Bact])
                em.barrier()

        class Gemm:
            def __init__(self, st, tag, nbanks=4, nw=3):
                self.wb = [SB(st, f"wb{tag}{i}", [128, 16, 512], BF16) for i in range(nw)]
                self.Bw = [[Buf(f"wb{i}a"), Buf(f"wb{i}b")] for i in range(nw)]
                self.pb = [PS(st, f"pb{tag}{i}", [128, 512], F32) for i in range(nbanks)]
                self.Bp = [Buf(f"pb{i}") for i in range(nbanks)]
                self.nw = nw
                self.bank = 0

            def next_bank(self):
                b = self.bank
                self.bank = (self.bank + 1) % len(self.pb)
                return b

            def wload(self, slot, c0, c1, src):
                part = 0 if c0 == 0 else 1
                em.dma("pool", f"ld_w{slot}_{part}", self.wb[slot][:, :, c0:c1], src.rearrange("(k p) n -> p k n", p=128), writes=[self.Bw[slot][part]])

            def run(self, blocks):
                n = len(blocks)
                for b in range(min(self.nw - 1, n)):
                    blocks[b]["load"](b % self.nw)
                for b in range(n):
                    if b + self.nw - 1 < n:
                        blocks[b + self.nw - 1]["load"]((b + self.nw - 1) % self.nw)
                    blocks[b]["run"](b % self.nw)

            def mm_fm(self, slot, c0, actT, Bact, t0, tn, nk=16):
                bk = self.next_bank()
                pbk = self.pb[bk]
                wbs = self.wb[slot]

                def f():
                    for kc in range(nk):
                        ins = nc.tensor.matmul(pbk[:, 0:tn], lhsT=wbs[:, kc, c0:c0 + 128], rhs=actT[:, kc, t0:t0 + tn],
                                               start=(kc == 0), stop=(kc == nk - 1))
                    return ins
                em.op("pe", f, reads=self.Bw[slot] + [Bact], writes=[self.Bp[bk]])
                return pbk, self.Bp[bk]

            def mm_tm(self, slot, actT, Bact, t, ncols=512, bk=None, first=True, last=True, nk=16, kofs=0):
                if bk is None:
                    bk = self.next_bank()
                pbk = self.pb[bk]
                wbs = self.wb[slot]

                def f():
                    for kc in range(nk):
                        ins = nc.tensor.matmul(pbk[:, 0:ncols], lhsT=actT[:, kofs + kc, t * 128:(t + 1) * 128], rhs=wbs[:, kc, 0:ncols],
                                               start=(first and kc == 0), stop=(last and kc == nk - 1))
                    return ins
                em.op("pe", f, reads=self.Bw[slot] + [Bact], writes=[self.Bp[bk]])
                return pbk, self.Bp[bk]

        cpy_ctr = [0]
        act_only = [False]

        def evac_copy(out, in_, reads, writes):
            cpy_ctr[0] += 1
            if act_only[0] or cpy_ctr[0] % 2:
                em.op("act", lambda: nc.scalar.copy(out=out, in_=in_), reads=reads, writes=writes)
            else:
                em.op("dve", lambda: nc.vector.tensor_copy(out=out, in_=in_), reads=reads, writes=writes)

        TG4 = [(i * 512, 512) for i in range(4)]

        def kv_blocks(G, st, actT, Bact, tok_off, tgroups, tag):
            kst = [SB(st, f"kst{tag}{i}", [128, 512], BF16) for i in range(2)]
            Bkst = [Buf("kst0"), Buf("kst1")]
            vst = [SB(st, f"vst{tag}{i}", [128, 512], BF16) for i in range(2)]
            Bvst = [Buf("vst0"), Buf("vst1")]
            blocks = []
            cnt = [0, 0]
            for kb in range(4):
                def load(slot, kb=kb):
                    G.wload(slot, 0, 512, w_in[:, 2048 + kb * 512: 2048 + (kb + 1) * 512])

                def run(slot, kb=kb):
                    for sub in range(4):
                        h = kb * 4 + sub
                        for (t0, tn) in tgroups:
                            s = cnt[0] % 2
                            cnt[0] += 1
                            pbk, Bp = G.mm_fm(slot, sub * 128, actT, Bact, t0, tn)
                            evac_copy(kst[s][:, 0:tn], pbk[:, 0:tn], [Bp], [Bkst[s]])
                            em.dma("sp", f"st_k{s}", S_kT[h, :, tok_off + t0:tok_off + t0 + tn], kst[s][:, 0:tn], reads=[Bkst[s]], writes=[B_d["kT"]])
                blocks.append(dict(load=load, run=run))
            for vb in range(4):
                def load(slot, vb=vb):
                    G.wload(slot, 0, 512, w_in[:, 4096 + vb * 512: 4096 + (vb + 1) * 512])

                def run(slot, vb=vb):
                    for t in range(16):
                        s = cnt[1] % 2
                        cnt[1] += 1
                        pbk, Bp = G.mm_tm(slot, actT, Bact, t)
                        evac_copy(vst[s][:], pbk[:, :], [Bp], [Bvst[s]])
                        em.dma("sp", f"st_v{s}", S_V[tok_off + t * 128: tok_off + (t + 1) * 128, vb * 512:(vb + 1) * 512], vst[s][:],
                               reads=[Bvst[s]], writes=[B_d["V"]])
                blocks.append(dict(load=load, run=run))
            return blocks

        with ExitStack() as st:
            actT = SB(st, "actT0", [128, 16, T], BF16)
            Bact = Buf("actT0")
            norm_T(st, lambda t: xa[2048 + t * 128: 2048 + (t + 1) * 128, :], 16, g_mix, actT, Bact, "a")
            G = Gemm(st, "a")
            G.run(kv_blocks(G, st, actT, Bact, 2048, TG4, "a"))
            em.barrier()

        checkpoint("B0")
        with ExitStack() as st:
            TA = T + 128
            actT = SB(st, "actT1", [128, 16, TA], BF16)
            Bact = Buf("actT1")

            def src1(t):
                if t < 16:
                    return xa[t * 128:(t + 1) * 128, :]
                return xa[4096:4096 + 128, :]
            norm_T(st, src1, 17, g_mix, actT, Bact, "b")
            act_only[0] = True
            G = Gemm(st, "b")
            cw = SB(st, "cw", [128, 16, 31], F32); cb = SB(st, "cb", [128, 16], F32)
            Bcw = Buf("cw")
            em.dma("sp", "ld_cw", cw[:], conv_wT[:, :, :], writes=[Bcw])
            em.dma("sp", "ld_cw", cb[:], conv_b[:, :], writes=[Bcw])
            csum = SB(st, "csum", [128, T], F32); csq = SB(st, "csq", [128, T], F32)
            Bcs = Buf("csum"); Bcq = Buf("csq")
            em.op("pool", lambda: nc.gpsimd.memset(csum[:], 0.0), writes=[Bcs])
            em.op("pool", lambda: nc.gpsimd.memset(csq[:], 0.0), writes=[Bcq])

            blocks_other = []
            qst = [SB(st, f"qst{i}", [128, 512], BF16) for i in range(2)]
            Bqst = [Buf("qst0"), Buf("qst1")]
            qcnt = [0]
            for qb in range(4):
                def load(slot, qb=qb):
                    G.wload(slot, 0, 512, w_in[:, qb * 512:(qb + 1) * 512])

                def run(slot, qb=qb):
                    for sub in range(4):
                        h = qb * 4 + sub
                        for (t0, tn) in TG4:
                            s = qcnt[0] % 2
                            qcnt[0] += 1
                            pbk, Bp = G.mm_fm(slot, sub * 128, actT, Bact, t0, tn)
                            evac_copy(qst[s][:, 0:tn], pbk[:, 0:tn], [Bp], [Bqst[s]])
                            em.dma("sp", f"st_q{s}", S_qT[h, :, t0:t0 + tn], qst[s][:, 0:tn], reads=[Bqst[s]], writes=[B_d["qT"]])
                blocks_other.append(dict(load=load, run=run))
            blocks_other += kv_blocks(G, st, actT, Bact, 0, TG4, "b")
            gst = [SB(st, f"gst{i}", [128, 512], F32) for i in range(2)]
            Bgst = [Buf("gst0"), Buf("gst1")]
            gcnt = [0]
            for gb in range(8):
                def load(slot, gb=gb):
                    G.wload(slot, 0, 512, w_in[:, 10240 + gb * 512: 10240 + (gb + 1) * 512])

                def run(slot, gb=gb):
                    for sub in range(4):
                        ch = (gb % 4) * 4 + sub
                        dst = S_gaT if gb < 4 else S_gcT
                        for (t0, tn) in TG4:
                            s = gcnt[0] % 2
                            gcnt[0] += 1
                            pbk, Bp = G.mm_fm(slot, sub * 128, actT, Bact, t0, tn)
                            em.op("act", lambda: nc.scalar.activation(out=gst[s][:, 0:tn], in_=pbk[:, 0:tn], func=AF.Sigmoid),
                                  reads=[Bp], writes=[Bgst[s]])
                            em.dma("sp", f"st_g{s}", dst[ch, :, t0:t0 + tn], gst[s][:, 0:tn], reads=[Bgst[s]], writes=[B_d["gaT" if gb < 4 else "gcT"]])
                blocks_other.append(dict(load=load, run=run))
            A_sb = SB(st, "A_sb", [128, TA], F32); BA = Buf("A")
            Us = [SB(st, f"U{i}", [128, 32 + T], F32) for i in range(2)]; BUs = [Buf("U0"), Buf("U1")]
            U = Us[0]; BU = BUs[0]
            acc = [SB(st, f"cacc{i}", [128, T], F32) for i in range(2)]
            Bacc = [Buf("cacc0"), Buf("cacc1")]
            sqb = SB(st, "sqb", [128, T], F32); Bsq = Buf("sqb")
            TG5 = TG4 + [(T, 128)]
            pending_stats = []

            def flush_stats():
                while pending_stats:
                    pending_stats.pop(0)()
            blocks_conv = []
            for cc in range(16):
                def load(slot, cc=cc):
                    G.wload(slot, 0, 128, w_in[:, 6144 + cc * 128: 6144 + (cc + 1) * 128])
                    G.wload(slot, 128, 256, w_in[:, 8192 + cc * 128: 8192 + (cc + 1) * 128])

                def run(slot, cc=cc):
                    flush_stats()
                    U = Us[cc % 2]
                    BU = BUs[cc % 2]
                    for (t0, tn) in TG5:
                        pbk, Bp = G.mm_fm(slot, 0, actT, Bact, t0, tn)
                        em.op("act", lambda: nc.scalar.copy(out=A_sb[:, t0:t0 + tn], in_=pbk[:, 0:tn]), reads=[Bp], writes=[BA])
                    for (t0, tn) in TG5:
                        pbk, Bp = G.mm_fm(slot, 128, actT, Bact, t0, tn)
                        if t0 < T:
                            em.op("act", lambda: nc.scalar.activation(out=U[:, 32 + t0:32 + t0 + tn], in_=pbk[:, 0:tn], func=AF.Sigmoid),
                                  reads=[Bp], writes=[BU])
                        else:
                            em.op("act", lambda: nc.scalar.activation(out=U[:, 0:32], in_=pbk[:, 96:128], func=AF.Sigmoid),
                                  reads=[Bp], writes=[BU])
                    em.op("dve", lambda: nc.vector.tensor_tensor(out=U[:, 0:32], in0=U[:, 0:32], in1=A_sb[:, T + 96:T + 128], op=ALU.mult),
                          reads=[BA, BU], writes=[BU])
                    em.op("dve", lambda: nc.vector.tensor_tensor(out=U[:, 32:32 + T], in0=U[:, 32:32 + T], in1=A_sb[:, 0:T], op=ALU.mult),
                          reads=[BA, BU], writes=[BU])
                    a = acc[cc % 2]
                    Ba = Bacc[cc % 2]
                    em.op("dve", lambda: nc.vector.tensor_scalar(out=a[:], in0=U[:, 2:2 + T], scalar1=cw[:, cc, 0:1], scalar2=cb[:, cc:cc + 1],
                                                                 op0=ALU.mult, op1=ALU.add), reads=[BU, Bcw], writes=[Ba])
                    for j in range(1, 31):
                        em.op("dve", lambda: nc.vector.scalar_tensor_tensor(out=a[:], in0=U[:, 2 + j:2 + j + T], scalar=cw[:, cc, j:j + 1], in1=a[:],
                                                                            op0=ALU.mult, op1=ALU.add), reads=[BU, Bcw, Ba], writes=[Ba])

                    def stats(a=a, Ba=Ba, cc=cc):
                        em.dma("sp", f"st_c{cc % 2}", S_cT[cc, :, :], a[:], reads=[Ba], writes=[B_d["cT"]])
                        em.op("pool", lambda: nc.gpsimd.tensor_tensor(out=sqb[:], in0=a[:], in1=a[:], op=ALU.mult), reads=[Ba], writes=[Bsq])
                        em.op("pool", lambda: nc.gpsimd.tensor_tensor(out=csum[:], in0=csum[:], in1=a[:], op=ALU.add), reads=[Ba, Bcs], writes=[Bcs])
                        em.op("pool", lambda: nc.gpsimd.tensor_tensor(out=csq[:], in0=csq[:], in1=sqb[:], op=ALU.add), reads=[Bsq, Bcq], writes=[Bcq])
                    pending_stats.append(stats)
                blocks_conv.append(dict(load=load, run=run))
            order = []
            for n in range(len(blocks_other)):
                order.append(blocks_other[n])
                if n < 16:
                    order.append(blocks_conv[n])
            G.run(order)
            flush_stats()
            act_only[0] = False
            mu = A_sb[:, 0:T]; rs = U[:, 0:T]
            Bmu = BA; Brs = BU
            for (t0, tn) in TG4:
                bk = G.next_bank()
                em.op("pe", lambda: nc.tensor.matmul(G.pb[bk][:, :], lhsT=onesf[:], rhs=csum[:, t0:t0 + tn], start=True, stop=True),
                      reads=[Bcs, B_c], writes=[G.Bp[bk]])
                em.op("dve", lambda: nc.vector.tensor_scalar(out=mu[:, t0:t0 + tn], in0=G.pb[bk][:, :], scalar1=1.0 / D, scalar2=None, op0=ALU.mult),
                      reads=[G.Bp[bk]], writes=[Bmu])
                bk = G.next_bank()
                em.op("pe", lambda: nc.tensor.matmul(G.pb[bk][:, :], lhsT=onesf[:], rhs=csq[:, t0:t0 + tn], start=True, stop=True),
                      reads=[Bcq, B_c], writes=[G.Bp[bk]])
                em.op("dve", lambda: nc.vector.tensor_scalar(out=rs[:, t0:t0 + tn], in0=G.pb[bk][:, :], scalar1=1.0 / D, scalar2=EPS, op0=ALU.mult, op1=ALU.add),
                      reads=[G.Bp[bk]], writes=[Brs])
            em.op("dve", lambda: nc.vector.tensor_tensor(out=sqb[:], in0=mu, in1=mu, op=ALU.mult), reads=[Bmu], writes=[Bsq])
            em.op("dve", lambda: nc.vector.tensor_tensor(out=rs, in0=rs, in1=sqb[:], op=ALU.subtract), reads=[Brs, Bsq], writes=[Brs])
            em.op("act", lambda: nc.scalar.activation(out=rs, in_=rs, func=AF.Sqrt), reads=[Brs], writes=[Brs])
            em.op("dve", lambda: nc.vector.reciprocal(out=rs, in_=rs), reads=[Brs], writes=[Brs])
            em.dma("sp", "st_mu", S_mu[:, :], mu, reads=[Bmu], writes=[B_d["mu"]])
            em.dma("sp", "st_mu", S_rs[:, :], rs, reads=[Brs], writes=[B_d["mu"]])
            em.barrier()

        checkpoint("B1")
        with ExitStack() as st:
            attnT = SB(st, "attnT", [128, 16, T], BF16)
            BattnT = Buf("attnT")
            qs = [SB(st, f"qs{i}", [128, 8, 256], BF16) for i in range(2)]
            ks = [SB(st, f"ks{i}", [128, 16, 256], BF16) for i in range(2)]
            vs = [SB(st, f"vs{i}", [128, 32, 136], BF16) for i in range(2)]
            bsf = [SB(st, f"bsf{i}", [128, 512], F32) for i in range(2)]
            baj = [SB(st, f"baj{i}", [128, 512], F32) for i in range(2)]
            Bhq = [Buf("hq0"), Buf("hq1")]; Bhk = [Buf("hk0"), Buf("hk1")]; Bhv = [Buf("hv0"), Buf("hv1")]; Bhb = [Buf("hb0"), Buf("hb1")]
            t31 = SB(st, "t31", [128, 16], F32)
            pastb = SB(st, "pastb", [128, 16, 16], F32)
            pastm = SB(st, "pastm", [128, 16, 16], F32)
            Bmk = Buf("masks")
            em.dma("sp", "ld_mk", t31[:], t31_d[:, :], writes=[Bmk])
            em.dma("sp", "ld_mk", pastb[:, :, :], pastb_d.rearrange("p (a b) -> p a b", b=16), writes=[Bmk])
            em.dma("sp", "ld_mk", pastm[:, :, :], pastm_d.rearrange("p (a b) -> p a b", b=16), writes=[Bmk])
            for i in range(2):
                em.op("dve", lambda: nc.vector.memset(vs[i][:, :, 128:136], 0.0), writes=[Bhv[i]])
                em.op("dve", lambda: nc.vector.memset(vs[i][:, :, 128:129], 1.0), writes=[Bhv[i]])
            km = SB(st, "km", [128, 16], F32); kmb = SB(st, "kmb", [128, 16], BF16); Bkm = Buf("km")
            gate = SB(st, "gate", [128, 16, 16], F32); Bgate = Buf("gate")
            m8 = SB(st, "m8", [128, 16, 8], F32); Bm8 = Buf("m8")
            selm = [SB(st, f"selm{i}", [128, 16, 16], F32) for i in range(2)]
            Bsel = [Buf("sel0"), Buf("sel1")]
            tmpb = [SB(st, f"tmpb{i}", [128, 512], F32) for i in range(2)]
            Btmp = [Buf("tmpb0"), Buf("tmpb1")]
            PT = [SB(st, f"PT{i}", [128, 512], BF16) for i in range(3)]
            BPT = [Buf(f"PT{i}") for i in range(3)]
            oacc = [SB(st, f"oacc{i}", [128, 2, 129], F32) for i in range(2)]
            Boacc = [Buf("oacc0"), Buf("oacc1")]
            rden = SB(st, "rden", [128, 2], F32); Brden = Buf("rden")
            obf = SB(st, "obf", [128, 2, 128], BF16); Bobf = Buf("obf")
            pS = [PS(st, f"pS{i}", [128, 512], F32) for i in range(3)]
            BpS = [Buf(f"pS{i}") for i in range(3)]
            pO = [PS(st, f"pO{i}", [128, 2, 256], F32) for i in range(3)]
            BpO = [Buf(f"pO{i}") for i in range(3)]
            pG = PS(st, "pG", [128, 32, 16], F32); BpG = Buf("pG")
            pTr = PS(st, "pTr", [128, 1024], BF16); BpTr = Buf("pTr")

            def head_load(h):
                s = h % 2
                em.dma("sp", f"ld_hq{s}", qs[s][:, :, :], S_qT[h].rearrange("p (j t) -> p j t", t=256), reads=[B_d["qT"]], writes=[Bhq[s]])
                em.dma("sp", f"ld_hk{s}", ks[s][:, :, :], S_kT[h].rearrange("p (j t) -> p j t", t=256), reads=[B_d["kT"]], writes=[Bhk[s]])
                em.dma("sp", f"ld_hv{s}", vs[s][:, :, 0:128], S_V[:, h * 128:(h + 1) * 128].rearrange("(kt p) d -> p kt d", p=128),
                       reads=[B_d["V"]], writes=[Bhv[s]])
                em.dma("sp", f"ld_hb{s}", bsf[s][:], bias_self[h], writes=[Bhb[s]])
                em.dma("sp", f"ld_hc{s}", baj[s][:], bias_adj[h], writes=[Bhb[s]])

            def head_prologue(h):
                s = h % 2
                for jq in range(4):
                    em.op("dve", lambda: nc.vector.tensor_reduce(out=km[:, jq * 4:(jq + 1) * 4], in_=ks[s][:, jq * 4:(jq + 1) * 4, :], axis=AX.X, op=ALU.add),
                          reads=[Bhk[s]], writes=[Bkm])
                    yield
                em.op("dve", lambda: nc.vector.tensor_scalar(out=kmb[:], in0=km[:], scalar1=1.0 / 256, scalar2=None, op0=ALU.mult), reads=[Bkm], writes=[Bkm])

                def f():
                    for qt in range(16):
                        ins = nc.tensor.matmul(pG[:, qt, :], lhsT=qs[s][:, qt // 2, (qt % 2) * 128:(qt % 2 + 1) * 128], rhs=kmb[:, :], start=True, stop=True)
                    return ins
                em.op("pe", f, reads=[Bhq[s], Bkm], writes=[BpG])
                yield
                em.op("dve", lambda: nc.vector.tensor_tensor(out=gate[:, :, :], in0=pG[:, 0:16, :], in1=pastb[:, :, :], op=ALU.add),
                      reads=[BpG, Bmk], writes=[Bgate])
                yield
                for qt in range(16):
                    em.op("dve", lambda: nc.vector.max(out=m8[:, qt, :], in_=gate[:, qt, :]), reads=[Bgate], writes=[Bm8])
                    if qt % 2:
                        yield
                sm = selm[s]
                for qt in range(16):
                    em.op("dve", lambda: nc.vector.tensor_scalar(out=sm[:, qt, :], in0=gate[:, qt, :], scalar1=m8[:, qt, 2:3], scalar2=None, op0=ALU.is_ge),
                          reads=[Bgate, Bm8], writes=[Bsel[s]])
                    if qt % 2:
                        yield
                em.op("dve", lambda: nc.vector.tensor_tensor(out=sm[:, :, :], in0=sm[:, :, :], in1=pastm[:, :, :], op=ALU.mult),
                      reads=[Bsel[s], Bmk], writes=[Bsel[s]])

            pairs = []
            for h in range(16):
                for i in range(8):
                    lst = [(h, i, i, 0)]
                    for j in range(i):
                        lst.append((h, i, j, 1 if j == i - 1 else 2))
                    for k in range(8):
                        lst.append((h, i, 8 + k, 1 if (i == 0 and k == 7) else 2))
                    for n, p in enumerate(lst):
                        pairs.append(p + (n == 0, n == len(lst) - 1))

            def emit_S(n):
                h, i, j, kind, first, last = pairs[n]
                s = h % 2
                b = n % 3

                def f():
                    for kt in range(2):
                        ins = nc.tensor.matmul(pS[b][:, kt * 256:(kt + 1) * 256], lhsT=ks[s][:, j, kt * 128:(kt + 1) * 128], rhs=qs[s][:, i, :],
                                               start=True, stop=True)
                    return ins
                em.op("pe", f, reads=[Bhq[s], Bhk[s]], writes=[BpS[b]])

            PLA = 14
            head_load(0)
            for _ in head_prologue(0):
                pass
            pro = [None]

            def pro_step(k):
                for _ in range(k):
                    if pro[0] is None:
                        return
                    try:
                        next(pro[0])
                    except StopIteration:
                        pro[0] = None
            emit_S(0)
            emit_S(1)
            for n in range(len(pairs)):
                h, i, j, kind, first, last = pairs[n]
                s = h % 2
                b = n % 3
                if first and i == 0 and h + 1 < 16:
                    head_load(h + 1)
                if n + PLA < len(pairs) and pairs[n + PLA][0] != pairs[n + PLA - 1][0]:
                    pro_step(1000)
                    pro[0] = head_prologue(pairs[n + PLA][0])
                if n + 3 < len(pairs) and pairs[n + 3][0] != h:
                    pro_step(1000)
                if n + 2 < len(pairs):
                    emit_S(n + 2)
                if kind == 2:
                    em.op("act", lambda: nc.scalar.activation(out=PT[b][:], in_=pS[b][:], func=AF.Exp, bias=t31[:, h:h + 1], scale=SCALE),
                          reads=[BpS[b], Bmk], writes=[BPT[b]])
                else:
                    tb = n % 2
                    btile = bsf[s] if kind == 0 else baj[s]
                    em.op("dve", lambda: nc.vector.scalar_tensor_tensor(out=tmpb[tb][:], in0=pS[b][:], scalar=SCALE, in1=btile[:], op0=ALU.mult, op1=ALU.add),
                          reads=[BpS[b], Bhb[s]], writes=[Btmp[tb]])
                    em.op("act", lambda: nc.scalar.activation(out=PT[b][:], in_=tmpb[tb][:], func=AF.Exp), reads=[Btmp[tb]], writes=[BPT[b]])

                def f():
                    for q2 in range(2):
                        for kt in range(2):
                            ins = nc.tensor.matmul(pO[b][:, q2, 0:132], lhsT=PT[b][:, kt * 256 + q2 * 128: kt * 256 + (q2 + 1) * 128],
                                                   rhs=vs[s][:, j * 2 + kt, 0:132], start=(kt == 0), stop=(kt == 1))
                    return ins
                em.op("pe", f, reads=[BPT[b], Bhv[s]], writes=[BpO[b]])
                oa = oacc[i % 2]
                Boa = Boacc[i % 2]
                if first:
                    em.op("dve", lambda: nc.vector.tensor_copy(out=oa[:, :, :], in_=pO[b][:, :, 0:129]), reads=[BpO[b]], writes=[Boa])
                else:
                    for q2 in range(2):
                        em.op("dve", lambda: nc.vector.scalar_tensor_tensor(out=oa[:, q2, :], in0=pO[b][:, q2, 0:129], scalar=selm[s][:, i * 2 + q2, j:j + 1],
                                                                            in1=oa[:, q2, :], op0=ALU.mult, op1=ALU.add),
                              reads=[BpO[b], Bsel[s], Boa], writes=[Boa])
                pro_step(2)
                if last:
                    em.op("dve", lambda: nc.vector.reciprocal(out=rden[:, :], in_=oa[:, :, 128]), reads=[Boa], writes=[Brden])
                    for q2 in range(2):
                        em.op("dve", lambda: nc.vector.tensor_scalar(out=obf[:, q2, :], in0=oa[:, q2, 0:128], scalar1=rden[:, q2:q2 + 1], scalar2=None, op0=ALU.mult),
                              reads=[Boa, Brden], writes=[Bobf])

                    def f():
                        for q2 in range(2):
                            ins = nc.tensor.transpose(out=pTr[:, q2 * 128:(q2 + 1) * 128], in_=obf[:, q2, :], identity=idb[:])
                        return ins
                    em.op("pe", f, reads=[Bobf, B_c], writes=[BpTr])
                    em.op("act", lambda: nc.scalar.copy(out=attnT[:, h, i * 256:(i + 1) * 256], in_=pTr[:, 0:256]), reads=[BpTr], writes=[BattnT])
            em.dma("sp", "st_at", S_attnT.rearrange("c p t -> p c t"), attnT[:, :, :], reads=[BattnT], writes=[B_d["attnT"]])
            em.barrier()

        checkpoint("C")
        with ExitStack() as st:
            D1_attnT = SB(st, "attnT1", [128, 16, T], BF16)
            D1_B = Buf("attnT1")
            em.dma("sp", "ld_act", D1_attnT[:, :, :], S_attnT.rearrange("c p t -> p c t"), reads=[B_d["attnT"]], writes=[D1_B])
            if True:
                st2 = st
                G = Gemm(st2, "d1")
                gsb = [SB(st2, f"gsb{i}", [128, T], F32) for i in range(2)]
                Bgsb = [Buf("gsb0"), Buf("gsb1")]
                zst = [SB(st2, f"zst{i}", [128, T], F32) for i in range(2)]
                Bzst = [Buf("zst0"), Buf("zst1")]
                blocks = []
                cnt = [0]
                for ob in range(4):
                    def load(slot, ob=ob):
                        G.wload(slot, 0, 512, w_attn[:, ob * 512:(ob + 1) * 512])

                    def run(slot, ob=ob):
                        for sub in range(4):
                            ch = ob * 4 + sub
                            s = cnt[0] % 2
                            cnt[0] += 1
                            em.dma("sp", f"ld_gs{s}", gsb[s][:], S_gaT[ch, :, :], reads=[B_d["gaT"]], writes=[Bgsb[s]])
                            for (t0, tn) in TG4:
                                pbk, Bp = G.mm_fm(slot, sub * 128, D1_attnT, D1_B, t0, tn)
                                em.op("dve", lambda: nc.vector.tensor_tensor(out=zst[s][:, t0:t0 + tn], in0=pbk[:, 0:tn], in1=gsb[s][:, t0:t0 + tn], op=ALU.mult),
                                      reads=[Bp, Bgsb[s]], writes=[Bzst[s]])
                            em.dma("sp", f"st_z{s}", S_z1T[ch, :, :], zst[s][:], reads=[Bzst[s]], writes=[B_d["z1T"]])
                    blocks.append(dict(load=load, run=run))
                G.run(blocks)
                em.barrier()

        checkpoint("D1")
        with ExitStack() as st:
            convT = SB(st, "convT", [128, 16, T], BF16)
            BconvT = Buf("convT")
            lg = SB(st, "lg", [128, 16], F32); lb = SB(st, "lb", [128, 16], F32); Blg = Buf("lg")
            em.dma("sp", "ld_lg", lg[:], ln_g[:, :], writes=[Blg])
            em.dma("sp", "ld_lg", lb[:], ln_b[:, :], writes=[Blg])
            mu = SB(st, "mu", [128, T], F32); rs = SB(st, "rs", [128, T], F32)
            Bmu = Buf("mu"); Brs = Buf("rs")
            em.dma("sp", "ld_mu", mu[:], S_mu[:, :], reads=[B_d["mu"]], writes=[Bmu])
            em.dma("sp", "ld_rs", rs[:], S_rs[:, :], reads=[B_d["mu"]], writes=[Brs])
            with ExitStack() as st2:
                cl = [SB(st2, f"cl{i}", [128, T], F32) for i in range(2)]
                Bcl = [Buf("cl0"), Buf("cl1")]
                for cc in range(16):
                    s = cc % 2
                    em.dma("sp", f"ld_cl{s}", cl[s][:], S_cT[cc, :, :], reads=[B_d["cT"]], writes=[Bcl[s]])
                    em.op("dve", lambda: nc.vector.tensor_tensor(out=cl[s][:], in0=cl[s][:], in1=mu[:], op=ALU.subtract), reads=[Bcl[s], Bmu], writes=[Bcl[s]])
                    em.op("dve", lambda: nc.vector.tensor_tensor(out=cl[s][:], in0=cl[s][:], in1=rs[:], op=ALU.mult), reads=[Bcl[s], Brs], writes=[Bcl[s]])
                    em.op("act", lambda: nc.scalar.activation(out=convT[:, cc, :], in_=cl[s][:], func=AF.Silu, scale=lg[:, cc:cc + 1], bias=lb[:, cc:cc + 1]),
                          reads=[Bcl[s], Blg], writes=[BconvT])
                if debug and "S_convT" in debug:
                    for cc in range(16):
                        em.dma("sp", "st_dbg", S_convT[cc, :, :], convT[:, cc, :], reads=[BconvT], writes=[Buf("dbg")])
                em.barrier()
            with ExitStack() as st2:
                G = Gemm(st2, "d2")
                gsb = [SB(st2, f"gsc{i}", [128, T], F32) for i in range(2)]
                Bgsb = [Buf("gsc0"), Buf("gsc1")]
                z1b = [SB(st2, f"z1b{i}", [128, T], F32) for i in range(2)]
                Bz1b = [Buf("z1b0"), Buf("z1b1")]
                zst = [SB(st2, f"zsb{i}", [128, T], BF16) for i in range(2)]
                Bzst = [Buf("zsb0"), Buf("zsb1")]
                tmpz = [SB(st2, f"tmpz{i}", [128, 512], F32) for i in range(2)]
                Btz = [Buf("tmpz0"), Buf("tmpz1")]
                blocks = []
                cnt = [0, 0]
                for ob in range(4):
                    def load(slot, ob=ob):
                        G.wload(slot, 0, 512, w_conv[:, ob * 512:(ob + 1) * 512])

                    def run(slot, ob=ob):
                        for sub in range(4):
                            ch = ob * 4 + sub
                            s = cnt[0] % 2
                            cnt[0] += 1
                            em.dma("sp", f"ld_gs{s}", gsb[s][:], S_gcT[ch, :, :], reads=[B_d["gcT"]], writes=[Bgsb[s]])
                            em.dma("sp", f"ld_z1{s}", z1b[s][:], S_z1T[ch, :, :], reads=[B_d["z1T"]], writes=[Bz1b[s]])
                            for (t0, tn) in TG4:
                                pbk, Bp = G.mm_fm(slot, sub * 128, convT, BconvT, t0, tn)
                                u = cnt[1] % 2
                                cnt[1] += 1
                                em.op("dve", lambda: nc.vector.tensor_tensor(out=tmpz[u][:, 0:tn], in0=pbk[:, 0:tn], in1=gsb[s][:, t0:t0 + tn], op=ALU.mult),
                                      reads=[Bp, Bgsb[s]], writes=[Btz[u]])
                                em.op("dve", lambda: nc.vector.tensor_tensor(out=zst[s][:, t0:t0 + tn], in0=tmpz[u][:, 0:tn], in1=z1b[s][:, t0:t0 + tn], op=ALU.add),
                                      reads=[Btz[u], Bz1b[s]], writes=[Bzst[s]])
                            em.dma("sp", f"st_z{s}", S_zT[ch, :, :], zst[s][:], reads=[Bzst[s]], writes=[B_d["zT"]])
                    blocks.append(dict(load=load, run=run))
                G.run(blocks)
                em.barrier()

        checkpoint("B3")
        def resid_gemm(st, tag, actT, Bact, w_ap, res_src, res_buf, dst, dst_key):
            G = Gemm(st, tag)
            xsl = [SB(st, f"xsl{tag}{i}", [128, 512], F32) for i in range(3)]
            Bxsl = [Buf(f"xsl{i}") for i in range(3)]
            hst = [SB(st, f"hst{tag}{i}", [128, 512], F32) for i in range(2)]
            Bhst = [Buf("hst0"), Buf("hst1")]
            blocks = []
            cnt = [0]
            for ob in range(4):
                def load(slot, ob=ob):
                    G.wload(slot, 0, 512, w_ap[:, ob * 512:(ob + 1) * 512])

                def run(slot, ob=ob):
                    def ldx(t):
                        em.dma("sp", f"ld_xsl{t % 3}", xsl[t % 3][:], res_src(t, ob), reads=res_buf, writes=[Bxsl[t % 3]])
                    ldx(0)
                    ldx(1)
                    for t in range(16):
                        if t + 2 < 16:
                            ldx(t + 2)
                        pbk, Bp = G.mm_tm(slot, actT, Bact, t)
                        s = cnt[0] % 2
                        cnt[0] += 1
                        em.op("dve", lambda: nc.vector.tensor_tensor(out=hst[s][:], in0=pbk[:, :], in1=xsl[t % 3][:], op=ALU.add),
                              reads=[Bp, Bxsl[t % 3]], writes=[Bhst[s]])
                        em.dma("sp", f"st_h{s}", dst[t * 128:(t + 1) * 128, ob * 512:(ob + 1) * 512], hst[s][:], reads=[Bhst[s]], writes=[B_d[dst_key]])
                blocks.append(dict(load=load, run=run))
            G.run(blocks)

        with ExitStack() as st:
            zT = SB(st, "zTa", [128, 16, T], BF16)
            BzT = Buf("zTa")
            em.dma("sp", "ld_act", zT[:, :, :], S_zT.rearrange("c p t -> p c t"), reads=[B_d["zT"]], writes=[BzT])
            resid_gemm(st, "e", zT, BzT, w_o, lambda t, ob: xa[t * 128:(t + 1) * 128, ob * 512:(ob + 1) * 512], [], S_h1, "h1")
            em.barrier()

        checkpoint("E")
        with ExitStack() as st:
            x2T = SB(st, "x2T", [128, 16, T], BF16)
            Bx2T = Buf("x2T")
            combT = SB(st, "combT", [16, T], F32)
            BcombT = Buf("combT")
            sel_sb = SB(st, "sel_sb", [16, D], F32)
            Bsl = Buf("sel_sb")
            em.dma("sp", "ld_sl", sel_sb[:], sel16_d[:, :], writes=[Bsl])
            with ExitStack() as s1:
                src = lambda t: S_h1[t * 128:(t + 1) * 128, :]
                rstd, Bss = rms_stats(s1, src, 16, "f")
                gB = SB(s1, "gBf", [128, D], F32); BgB = Buf("gBf")
                em.dma("sp", "ld_g", gB[:], g_ffn.partition_broadcast(128), writes=[BgB])
                wr = SB(s1, "wr", [128, 16, 20], F32); Bwr = Buf("wr")
                em.dma("sp", "ld_wr", wr[:], w_r.rearrange("(k p) n -> p k n", p=128), writes=[Bwr])
                brb = SB(s1, "brb", [128, 20], F32)
                em.dma("sp", "ld_wr", brb[:], b_r.partition_broadcast(128), writes=[Bwr])
                xt = [SB(s1, f"xtf{i}", [128, D], F32) for i in range(2)]
                Bx = [Buf("xtf0"), Buf("xtf1")]
                xn = [SB(s1, f"xnf{i}", [128, D], F32) for i in range(2)]
                Bxn = [Buf("xnf0"), Buf("xnf1")]
                xf = [SB(s1, f"xf{i}", [128, 16, 128], F32) for i in range(2)]
                Bxf = [Buf("xf0"), Buf("xf1")]
                pF = [PS(s1, f"pF{i}", [128, 4, 128], F32) for i in range(4)]
                BpF = [Buf(f"pF{i}") for i in range(4)]
                pR = PS(s1, "pR", [128, 512], F32); BpR = Buf("pR")
                pC = PS(s1, "pC", [128, 512], F32); BpC = Buf("pC")
                comb = SB(s1, "comb", [128, 16, 16], F32); Bcomb = Buf("comb")
                R = {k: SB(s1, "r_" + k, [128, n], F32) for k, n in
                     dict(L=20, cmax=1, ncmax=1, ohg=4, ecl=4, esum=1, pg=1, fsel=4, v1=1, m1=4, fs2=4, v2=1, m2=4, d=1, e=1, den=1,
                          w1=1, w2=1, t1=4, fine=4, pf=4).items()}
                BR = Buf("router_scratch")

                def dv(fn, reads=(), writes=()):
                    em.op("dve", fn, reads=[BR] + list(reads), writes=[BR] + list(writes))

                for t in range(16):
                    s = t % 2
                    em.dma("sp", f"ld_xt{s}", xt[s][:], S_h1[t * 128:(t + 1) * 128, :], reads=[B_d["h1"]], writes=[Bx[s]])
                    em.op("dve", lambda: nc.vector.scalar_tensor_tensor(out=xn[s][:], in0=xt[s][:], scalar=rstd[:, t:t + 1], in1=gB[:], op0=ALU.mult, op1=ALU.mult),
                          reads=[Bx[s], Bss, BgB], writes=[Bxn[s]])
                    for k in range(4):
                        def f():
                            for j in range(4):
                                c = k * 4 + j
                                ins = nc.tensor.transpose(out=pF[k][:, j, :], in_=xn[s][:, c * 128:(c + 1) * 128], identity=idf[:])
                            return ins
                        em.op("pe", f, reads=[Bxn[s], B_c], writes=[BpF[k]])
                        em.op("dve", lambda: nc.vector.tensor_copy(out=xf[s][:, k * 4:(k + 1) * 4, :], in_=pF[k][:, :, :]), reads=[BpF[k]], writes=[Bxf[s]])
                        em.op("act", lambda: nc.scalar.copy(out=x2T[:, k * 4:(k + 1) * 4, t * 128:(t + 1) * 128], in_=xf[s][:, k * 4:(k + 1) * 4, :]), reads=[Bxf[s]], writes=[Bx2T])

                    def f():
                        for c in range(16):
                            ins = nc.tensor.matmul(pR[:, 0:20], lhsT=xf[s][:, c, :], rhs=wr[:, c, :], start=(c == 0), stop=(c == 15))
                        return ins
                    em.op("pe", f, reads=[Bxf[s], Bwr], writes=[BpR])
                    L = R["L"]
                    dv(lambda: nc.vector.tensor_tensor(out=L[:], in0=pR[:, 0:20], in1=brb[:], op=ALU.add), reads=[BpR, Bwr])
                    dv(lambda: nc.vector.tensor_reduce(out=R["cmax"][:], in_=L[:, 0:4], axis=AX.X, op=ALU.max))
                    dv(lambda: nc.vector.tensor_scalar(out=R["ncmax"][:], in0=R["cmax"][:], scalar1=-1.0, scalar2=None, op0=ALU.mult))
                    dv(lambda: nc.vector.tensor_scalar(out=R["ohg"][:], in0=L[:, 0:4], scalar1=R["cmax"][:, 0:1], scalar2=None, op0=ALU.is_ge))
                    em.op("act", lambda: nc.scalar.activation(out=R["ecl"][:], in_=L[:, 0:4], func=AF.Exp, bias=R["ncmax"][:, 0:1], scale=1.0),
                          reads=[BR], writes=[BR])
                    dv(lambda: nc.vector.tensor_reduce(out=R["esum"][:], in_=R["ecl"][:], axis=AX.X, op=ALU.add))
                    dv(lambda: nc.vector.reciprocal(out=R["pg"][:], in_=R["esum"][:]))
                    dv(lambda: nc.vector.tensor_scalar(out=R["fsel"][:], in0=L[:, 4:8], scalar1=R["ohg"][:, 0:1], scalar2=None, op0=ALU.mult))
                    for g in range(1, 4):
                        dv(lambda: nc.vector.scalar_tensor_tensor(out=R["fsel"][:], in0=L[:, 4 + 4 * g:8 + 4 * g], scalar=R["ohg"][:, g:g + 1], in1=R["fsel"][:],
                                                                  op0=ALU.mult, op1=ALU.add))
                    dv(lambda: nc.vector.tensor_reduce(out=R["v1"][:], in_=R["fsel"][:], axis=AX.X, op=ALU.max))
                    dv(lambda: nc.vector.tensor_scalar(out=R["m1"][:], in0=R["fsel"][:], scalar1=R["v1"][:, 0:1], scalar2=None, op0=ALU.is_ge))
                    dv(lambda: nc.vector.scalar_tensor_tensor(out=R["fs2"][:], in0=R["m1"][:], scalar=NEG, in1=R["fsel"][:], op0=ALU.mult, op1=ALU.add))
                    dv(lambda: nc.vector.tensor_reduce(out=R["v2"][:], in_=R["fs2"][:], axis=AX.X, op=ALU.max))
                    dv(lambda: nc.vector.tensor_scalar(out=R["m2"][:], in0=R["fs2"][:], scalar1=R["v2"][:, 0:1], scalar2=None, op0=ALU.is_ge))
                    dv(lambda: nc.vector.tensor_tensor(out=R["d"][:], in0=R["v2"][:], in1=R["v1"][:], op=ALU.subtract))
                    em.op("act", lambda: nc.scalar.activation(out=R["e"][:], in_=R["d"][:], func=AF.Exp), reads=[BR], writes=[BR])
                    dv(lambda: nc.vector.tensor_scalar(out=R["den"][:], in0=R["e"][:], scalar1=1.0, scalar2=None, op0=ALU.add))
                    dv(lambda: nc.vector.reciprocal(out=R["w1"][:], in_=R["den"][:]))
                    dv(lambda: nc.vector.tensor_tensor(out=R["w2"][:], in0=R["e"][:], in1=R["w1"][:], op=ALU.mult))
                    dv(lambda: nc.vector.tensor_scalar(out=R["t1"][:], in0=R["m1"][:], scalar1=R["w1"][:, 0:1], scalar2=None, op0=ALU.mult))
                    dv(lambda: nc.vector.scalar_tensor_tensor(out=R["fine"][:], in0=R["m2"][:], scalar=R["w2"][:, 0:1], in1=R["t1"][:], op0=ALU.mult, op1=ALU.add))
                    dv(lambda: nc.vector.tensor_scalar(out=R["pf"][:], in0=R["fine"][:], scalar1=R["pg"][:, 0:1], scalar2=None, op0=ALU.mult))
                    for g in range(4):
                        dv(lambda: nc.vector.tensor_scalar(out=comb[:, t, 4 * g:4 * g + 4], in0=R["pf"][:], scalar1=R["ohg"][:, g:g + 1], scalar2=None, op0=ALU.mult),
                           writes=[Bcomb])
                    em.op("pe", lambda: nc.tensor.matmul(pC[0:16, 0:128], lhsT=comb[:, t, :], rhs=idf[:], start=True, stop=True), reads=[Bcomb, B_c], writes=[BpC])
                    em.op("act", lambda: nc.scalar.copy(out=combT[:, t * 128:(t + 1) * 128], in_=pC[0:16, 0:128]), reads=[BpC], writes=[BcombT])
                if debug and "S_comb" in debug:
                    em.dma("sp", "st_dbg", S_comb[:, :, :], comb[:, :, :], reads=[Bcomb], writes=[Buf("dbg")])
                em.barrier()
                checkpoint("F1")
            with ExitStack() as s1:
                G = Gemm(s1, "f1")
                pCB = PS(s1, "pCB", [128, 512], F32); BpCB = Buf("pCB")
                combB = [SB(s1, f"combB{i}", [128, T], F32) for i in range(2)]
                BcombB = [Buf("combB0"), Buf("combB1")]
                sg = [SB(s1, f"sg{i}", [128, T], F32) for i in range(4)]
                Bsg = [Buf(f"sg{i}") for i in range(4)]
                hst = [SB(s1, f"hstb{i}", [128, T], BF16) for i in range(2)]
                Bhst = [Buf("hstb0"), Buf("hstb1")]
                tmph = [SB(s1, f"tmph{i}", [128, 512], F32) for i in range(2)]
                Btmph = [Buf("tmph0"), Buf("tmph1")]
                blocks = []
                cnt = [0, 0]
                for e in range(16):
                    for fp in range(2):
                        def load(slot, e=e, fp=fp):
                            G.wload(slot, 0, 256, w_eg[e, :, fp * 256:(fp + 1) * 256])
                            G.wload(slot, 256, 512, w_eu[e, :, fp * 256:(fp + 1) * 256])

                        def run(slot, e=e, fp=fp):
                            cbs = combB[e % 2]
                            if fp == 0:
                                for (t0, tn) in TG4:
                                    em.op("pe", lambda: nc.tensor.matmul(pCB[:, 0:tn], lhsT=sel_sb[:, e * 128:(e + 1) * 128], rhs=combT[:, t0:t0 + tn], start=True, stop=True),
                                          reads=[Bsl, BcombT], writes=[BpCB])
                                    em.op("act", lambda: nc.scalar.copy(out=cbs[:, t0:t0 + tn], in_=pCB[:, 0:tn]), reads=[BpCB], writes=[BcombB[e % 2]])
                            par = (e * 2 + fp) % 2
                            for fl in range(2):
                                for (t0, tn) in TG4:
                                    pbk, Bp = G.mm_fm(slot, fl * 128, x2T, Bx2T, t0, tn)
                                    em.op("act", lambda: nc.scalar.activation(out=sg[par * 2 + fl][:, t0:t0 + tn], in_=pbk[:, 0:tn], func=AF.Silu),
                                          reads=[Bp], writes=[Bsg[par * 2 + fl]])
                            for fl in range(2):
                                s = cnt[0] % 2
                                cnt[0] += 1
                                for (t0, tn) in TG4:
                                    pbk, Bp = G.mm_fm(slot, 256 + fl * 128, x2T, Bx2T, t0, tn)
                                    u = cnt[1] % 2
                                    cnt[1] += 1
                                    em.op("dve", lambda: nc.vector.tensor_tensor(out=tmph[u][:, 0:tn], in0=pbk[:, 0:tn], in1=sg[par * 2 + fl][:, t0:t0 + tn], op=ALU.mult),
                                          reads=[Bp, Bsg[par * 2 + fl]], writes=[Btmph[u]])
                                    em.op("dve", lambda: nc.vector.tensor_tensor(out=hst[s][:, t0:t0 + tn], in0=tmph[u][:, 0:tn], in1=cbs[:, t0:t0 + tn], op=ALU.mult),
                                          reads=[Btmph[u], BcombB[e % 2]], writes=[Bhst[s]])
                                em.dma("sp", f"st_H{s}", S_HT[e * 4 + fp * 2 + fl, :, :], hst[s][:], reads=[Bhst[s]], writes=[B_d["HT"]])
                        blocks.append(dict(load=load, run=run))
                G.run(blocks)
                em.barrier()
                checkpoint("F2")
        with ExitStack() as st:
            G = Gemm(st, "f2", nbanks=8, nw=3)
            HTq = [SB(st, f"HTq{i}", [128, 16, 512], BF16) for i in range(4)]
            BHTq = [Buf(f"HTq{i}") for i in range(4)]
            xsl = [SB(st, f"xslf{i}", [128, 512], F32) for i in range(4)]
            Bxsl = [Buf(f"xslf{i}") for i in range(4)]
            hst = [SB(st, f"hstf{i}", [128, 512], F32) for i in range(2)]
            Bhst = [Buf("hstf0"), Buf("hstf1")]
            S_HTv = S_HT.rearrange("c p t -> p c t")
            blocks = []
            cnt = [0]

            def ldH(Gi, kq):
                em.dma("sp", f"ld_HT{kq}", HTq[kq][:, :, :], S_HTv[:, kq * 16:(kq + 1) * 16, Gi * 512:(Gi + 1) * 512], reads=[B_d["HT"]], writes=[BHTq[kq]])
            for kq in range(4):
                ldH(0, kq)
            for Gi in range(4):
                for cb in range(4):
                    for kq in range(4):
                        def load(slot, cb=cb, kq=kq):
                            G.wload(slot, 0, 512, w_ed[kq * 2048:(kq + 1) * 2048, cb * 512:(cb + 1) * 512])

                        def run(slot, Gi=Gi, cb=cb, kq=kq):
                            base = (cb % 2) * 4
                            if kq == 0:
                                for tl in range(4):
                                    em.dma("sp", f"ld_xslf{tl}", xsl[tl][:], S_h1[(Gi * 4 + tl) * 128:(Gi * 4 + tl + 1) * 128, cb * 512:(cb + 1) * 512],
                                           reads=[B_d["h1"]], writes=[Bxsl[tl]])
                            for tl in range(4):
                                G.mm_tm(slot, HTq[kq], BHTq[kq], tl, bk=base + tl, first=(kq == 0), last=(kq == 3))
                            if cb == 3 and Gi + 1 < 4:
                                ldH(Gi + 1, kq)
                            if kq == 3:
                                for tl in range(4):
                                    s = cnt[0] % 2
                                    cnt[0] += 1
                                    em.op("dve", lambda: nc.vector.tensor_tensor(out=hst[s][:], in0=G.pb[base + tl][:, :], in1=xsl[tl][:], op=ALU.add),
                                          reads=[G.Bp[base + tl], Bxsl[tl]], writes=[Bhst[s]])
                                    em.dma("sp", f"st_h{s}", S_h2[(Gi * 4 + tl) * 128:(Gi * 4 + tl + 1) * 128, cb * 512:(cb + 1) * 512], hst[s][:],
                                           reads=[Bhst[s]], writes=[B_d["h2"]])
                        blocks.append(dict(load=load, run=run))
            G.run(blocks)
            em.barrier()

        checkpoint("F")
        with ExitStack() as st:
            x3T = SB(st, "x3T", [128, 16, T], BF16)
            Bx3T = Buf("x3T")
            norm_T(st, lambda t: S_h2[t * 128:(t + 1) * 128, :], 16, g_ple, x3T, Bx3T, "g")
            pT = SB(st, "pT", [128, 2, T], BF16); BpT = Buf("pT")
            wp = SB(st, "wp", [128, 2, D], BF16); Bwp = Buf("wp")
            em.dma("pool", "ld_wp", wp[:, :, :], w_pp.rearrange("(k p) n -> p k n", p=128), writes=[Bwp])
            with ExitStack() as s1:
                pl = [SB(s1, f"pl{i}", [128, 256], F32) for i in range(2)]
                Bpl = [Buf("pl0"), Buf("pl1")]
                plb = [SB(s1, f"plb{i}", [128, 256], BF16) for i in range(2)]
                Bplb = [Buf("plb0"), Buf("plb1")]
                ptp = PS(s1, "ptp", [128, 8, 128], BF16); Bptp = Buf("ptp")
                for t in range(16):
                    s = t % 2
                    em.dma("sp", f"ld_pl{s}", pl[s][:], pp[t * 128:(t + 1) * 128, :], writes=[Bpl[s]])
                    em.op("dve", lambda: nc.vector.tensor_copy(out=plb[s][:], in_=pl[s][:]), reads=[Bpl[s]], writes=[Bplb[s]])

                    def f():
                        for j in range(2):
                            ins = nc.tensor.transpose(out=ptp[:, j, :], in_=plb[s][:, j * 128:(j + 1) * 128], identity=idb[:])
                        return ins
                    em.op("pe", f, reads=[Bplb[s], B_c], writes=[Bptp])
                    em.op("act", lambda: nc.scalar.copy(out=pT[:, :, t * 128:(t + 1) * 128], in_=ptp[:, 0:2, :]), reads=[Bptp], writes=[BpT])
                em.barrier()
            G = Gemm(st, "g")
            pP = [PS(st, f"pP{i}", [128, 512], F32) for i in range(2)]
            BpP = [Buf("pP0"), Buf("pP1")]
            xsl = [SB(st, f"xslg{i}", [128, 512], F32) for i in range(3)]
            Bxsl = [Buf(f"xslg{i}") for i in range(3)]
            sgg = [SB(st, f"sgg{i}", [128, 512], F32) for i in range(2)]
            Bsgg = [Buf("sgg0"), Buf("sgg1")]
            hst = [SB(st, f"hstg{i}", [128, 512], F32) for i in range(2)]
            Bhst = [Buf("hstg0"), Buf("hstg1")]
            blocks = []
            cnt = [0]
            for ob in range(4):
                def load(slot, ob=ob):
                    G.wload(slot, 0, 512, w_pg[:, ob * 512:(ob + 1) * 512])

                def run(slot, ob=ob):
                    def ldx(t):
                        em.dma("sp", f"ld_xsl{t % 3}", xsl[t % 3][:], S_h2[t * 128:(t + 1) * 128, ob * 512:(ob + 1) * 512], reads=[B_d["h2"]], writes=[Bxsl[t % 3]])
                    ldx(0)
                    ldx(1)
                    for t in range(16):
                        if t + 2 < 16:
                            ldx(t + 2)
                        pbk, Bp = G.mm_tm(slot, x3T, Bx3T, t)
                        s = cnt[0] % 2
                        cnt[0] += 1

                        def f():
                            for kc in range(2):
                                ins = nc.tensor.matmul(pP[s][:, :], lhsT=pT[:, kc, t * 128:(t + 1) * 128], rhs=wp[:, kc, ob * 512:(ob + 1) * 512],
                                                       start=(kc == 0), stop=(kc == 1))
                            return ins
                        em.op("pe", f, reads=[BpT, Bwp], writes=[BpP[s]])
                        em.op("act", lambda: nc.scalar.activation(out=sgg[s][:], in_=pbk[:, :], func=AF.Sigmoid), reads=[Bp], writes=[Bsgg[s]])
                        em.op("dve", lambda: nc.vector.tensor_tensor(out=hst[s][:], in0=pP[s][:, :], in1=sgg[s][:], op=ALU.mult),
                              reads=[BpP[s], Bsgg[s]], writes=[Bhst[s]])
                        em.op("dve", lambda: nc.vector.tensor_tensor(out=hst[s][:], in0=hst[s][:], in1=xsl[t % 3][:], op=ALU.add),
                              reads=[Bxsl[t % 3], Bhst[s]], writes=[Bhst[s]])
                        em.dma("sp", f"st_h{s}", S_h3[t * 128:(t + 1) * 128, ob * 512:(ob + 1) * 512], hst[s][:], reads=[Bhst[s]], writes=[B_d["h3"]])
                blocks.append(dict(load=load, run=run))
            G.run(blocks)
            em.barrier()

        checkpoint("G")
        with ExitStack() as st:
            src = lambda t: S_h3[t * 128:(t + 1) * 128, :]
            rstd, Bss = rms_stats(st, src, 16, "h")
            gB = SB(st, "gBh", [128, D], F32); BgB = Buf("gBh")
            em.dma("sp", "ld_g", gB[:], g_fin.partition_broadcast(128), writes=[BgB])
            xt = [SB(st, f"xth{i}", [128, D], F32) for i in range(2)]
            Bx = [Buf("xth0"), Buf("xth1")]
            yo = [SB(st, f"yo{i}", [128, D], F32) for i in range(2)]
            Byo = [Buf("yo0"), Buf("yo1")]
            for t in range(16):
                s = t % 2
                em.dma("sp", f"ld_xt{s}", xt[s][:], src(t), reads=[B_d["h3"]], writes=[Bx[s]])
                em.op("dve", lambda: nc.vector.scalar_tensor_tensor(out=yo[s][:], in0=xt[s][:], scalar=rstd[:, t:t + 1], in1=gB[:], op0=ALU.mult, op1=ALU.mult),
                      reads=[Bx[s], Bss, BgB], writes=[Byo[s]])
                em.dma("sp", f"st_o{s}", out_d[t * 128:(t + 1) * 128, :], yo[s][:], reads=[Byo[s]], writes=[B_d["out"]])
            em.barrier()
        es.close()
      except _Stop:
        em.barrier()
        try:
            es.close()
        except AssertionError:
            pass
    return nc


def _rel_bucket(dist):
    n = np.maximum(dist, 0)
    max_exact = 16
    nf = np.maximum(n, 1).astype(np.float32)
    large = max_exact + (np.log(nf / np.float32(max_exact)) / np.float32(math.log(128 / max_exact)) * np.float32(32 - max_exact)).astype(np.int32)
    large = np.minimum(large, 31)
    return np.where(n < max_exact, n, large)


def make_in_maps(inp):
    f = lambda k: np.ascontiguousarray(np.asarray(inp[k], dtype=np.float32))
    x = f("x"); p = f("p")[0]
    rel_bias = f("rel_bias")
    kk = np.arange(256)[:, None]
    qq = np.arange(256)[None, :]
    bs = rel_bias[_rel_bucket(qq - kk)]
    bs = np.where((qq >= kk)[:, :, None], bs, np.float32(NEG)).astype(np.float32)
    ba = rel_bias[_rel_bucket(qq + 256 - kk)].astype(np.float32)

    def lay(b):
        b = b.transpose(2, 0, 1).reshape(16, 2, 128, 256).transpose(0, 2, 1, 3).reshape(16, 128, 512)
        return np.ascontiguousarray(b)
    bias_self = lay(bs); bias_adj = lay(ba)
    t31 = np.ascontiguousarray(np.broadcast_to(rel_bias[31][None, :], (128, 16))).astype(np.float32)
    colvec = lambda v: np.ascontiguousarray(v.reshape(16, 128).T)
    shared = {
        "w_in": f("w_in")[0], "w_attn_br": f("w_attn_br")[0], "w_conv_br": f("w_conv_br")[0], "w_o": f("w_o")[0],
        "w_ple_gate": f("w_ple_gate")[0], "w_ple_proj": f("w_ple_proj")[0],
        "w_e_gate": f("w_e_gate")[0], "w_e_up": f("w_e_up")[0], "w_e_down": f("w_e_down")[0].reshape(16 * 512, D),
        "w_r": np.ascontiguousarray(np.concatenate([f("w_router_g")[0], f("w_router_e")[0]], axis=1)),
        "b_r": np.ascontiguousarray(np.concatenate([f("b_router_g")[0], f("b_router_e")[0]])[None, :]),
        "g_mix": f("g_mix"), "g_ffn": f("g_ffn"), "g_ple": f("g_ple"), "g_final": f("g_final")[None, :],
        "conv_wT": np.ascontiguousarray(f("conv_w")[0].T.reshape(16, 128, 31).transpose(1, 0, 2)),
        "conv_b": colvec(f("conv_b")[0]), "ln_g": colvec(f("ln_g")[0]), "ln_b": colvec(f("ln_b")[0]),
        "ident": np.eye(128, dtype=np.float32),
        "sel16": np.ascontiguousarray(np.repeat(np.eye(16, dtype=np.float32), 128, axis=1)),
        "bias_self": bias_self, "bias_adj": bias_adj, "t31": t31,
    }
    maps = []
    for c in range(8):
        b, hf = c // 2, c % 2
        own = x[b, hf * T:(hf + 1) * T]
        oth = x[b, (1 - hf) * T:(2 - hf) * T]
        halo = np.zeros((128, D), np.float32)
        if hf == 1:
            halo[96:128] = x[b, T - 32:T]
        xa = np.concatenate([own, oth, halo], axis=0)
        gbl = np.concatenate([np.arange(8) + 8 * hf, np.arange(8) + 8 * (1 - hf)])
        past = (gbl[None, :] < (np.arange(8) + 8 * hf)[:, None])
        past_q = np.repeat(past, 2, axis=0)
        pastm = np.broadcast_to(past_q.astype(np.float32).reshape(1, 256), (128, 256))
        pastb = np.where(pastm > 0, np.float32(0), np.float32(NEG)).astype(np.float32)
        m = dict(shared)
        m.update({"xa": np.ascontiguousarray(xa), "pp": np.ascontiguousarray(p[b, hf * T:(hf + 1) * T]),
                  "pastm": np.ascontiguousarray(pastm), "pastb": np.ascontiguousarray(pastb)})
        maps.append(m)
    return maps


_NC = {}


def kernel(**inputs):
    if "nc" not in _NC:
        _NC["nc"] = build(False)
    maps = make_in_maps(inputs)
    res = run_bass_kernel_spmd(_NC["nc"], maps, core_ids=list(range(8)))
    out = np.empty((4, 4096, D), np.float32)
    for c in range(8):
        b, hf = c // 2, c % 2
        out[b, hf * T:(hf + 1) * T] = np.asarray(res.results[c]["out"], dtype=np.float32)
    return out
```
